# Optimizing a Trainium2 kernel written in Bass

```python
import math
import jax, jax.numpy as jnp
from jax import lax
import numpy as np

D_MODEL = 1024
BATCH = 4
SEQ = 8192
DEPTH = 2

D_MIX = 1024
MLA_HEADS = 4
MLA_NOPE = 128
MLA_ROPE = 64
MLA_V = 128
MLA_QK = MLA_NOPE + MLA_ROPE
MLA_Q_LORA = 384
MLA_KV_LORA = 256
MLA_WIDTH = MLA_HEADS * MLA_V
ROPE_THETA = 10000.0
Q_BLOCK = 128
GLA_HEADS = 4
GLA_DK = 32
GLA_DV = 64
GLA_QK_WIDTH = GLA_HEADS * GLA_DK
GLA_WIDTH = GLA_HEADS * GLA_DV
GLA_GATE_RANK = 16
GLA_GATE_NORM = 16.0
GLA_CHUNK = 64
RWKV_HEADS = 4
RWKV_N = 64
RWKV_WIDTH = RWKV_HEADS * RWKV_N
RWKV_DECAY_RANK = 64
RWKV_A_RANK = 64
RWKV_GATE_RANK = 128
RWKV_LN_EPS = 64e-5
MLA_COLS = MLA_Q_LORA + MLA_KV_LORA + MLA_ROPE
GLA_COLS = 2 * GLA_QK_WIDTH + GLA_WIDTH + GLA_GATE_RANK + GLA_WIDTH
RWKV_COLS = 3 * RWKV_WIDTH + RWKV_DECAY_RANK + RWKV_A_RANK + RWKV_GATE_RANK
D_IN = MLA_COLS + GLA_COLS + RWKV_COLS
D_FF = 2816
CONV_WIDTH = 3
NORM_EPS = 1e-6

kernel_name = "hybrid_mla_gla_rwkv7_convffn"


def _split(t, sizes):
    out, start = [], 0
    for s in sizes:
        out.append(t[..., start:start + s])
        start += s
    return out


def rms_norm(x, g, eps=NORM_EPS):
    xf = x.astype(jnp.float32)
    y = xf * lax.rsqrt(jnp.mean(xf * xf, axis=-1, keepdims=True) + eps)
    return (y * g.astype(jnp.float32)).astype(x.dtype)


def rope_tables(seq, dim):
    inv = 1.0 / (ROPE_THETA ** (jnp.arange(0, dim, 2, dtype=jnp.float32) / dim))
    ang = jnp.arange(seq, dtype=jnp.float32)[:, None] * inv[None, :]
    return jnp.cos(ang), jnp.sin(ang)


def apply_rope(x, cos, sin):
    half = x.shape[-1] // 2
    xf = x.astype(jnp.float32)
    x1, x2 = xf[..., :half], xf[..., half:]
    return jnp.concatenate([x1 * cos - x2 * sin, x2 * cos + x1 * sin], axis=-1).astype(x.dtype)


def mla_mixer(c_q, c_kv, k_pe, q_norm_g, w_uq, kv_norm_g, w_ukv, out_norm_g, cos, sin):
    B, S, _ = c_q.shape
    q = jnp.einsum('bsr,rf->bsf', rms_norm(c_q, q_norm_g), w_uq).reshape(B, S, MLA_HEADS, MLA_QK)
    q_nope, q_pe = q[..., :MLA_NOPE], q[..., MLA_NOPE:]
    q_pe = apply_rope(q_pe, cos[None, :, None], sin[None, :, None])
    kv = jnp.einsum('bsr,rf->bsf', rms_norm(c_kv, kv_norm_g), w_ukv).reshape(B, S, MLA_HEADS, MLA_NOPE + MLA_V)
    k_nope, v = kv[..., :MLA_NOPE], kv[..., MLA_NOPE:]
    k_pe = apply_rope(k_pe, cos[None], sin[None])
    k = jnp.concatenate([k_nope, jnp.broadcast_to(k_pe[:, :, None, :], (B, S, MLA_HEADS, MLA_ROPE))], axis=-1)
    q = jnp.concatenate([q_nope, q_pe], axis=-1) * (MLA_QK ** -0.5)
    local = jnp.arange(Q_BLOCK)
    outs = []
    for i in range(S // Q_BLOCK):
        end = (i + 1) * Q_BLOCK
        qb = q[:, i * Q_BLOCK:end]
        s = jnp.einsum('bqhd,bkhd->bhqk', qb, k[:, :end], preferred_element_type=jnp.float32)
        mask = (i * Q_BLOCK + local)[:, None] >= jnp.arange(end)[None, :]
        p = jax.nn.softmax(jnp.where(mask, s, -jnp.inf), axis=-1)
        outs.append(jnp.einsum('bhqk,bkhd->bqhd', p.astype(v.dtype), v[:, :end]))
    o = jnp.concatenate(outs, axis=1).reshape(B, S, MLA_WIDTH)
    return rms_norm(o, out_norm_g)


def gla_mixer(q, k, v, gk_lo, g_out, w_gk, b_gk, norm_g):
    B, S, _ = q.shape
    H, DK, DV, C = GLA_HEADS, GLA_DK, GLA_DV, GLA_CHUNK
    N = S // C
    f32 = jnp.float32
    gk = jax.nn.log_sigmoid((jnp.einsum('bsr,rf->bsf', gk_lo, w_gk) + b_gk).astype(f32)) / GLA_GATE_NORM

    def chunks(t, d):
        return t.astype(f32).reshape(B, N, C, H, d).transpose(0, 3, 1, 2, 4)

    qc = chunks(q, DK) * (DK ** -0.5)
    kc, vc, gc = chunks(k, DK), chunks(v, DV), chunks(gk, DK)
    b = jnp.cumsum(gc, axis=3)
    b_last = b[:, :, :, -1:, :]
    q_t = qc * jnp.exp(b)
    k_t = kc * jnp.exp(-b)
    k_end = kc * jnp.exp(b_last - b)
    causal = jnp.tril(jnp.ones((C, C), dtype=bool))
    attn = jnp.where(causal, jnp.einsum('bhnid,bhnjd->bhnij', q_t, k_t), 0.0)
    o_intra = jnp.einsum('bhnij,bhnjv->bhniv', attn, vc)

    def step(state, inp):
        q_n, k_n, v_n, dec_n = inp
        o_n = jnp.einsum('bhcd,bhdv->bhcv', q_n, state)
        state = dec_n[..., None] * state + jnp.einsum('bhcd,bhcv->bhdv', k_n, v_n)
        return state, o_n

    xs = (jnp.moveaxis(q_t, 2, 0), jnp.moveaxis(k_end, 2, 0), jnp.moveaxis(vc, 2, 0),
          jnp.moveaxis(jnp.exp(b_last[:, :, :, 0]), 2, 0))
    _, o_inter = lax.scan(step, jnp.zeros((B, H, DK, DV), f32), xs)
    o = o_intra + jnp.moveaxis(o_inter, 0, 2)
    o = o.transpose(0, 2, 3, 1, 4).reshape(B, S, H, DV)
    o = rms_norm(o, norm_g) * jax.nn.silu(g_out.astype(f32).reshape(B, S, H, DV))
    return o.reshape(B, S, GLA_WIDTH).astype(q.dtype)


def rwkv7_mixer(xc, mu, w0, w2, a0, a2, g2, k_k, k_a, r_k, ln_g, ln_b):
    B, S, _ = xc.shape
    H, N = RWKV_HEADS, RWKV_N
    f32 = jnp.float32
    prev = jnp.pad(xc, ((0, 0), (1, 0), (0, 0)))[:, :-1]
    xm = xc + (prev - xc) * mu
    r, k, v, w_lo, a_lo, g_lo = _split(xm, (RWKV_WIDTH, RWKV_WIDTH, RWKV_WIDTH,
                                            RWKV_DECAY_RANK, RWKV_A_RANK, RWKV_GATE_RANK))
    w = -jax.nn.softplus(-(w0 + jnp.tanh(w_lo) @ w2).astype(f32)) - 0.5
    decay = jnp.exp(-jnp.exp(w))
    a = jax.nn.sigmoid((a0 + a_lo @ a2).astype(f32))
    g = (jax.nn.sigmoid(g_lo) @ g2).astype(f32)

    def heads(t):
        return t.astype(f32).reshape(B, S, H, N)

    kk = heads(k * k_k)
    kk = kk / jnp.maximum(jnp.sqrt(jnp.sum(kk * kk, axis=-1, keepdims=True)), 1e-12)
    k = k.astype(f32) * (1.0 + (a - 1.0) * k_a)
    rh, kh, vh, wh, ah = heads(r), heads(k), heads(v), heads(decay), heads(a)
    a_vec = -kk
    b_vec = kk * ah

    def step(state, inp):
        r_t, w_t, k_t, v_t, a_t, b_t = inp
        sa = jnp.einsum('bhij,bhj->bhi', state, a_t)
        state = (state * w_t[:, :, None, :] + sa[..., None] * b_t[:, :, None, :]
                 + v_t[..., None] * k_t[:, :, None, :])
        return state, jnp.einsum('bhij,bhj->bhi', state, r_t)

    xs = tuple(jnp.moveaxis(t, 1, 0) for t in (rh, wh, kh, vh, a_vec, b_vec))
    _, y = lax.scan(step, jnp.zeros((B, H, N, N), f32), xs)
    y = jnp.moveaxis(y, 0, 1)
    mean = jnp.mean(y, axis=-1, keepdims=True)
    var = jnp.mean(jnp.square(y - mean), axis=-1, keepdims=True)
    y = ((y - mean) * lax.rsqrt(var + RWKV_LN_EPS)).reshape(B, S, RWKV_WIDTH) * ln_g + ln_b
    bonus = jnp.sum(rh * kh * r_k, axis=-1, keepdims=True) * vh
    y = (y + bonus.reshape(B, S, RWKV_WIDTH)) * g
    return y.astype(xc.dtype)


def conv_ffn(h, w_up, conv_w, conv_b, w_down):
    u = jnp.einsum('bsd,df->bsf', h, w_up)
    u = lax.conv_general_dilated(u, conv_w[:, None, :].astype(u.dtype), window_strides=(1,),
                                 padding=[(CONV_WIDTH - 1, 0)],
                                 dimension_numbers=('NWC', 'WIO', 'NWC'),
                                 feature_group_count=u.shape[-1]) + conv_b
    gate, val = u[..., :D_FF], u[..., D_FF:]
    return jnp.einsum('bsf,fd->bsd', jax.nn.silu(gate) * val, w_down)


def setup_inputs(seed: int = 0) -> dict:
    key = jax.random.key(seed)
    ks = iter(jax.random.split(key, 32))
    L = DEPTH

    def nrm(shape, scale):
        return jax.random.normal(next(ks), shape, jnp.float32) * scale

    def gain(shape):
        return 1.0 + 0.02 * jax.random.normal(next(ks), shape, jnp.float32)

    return {
        'x': nrm((BATCH, SEQ, D_MODEL), 1.0),
        'ln1_g': gain((L, D_MODEL)),
        'w_in': nrm((L, D_MODEL, D_IN), D_MODEL ** -0.5),
        'mla_q_norm_g': gain((L, MLA_Q_LORA)),
        'mla_w_uq': nrm((L, MLA_Q_LORA, MLA_HEADS * MLA_QK), MLA_Q_LORA ** -0.5),
        'mla_kv_norm_g': gain((L, MLA_KV_LORA)),
        'mla_w_ukv': nrm((L, MLA_KV_LORA, MLA_HEADS * (MLA_NOPE + MLA_V)), MLA_KV_LORA ** -0.5),
        'mla_out_norm_g': gain((L, MLA_WIDTH)),
        'gla_w_gk': nrm((L, GLA_GATE_RANK, GLA_QK_WIDTH), GLA_GATE_RANK ** -0.5),
        'gla_b_gk': nrm((L, GLA_QK_WIDTH), 0.1),
        'gla_norm_g': gain((L, GLA_DV)),
        'rwkv_mu': jax.random.uniform(next(ks), (L, RWKV_COLS), jnp.float32),
        'rwkv_w0': jax.random.uniform(next(ks), (L, RWKV_WIDTH), jnp.float32, -5.0, 0.0),
        'rwkv_w2': nrm((L, RWKV_DECAY_RANK, RWKV_WIDTH), 0.1 * RWKV_DECAY_RANK ** -0.5),
        'rwkv_a0': nrm((L, RWKV_WIDTH), 0.1),
        'rwkv_a2': nrm((L, RWKV_A_RANK, RWKV_WIDTH), 0.1 * RWKV_A_RANK ** -0.5),
        'rwkv_g2': nrm((L, RWKV_GATE_RANK, RWKV_WIDTH), RWKV_GATE_RANK ** -0.5),
        'rwkv_k_k': 0.85 + nrm((L, RWKV_WIDTH), 0.02),
        'rwkv_k_a': gain((L, RWKV_WIDTH)),
        'rwkv_r_k': nrm((L, RWKV_HEADS, RWKV_N), 0.1),
        'rwkv_ln_g': gain((L, RWKV_WIDTH)),
        'rwkv_ln_b': nrm((L, RWKV_WIDTH), 0.02),
        'w_out': nrm((L, D_MIX, D_MODEL), D_MIX ** -0.5),
        'ln2_g': gain((L, D_MODEL)),
        'ffn_w_up': nrm((L, D_MODEL, 2 * D_FF), D_MODEL ** -0.5),
        'ffn_conv_w': nrm((L, CONV_WIDTH, 2 * D_FF), CONV_WIDTH ** -0.5),
        'ffn_conv_b': nrm((L, 2 * D_FF), 0.02),
        'ffn_w_down': nrm((L, D_FF, D_MODEL), D_FF ** -0.5),
        'final_g': gain((D_MODEL,)),
    }


def reference(x, ln1_g, w_in, mla_q_norm_g, mla_w_uq, mla_kv_norm_g, mla_w_ukv, mla_out_norm_g,
              gla_w_gk, gla_b_gk, gla_norm_g, rwkv_mu, rwkv_w0, rwkv_w2, rwkv_a0, rwkv_a2, rwkv_g2,
              rwkv_k_k, rwkv_k_a, rwkv_r_k, rwkv_ln_g, rwkv_ln_b, w_out, ln2_g, ffn_w_up, ffn_conv_w,
              ffn_conv_b, ffn_w_down, final_g):
    S = x.shape[1]
    cos, sin = rope_tables(S, MLA_ROPE)
    for l in range(DEPTH):
        h = rms_norm(x, ln1_g[l])
        p = jnp.einsum('bsd,dp->bsp', h, w_in[l])
        pa, pb, pc = _split(p, (MLA_COLS, GLA_COLS, RWKV_COLS))
        c_q, c_kv, k_pe = _split(pa, (MLA_Q_LORA, MLA_KV_LORA, MLA_ROPE))
        g_q, g_k, g_v, g_lo, g_out = _split(pb, (GLA_QK_WIDTH, GLA_QK_WIDTH, GLA_WIDTH, GLA_GATE_RANK, GLA_WIDTH))
        y_a = mla_mixer(c_q, c_kv, k_pe, mla_q_norm_g[l], mla_w_uq[l], mla_kv_norm_g[l], mla_w_ukv[l],
                        mla_out_norm_g[l], cos, sin)
        y_b = gla_mixer(g_q, g_k, g_v, g_lo, g_out, gla_w_gk[l], gla_b_gk[l], gla_norm_g[l])
        y_c = rwkv7_mixer(pc, rwkv_mu[l], rwkv_w0[l], rwkv_w2[l], rwkv_a0[l], rwkv_a2[l], rwkv_g2[l],
                          rwkv_k_k[l], rwkv_k_a[l], rwkv_r_k[l], rwkv_ln_g[l], rwkv_ln_b[l])
        y = jnp.concatenate([y_a.astype(x.dtype), y_b.astype(x.dtype), y_c.astype(x.dtype)], axis=-1)
        x = x + jnp.einsum('bsm,md->bsd', y, w_out[l])
        x = x + conv_ffn(rms_norm(x, ln2_g[l]), ffn_w_up[l], ffn_conv_w[l], ffn_conv_b[l], ffn_w_down[l])
    return rms_norm(x, final_g)
```

```python
import contextlib
import numpy as np
import concourse.bass as bass
import concourse.mybir as mybir
from concourse.bass_utils import run_bass_kernel_spmd

F32 = mybir.dt.float32
BF16 = mybir.dt.bfloat16
AF = mybir.ActivationFunctionType
ALU = mybir.AluOpType

D = 1024
DEPTH = 2
MLA_H, NOPE, ROPE, DV = 4, 128, 64, 128
QLORA, KVLORA = 384, 256
GLA_H, GDK, GDV, GRANK = 4, 32, 64, 16
RW_H, RW_N = 4, 64
DFF = 2816
NFT = DFF // 128
EPS = 1e-6
RW_EPS = 64e-5
TB = 512
REPORT_SBUF = False
NO_SELF_WAIT = False


class Buf:
    def __init__(self, ap=None, name=""):
        self.ap = ap
        self.name = name
        self.w = None
        self.r = {}
        self.d = None

    def __getitem__(self, idx):
        return self.ap[idx]


class Eng:
    def __init__(self, k, name, eng, sem):
        self.k, self.name, self.eng, self.sem = k, name, eng, sem
        self.count = 0
        self.known = {}
        self.pending = False

    def wait(self, tok):
        if tok is None:
            return
        sem, val = tok
        if sem is self.sem and (self.name == "pe" or val > self.count or NO_SELF_WAIT):
            return
        if self.known.get(sem, 0) >= val:
            return
        self.eng.wait_ge(sem, val)
        self.known[sem] = val

    def deps(self, reads, writes):
        for b in reads:
            self.wait(b.w)
        for b in writes:
            self.wait(b.w)
            for s, v in list(b.r.items()):
                self.wait((s, v))

    def mark(self, tok, reads, writes):
        s, v = tok
        for b in reads:
            if b.r.get(s, 0) < v:
                b.r[s] = v
        for b in writes:
            b.w = tok
            b.r = {}

    def op(self, fn, reads=(), writes=(), inc=True):
        self.deps(reads, writes)
        inst = fn()
        tok = (self.sem, self.count + 1)
        if inc:
            inst.then_inc(self.sem, 1)
            self.count += 1
            self.pending = False
        else:
            self.pending = True
        self.mark(tok, reads, writes)
        return inst

    def dma(self, out, in_, sb, reads=(), writes=()):
        self.deps(reads, writes)
        if sb.d is None:
            sb.d = {}
        if self.name not in sb.d:
            sb.d[self.name] = self.k.get_dsem(self.name)
            self.k.phase_dma.append((sb, self.name))
        e = sb.d[self.name]
        e[1] += 16
        self.eng.dma_start(out=out, in_=in_).then_inc(e[0], 16)
        self.mark((e[0], e[1]), reads, writes)


class K:
    def __init__(self, nc, es):
        self.nc, self.es = nc, es
        self.nsem = 0
        self.pe = Eng(self, "pe", nc.tensor, self.new_sem("pe"))
        self.act = Eng(self, "act", nc.scalar, self.new_sem("act"))
        self.dve = Eng(self, "dve", nc.vector, self.new_sem("dve"))
        self.pool = Eng(self, "pool", nc.gpsimd, self.new_sem("pool"))
        self.sp = Eng(self, "sp", nc.sync, self.new_sem("sp"))
        self.engs = [self.pe, self.act, self.dve, self.pool, self.sp]
        self.dsem_free = {"sp": [], "pool": []}
        self.phase_dma = []

    def get_dsem(self, q):
        if self.dsem_free[q]:
            return self.dsem_free[q].pop()
        return [self.new_sem("dma" + q), 0]

    @contextlib.contextmanager
    def phase(self):
        with contextlib.ExitStack() as es:
            yield es
            if REPORT_SBUF:
                print("sbuf remaining at phase end:", self.nc.sbuf_bytes_remaining)
            self.barrier()
            for b, q in self.phase_dma:
                self.dsem_free[q].append(b.d.pop(q))
            self.phase_dma = []

    def new_sem(self, name):
        self.nsem += 1
        return self.es.enter_context(self.nc.semaphore(f"{name}_{self.nsem}"))

    def sb(self, es, name, shape, dtype):
        self.nsem += 1
        t = es.enter_context(self.nc.sbuf_tensor(f"{name}_{self.nsem}", list(shape), dtype))
        return Buf(t, name)

    def ps(self, es, name, shape, dtype=F32):
        self.nsem += 1
        t = es.enter_context(self.nc.psum_tensor(f"{name}_{self.nsem}", list(shape), dtype))
        return Buf(t, name)

    def barrier(self):
        toks = []
        for e in self.engs:
            assert not e.pending, e.name
            if e.count:
                toks.append((e.sem, e.count))
        for b, q in self.phase_dma:
            toks.append((b.d[q][0], b.d[q][1]))
        for e in self.engs:
            for t in toks:
                e.wait(t)


def mm(k, ps_buf, ps_ap, pairs, reads):
    n = len(pairs)
    for i, (l, r) in enumerate(pairs):
        k.pe.op(lambda l=l, r=r, i=i: k.nc.tensor.matmul(ps_ap, lhsT=l, rhs=r, start=(i == 0),
                                                         stop=(i == n - 1)),
                reads=reads, writes=[ps_buf], inc=(i == n - 1))


def load_cast(k, wbuf, dst_ap, src_ap, dram_reads=()):
    n = src_ap.shape[-1]
    for c0 in range(0, n, 2048):
        c1 = min(n, c0 + 2048)
        k.pool.dma(dst_ap[:, c0:c1], src_ap[:, c0:c1], wbuf, reads=list(dram_reads), writes=[wbuf])


def load_cast3(k, wbuf, dst3, src2, c0, c1):
    src3 = src2.rearrange("(c p) n -> p c n", p=128)
    n = c1 - c0
    for o in range(0, n, 2048):
        e = min(n, o + 2048)
        k.pool.dma(dst3[:, :, o:e], src3[:, :, c0 + o:c0 + e], wbuf, writes=[wbuf])


class Norm:
    def __init__(self, k, es, name, n, ones_bf, ps=None):
        self.k, self.n, self.ones = k, n, ones_bf
        self.sq = [k.sb(es, f"{name}_sq{i}", [128, n], BF16) for i in range(2)]
        self.ps = k.ps(es, f"{name}_ps", [128, n]) if ps is None else ps
        self.sd = k.sb(es, f"{name}_sd", [128, n], F32)
        self.rstd = self.sd
        self.i = 0

    def stats(self, srcs, src_bufs, dim, eps, ones_ap=None, np_=128):
        k, n = self.k, self.n
        ones_ap = self.ones[0:np_, 0:np_] if ones_ap is None else ones_ap
        nchunk = len(srcs)
        for c, (s, sbuf) in enumerate(zip(srcs, src_bufs)):
            sq = self.sq[self.i % 2]
            self.i += 1
            k.act.op(lambda s=s, sq=sq: k.nc.scalar.activation(out=sq[0:np_, :], in_=s, func=AF.Square),
                     reads=[sbuf], writes=[sq])
            k.pe.op(lambda sq=sq, c=c: k.nc.tensor.matmul(self.ps[0:np_, :], lhsT=ones_ap, rhs=sq[0:np_, :],
                                                          start=(c == 0), stop=(c == nchunk - 1)),
                    reads=[sq, self.ones], writes=[self.ps], inc=True)
        k.act.op(lambda: k.nc.scalar.activation(out=self.sd[0:np_, :], in_=self.ps[0:np_, :], func=AF.Ln,
                                                scale=1.0 / dim, bias=float(eps)),
                 reads=[self.ps], writes=[self.sd])
        k.act.op(lambda: k.nc.scalar.activation(out=self.sd[0:np_, :], in_=self.sd[0:np_, :], func=AF.Exp,
                                                scale=-0.5),
                 reads=[self.sd], writes=[self.sd])
        return self.rstd


VEC_LAYOUT = [
    ("ln1_g", 8), ("ln2_g", 8), ("final_g", 8), ("qn_g", 3), ("kvn_g", 2), ("on_g", 4),
    ("cw0", 44), ("cw1", 44), ("cw2", 44), ("cb", 44),
    ("gla_nb", 1), ("gla_ng", 1),
    ("rw_mu", 8), ("rw_1mmu", 8), ("rw_w0", 2), ("rw_a0", 2), ("rw_kk", 2), ("rw_ka", 2), ("rw_rk", 2),
    ("rw_lng", 2), ("rw_lnb", 2),
]
VOFF = {}
_o = 0
for _n, _c in VEC_LAYOUT:
    VOFF[_n] = (_o, _c)
    _o += _c
NV = _o


def _cols(v, n):
    return np.ascontiguousarray(np.asarray(v, np.float32).reshape(n, 128).T)


def phase_ffn(k, T, l, dr, cst, last):
    nc = k.nc
    nblk = T // TB
    with k.phase() as es:
        vec = k.sb(es, "f_vec", [128, NV], F32)
        k.sp.dma(vec[:, :], dr["vecs"][l], vec, writes=[vec])
        vnext = k.sb(es, "f_vnext", [128, 8], F32)
        if not last:
            o1 = VOFF["ln1_g"][0]
            k.sp.dma(vnext[:, :], dr["vecs"][l + 1][:, o1:o1 + 8], vnext, writes=[vnext])
        ones = k.sb(es, "f_ones", [128, 128], BF16)
        load_cast(k, ones, ones[:, :], cst["ones"])
        wup = k.sb(es, "f_wup", [128, 8, 2 * DFF], BF16)
        load_cast3(k, wup, wup[:, :, :], dr["ffn_w_up"][l], 0, 2 * DFF)
        wdn = k.sb(es, "f_wdn", [128, NFT, D], BF16)
        load_cast3(k, wdn, wdn[:, :, :], dr["ffn_w_down"][l], 0, D)
        xbs = [k.sb(es, f"f_xb{i}", [128, 8, TB], F32) for i in range(2)]
        hT = k.sb(es, "f_hT", [128, 8, TB], BF16)
        gT = [k.sb(es, f"f_gT{j}", [128, TB], BF16) for j in range(NFT)]
        halo = k.sb(es, "f_halo", [128, 2 * NFT, 2], F32)
        k.pool.op(lambda: nc.gpsimd.memset(halo[:, :, :], 0.0), writes=[halo])
        ug = [k.sb(es, f"f_ug{i}", [128, TB + 2], F32) for i in range(2)]
        ugh = [Buf(ug[i].ap[:, 0:2], f"ugh{i}") for i in range(2)]
        ugm = [Buf(ug[i].ap[:, 2:TB + 2], f"ugm{i}") for i in range(2)]
        a1 = [k.sb(es, f"f_a1{i}", [128, TB], F32) for i in range(2)]
        psu = [k.ps(es, f"f_psu{i}", [128, TB]) for i in range(4)]
        psd = [k.ps(es, f"f_psd{i}", [128, TB]) for i in range(2)]
        nrm = Norm(k, es, "f_n", TB, ones)
        o_g = VOFF["ln2_g"][0]
        o_w0, o_w1, o_w2, o_cb = (VOFF[n][0] for n in ("cw0", "cw1", "cw2", "cb"))
        o_fg = VOFF["final_g"][0]
        xT = dr["xT"]
        xTb = dr["xT_buf"]
        it = 0
        def norm_in(xb):
            rstd = nrm.stats([xb[:, c, :] for c in range(8)], [xb] * 8, D, EPS)
            for c in range(8):
                k.dve.op(lambda c=c, xb=xb: nc.vector.scalar_tensor_tensor(
                    out=hT[:, c, :], in0=xb[:, c, :], scalar=vec[:, o_g + c:o_g + c + 1], in1=rstd[:, :],
                    op0=ALU.mult, op1=ALU.mult), reads=[xb, vec, rstd], writes=[hT])

        k.sp.dma(xbs[0][:, :, :], xT[:, :, 0:TB], xbs[0], reads=[xTb], writes=[xbs[0]])
        norm_in(xbs[0])
        for b in range(nblk):
            t0 = b * TB
            xb = xbs[b % 2]
            if b + 1 < nblk:
                xn = xbs[(b + 1) % 2]
                k.sp.dma(xn[:, :, :], xT[:, :, t0 + TB:t0 + 2 * TB], xn, reads=[xTb], writes=[xn])
            for j in range(NFT):
                res = []
                for half in range(2):
                    jt = j + half * NFT
                    u = half
                    pu = psu[it % 4]
                    it += 1
                    col0 = half * DFF + j * 128
                    mm(k, pu, pu[:, :],
                       [(wup[:, c, col0:col0 + 128], hT[:, c, :]) for c in range(8)], [wup, hT])
                    k.pool.op(lambda u=u, jt=jt: nc.gpsimd.tensor_copy(out=ug[u][:, 0:2], in_=halo[:, jt, :]),
                              reads=[halo], writes=[ugh[u]])
                    k.act.op(lambda u=u, pu=pu: nc.scalar.activation(out=ug[u][:, 2:TB + 2], in_=pu[:, :],
                                                                     func=AF.Copy), reads=[pu], writes=[ugm[u]])
                    k.act.op(lambda u=u, jt=jt, pu=pu: nc.scalar.activation(
                        out=a1[u][:, :], in_=pu[:, :], func=AF.Identity,
                        scale=vec[:, o_w2 + jt:o_w2 + jt + 1], bias=vec[:, o_cb + jt:o_cb + jt + 1]),
                        reads=[pu, vec], writes=[a1[u]])
                    k.pool.op(lambda u=u, jt=jt: nc.gpsimd.tensor_copy(out=halo[:, jt, :], in_=ug[u][:, TB:TB + 2]),
                              reads=[ugm[u]], writes=[halo])
                    k.dve.op(lambda u=u, jt=jt: nc.vector.scalar_tensor_tensor(
                        out=a1[u][:, :], in0=ug[u][:, 1:TB + 1], scalar=vec[:, o_w1 + jt:o_w1 + jt + 1],
                        in1=a1[u][:, :], op0=ALU.mult, op1=ALU.add), reads=[ugm[u], ugh[u], vec, a1[u]], writes=[a1[u]])
                    k.dve.op(lambda u=u, jt=jt: nc.vector.scalar_tensor_tensor(
                        out=a1[u][:, :], in0=ug[u][:, 0:TB], scalar=vec[:, o_w0 + jt:o_w0 + jt + 1],
                        in1=a1[u][:, :], op0=ALU.mult, op1=ALU.add), reads=[ugm[u], ugh[u], vec, a1[u]], writes=[a1[u]])
                    res.append(a1[u])
                s = res[0]
                k.act.op(lambda s=s: nc.scalar.activation(out=s[:, :], in_=s[:, :], func=AF.Silu),
                         reads=[s], writes=[s])
                k.pool.op(lambda s=s, v=res[1], j=j: nc.gpsimd.tensor_tensor(
                    out=gT[j][:, :], in0=s[:, :], in1=v[:, :], op=ALU.mult), reads=[s, res[1]], writes=[gT[j]])
            if b + 1 < nblk:
                norm_in(xbs[(b + 1) % 2])
            for m in range(8):
                pd = psd[m % 2]
                mm(k, pd, pd[:, :], [(wdn[:, j, m * 128:(m + 1) * 128], gT[j][:, :]) for j in range(NFT)],
                   [wdn] + gT)
                k.dve.op(lambda m=m, pd=pd, xb=xb: nc.vector.tensor_tensor(out=xb[:, m, :], in0=pd[:, :],
                                                                           in1=xb[:, m, :], op=ALU.add),
                         reads=[pd, xb], writes=[xb])
            if not last:
                k.sp.dma(xT[:, :, t0:t0 + TB], xb[:, :, :], xb, reads=[xb], writes=[xTb])
                rstd = nrm.stats([xb[:, c, :] for c in range(8)], [xb] * 8, D, EPS)
                for c in range(8):
                    hn = gT[NFT - 8 + c]
                    k.dve.op(lambda c=c, hn=hn, xb=xb: nc.vector.scalar_tensor_tensor(
                        out=hn[:, :], in0=xb[:, c, :], scalar=vnext[:, c:c + 1], in1=rstd[:, :],
                        op0=ALU.mult, op1=ALU.mult), reads=[xb, vnext, rstd], writes=[hn])
                    k.sp.dma(dr["hT"][:, c, t0:t0 + TB], hn[:, :], hn, reads=[hn], writes=[dr["hT_buf"]])
            else:
                rstd = nrm.stats([xb[:, c, :] for c in range(8)], [xb] * 8, D, EPS)
                for c in range(8):
                    k.dve.op(lambda c=c, xb=xb: nc.vector.scalar_tensor_tensor(
                        out=xb[:, c, :], in0=xb[:, c, :], scalar=vec[:, o_fg + c:o_fg + c + 1], in1=rstd[:, :],
                        op0=ALU.mult, op1=ALU.mult), reads=[xb, vec, rstd], writes=[xb])
                k.sp.dma(dr["outT"][:, :, t0:t0 + TB], xb[:, :, :], xb, reads=[xb], writes=[dr["outT_buf"]])


MISC_LAYOUT = [("ones", 128), ("ones2", 128), ("ident", 128), ("cmask", 512), ("mask4", 512), ("bd", 256),
               ("hm", 4), ("smask", 128), ("imaskT", 128)]
MOFF = {}
_o = 0
for _n, _c in MISC_LAYOUT:
    MOFF[_n] = (_o, _o + _c)
    _o += _c
NMISC = _o


def make_misc():
    m = np.zeros((128, NMISC), np.float32)
    p = np.arange(128)[:, None]
    f = lambda n: np.arange(n)[None, :]
    m[:, slice(*MOFF["ones"])] = 1.0
    m[:, slice(*MOFF["ones2"])] = (p // 64 == f(128) // 64)
    m[:, slice(*MOFF["ident"])] = (p == f(128))
    m[:, slice(*MOFF["cmask"])] = (f(512) % 128 != 0)
    m[:, slice(*MOFF["mask4"])] = (p <= f(512) % 128)
    m[:, slice(*MOFF["bd"])] = (p // 32 == f(256) // 64)
    m[:, slice(*MOFF["hm"])] = (p // 32 == f(4))
    m[:, slice(*MOFF["smask"])] = (p < f(128))
    m[:, slice(*MOFF["imaskT"])] = (p > f(128))
    return m


def load_misc(k, es, name, cst, names, dtype):
    out = {}
    for n in names:
        a, b = MOFF[n]
        t = k.sb(es, f"{name}_{n}", [128, b - a], dtype)
        if dtype == BF16:
            load_cast(k, t, t[:, :], cst["misc"][:, a:b])
        else:
            k.sp.dma(t[:, :], cst["misc"][:, a:b], t, writes=[t])
        out[n] = t
    return out


def phase_gla(k, T, l, dr, cst):
    nc = k.nc
    C = 128
    NCH = TB // C
    w_in = dr["w_in"]
    G0 = 704
    with k.phase() as es:
        vec = k.sb(es, "g_vec", [128, NV], F32)
        k.sp.dma(vec[:, :], dr["vecs"][l], vec, writes=[vec])
        cb = load_misc(k, es, "g", cst, ["ones2", "ident", "mask4", "bd"], BF16)
        cf = load_misc(k, es, "gf", cst, ["cmask", "hm"], F32)
        ones2, ident, mask4, bd, cmask, hm = cb["ones2"], cb["ident"], cb["mask4"], cb["bd"], cf["cmask"], cf["hm"]
        wG = k.sb(es, "g_wG", [128, 8, 784], BF16)
        load_cast3(k, wG, wG[:, :, 0:256], w_in[l], G0, G0 + 256)
        load_cast3(k, wG, wG[:, :, 256:512], w_in[l], G0 + 528, G0 + 784)
        load_cast3(k, wG, wG[:, :, 512:768], w_in[l], G0 + 256, G0 + 512)
        load_cast3(k, wG, wG[:, :, 768:784], w_in[l], G0 + 512, G0 + 528)
        wgk = k.sb(es, "g_wgk", [16, 128], BF16)
        load_cast(k, wgk, wgk[:, :], dr["gla_w_gk"][l])
        nb = k.sb(es, "g_nb", [128, 1], F32)
        o_nb, o_ng = VOFF["gla_nb"][0], VOFF["gla_ng"][0]
        k.dve.op(lambda: nc.vector.tensor_scalar(out=nb[:, :], in0=vec[:, o_nb:o_nb + 1], scalar1=-1.0, scalar2=None,
                                                 op0=ALU.mult), reads=[vec], writes=[nb])
        hT = [k.sb(es, f"g_hT{i}", [128, 8, TB], BF16) for i in range(2)]
        q32 = k.sb(es, "g_q32", [128, TB], F32)
        k32 = k.sb(es, "g_k32", [128, TB], F32)
        glo = k.sb(es, "g_glo", [16, TB], BF16)
        Lt = k.sb(es, "g_L", [128, TB], F32)
        Bc = k.sb(es, "g_Bc", [128, TB], F32)
        eb = k.sb(es, "g_eb", [128, TB], F32)
        enb = k.sb(es, "g_enb", [128, TB], F32)
        kef = k.sb(es, "g_kef", [128, TB], F32)
        nbl = k.sb(es, "g_nbl", [128, NCH], F32)
        dec = k.sb(es, "g_dec", [128, NCH], F32)
        qth = [k.sb(es, f"g_qth{i}", [128, TB], BF16) for i in range(4)]
        kt = k.sb(es, "g_kt", [128, TB], BF16)
        kend = k.sb(es, "g_kend", [128, TB], BF16)
        kendT = k.sb(es, "g_kendT", [128, NCH, 128], BF16)
        vT = k.sb(es, "g_vT", [128, NCH, 256], BF16)
        sgo = [k.sb(es, f"g_sgo{i}", [128, TB], F32) for i in range(2)]
        attnT = k.sb(es, "g_attnT", [128, 4 * C], BF16)
        S4 = k.sb(es, "g_S4", [128, 256], F32)
        Sb = k.sb(es, "g_Sb", [128, 256], BF16)
        kvm = k.sb(es, "g_kvm", [128, 256], F32)
        oacc = [k.sb(es, f"g_oacc{i}", [128, TB], F32) for i in range(2)]
        yo = [k.sb(es, f"g_yo{i}", [128, TB], BF16) for i in range(2)]
        tmp = k.sb(es, "g_tmp", [128, TB], F32)
        k.pool.op(lambda: nc.gpsimd.memset(S4[:, :], 0.0), writes=[S4])
        k.pool.op(lambda: nc.gpsimd.memset(Sb[:, :], 0.0), writes=[Sb])
        nrm = Norm(k, es, "g_n", TB, ones2)
        ps_p = [k.ps(es, f"g_psp{i}", [128, TB]) for i in range(2)]
        ps_at = k.ps(es, "g_psat", [128, 4 * C])
        ps_o = [k.ps(es, f"g_pso{i}", [128, C]) for i in range(2)]
        ps_kv = k.ps(es, "g_pskv", [128, 256])
        ps_tr = k.ps(es, "g_pstr", [128, 128])
        ip = 0

        def proj(pairs, reads, m=128, n=TB):
            nonlocal ip
            p = ps_p[ip % 2]
            ip += 1
            mm(k, p, p[0:m, 0:n], pairs, reads)
            return p

        qs = float(GDK ** -0.5)
        for b in range(T // TB):
            t0 = b * TB
            h = hT[b % 2]
            k.sp.dma(h[:, :, :], dr["hT"][:, :, t0:t0 + TB], h, reads=[dr["hT_buf"]], writes=[h])
            p = proj([(wG[:, c, 0:128], h[:, c, :]) for c in range(8)], [wG, h])
            k.act.op(lambda p=p: nc.scalar.activation(out=q32[:, :], in_=p[:, :], func=AF.Copy), reads=[p], writes=[q32])
            p = proj([(wG[:, c, 128:256], h[:, c, :]) for c in range(8)], [wG, h])
            k.act.op(lambda p=p: nc.scalar.activation(out=k32[:, :], in_=p[:, :], func=AF.Copy), reads=[p], writes=[k32])
            p = proj([(wG[:, c, 768:784], h[:, c, :]) for c in range(8)], [wG, h], m=16)
            k.act.op(lambda p=p: nc.scalar.activation(out=glo[:, :], in_=p[0:16, :], func=AF.Copy), reads=[p], writes=[glo])
            for i in range(2):
                p = proj([(wG[:, c, 256 + i * 128:384 + i * 128], h[:, c, :]) for c in range(8)], [wG, h])
                k.act.op(lambda p=p, i=i: nc.scalar.activation(out=sgo[i][:, :], in_=p[:, :], func=AF.Silu),
                         reads=[p], writes=[sgo[i]])
            for c in range(NCH):
                p = proj([(h[:, c8, c * C:(c + 1) * C], wG[:, c8, 512:768]) for c8 in range(8)], [wG, h], n=256)
                k.act.op(lambda p=p, c=c: nc.scalar.activation(out=vT[:, c, :], in_=p[:, 0:256], func=AF.Copy),
                         reads=[p], writes=[vT])
            p = proj([(wgk[:, :], glo[:, :])], [wgk, glo])
            k.act.op(lambda p=p: nc.scalar.activation(out=Lt[:, :], in_=p[:, :], func=AF.Exp, scale=-1.0, bias=nb[:, 0:1]),
                     reads=[p, nb], writes=[Lt])
            k.act.op(lambda: nc.scalar.activation(out=Lt[:, :], in_=Lt[:, :], func=AF.Ln, bias=1.0), reads=[Lt], writes=[Lt])
            k.dve.op(lambda: nc.vector.tensor_tensor_scan(out=Bc[:, :], data0=cmask[:, :], data1=Lt[:, :], initial=0.0,
                                                          op0=ALU.mult, op1=ALU.add), reads=[cmask, Lt], writes=[Bc])
            k.act.op(lambda: nc.scalar.activation(out=eb[:, :], in_=Bc[:, :], func=AF.Exp, scale=-1.0 / 16), reads=[Bc], writes=[eb])
            k.act.op(lambda: nc.scalar.activation(out=enb[:, :], in_=Bc[:, :], func=AF.Exp, scale=1.0 / 16), reads=[Bc], writes=[enb])
            k.dve.op(lambda: nc.vector.tensor_scalar(out=nbl[:, :], in0=Bc[:, C - 1::C], scalar1=-1.0 / 16, scalar2=None,
                                                     op0=ALU.mult), reads=[Bc], writes=[nbl])
            k.act.op(lambda: nc.scalar.activation(out=dec[:, :], in_=nbl[:, :], func=AF.Exp), reads=[nbl], writes=[dec])
            for c in range(NCH):
                k.act.op(lambda c=c: nc.scalar.activation(out=kef[:, c * C:(c + 1) * C], in_=Bc[:, c * C:(c + 1) * C],
                                                          func=AF.Exp, scale=1.0 / 16, bias=nbl[:, c:c + 1]),
                         reads=[Bc, nbl], writes=[kef])
            k.dve.op(lambda: nc.vector.tensor_tensor(out=tmp[:, :], in0=q32[:, :], in1=eb[:, :], op=ALU.mult),
                     reads=[q32, eb], writes=[tmp])
            for hh in range(4):
                k.dve.op(lambda hh=hh: nc.vector.tensor_scalar(out=qth[hh][:, :], in0=tmp[:, :], scalar1=hm[:, hh:hh + 1],
                                                               scalar2=qs, op0=ALU.mult, op1=ALU.mult),
                         reads=[tmp, hm], writes=[qth[hh]])
            k.dve.op(lambda: nc.vector.tensor_tensor(out=kt[:, :], in0=k32[:, :], in1=enb[:, :], op=ALU.mult),
                     reads=[k32, enb], writes=[kt])
            k.dve.op(lambda: nc.vector.tensor_tensor(out=kend[:, :], in0=k32[:, :], in1=kef[:, :], op=ALU.mult),
                     reads=[k32, kef], writes=[kend])
            for c in range(NCH):
                cs = slice(c * C, (c + 1) * C)
                mm(k, ps_tr, ps_tr[:, :], [(kend[:, cs], ident[:, :])], [kend, ident])
                k.act.op(lambda c=c: nc.scalar.activation(out=kendT[:, c, :], in_=ps_tr[:, :], func=AF.Copy),
                         reads=[ps_tr], writes=[kendT])
            for c in range(NCH):
                cs = slice(c * C, (c + 1) * C)
                for hh in range(4):
                    k.pe.op(lambda hh=hh, cs=cs: nc.tensor.matmul(ps_at[:, hh * C:(hh + 1) * C], lhsT=kt[:, cs],
                                                                  rhs=qth[hh][:, cs], start=True, stop=True),
                            reads=[kt, qth[hh]], writes=[ps_at], inc=(hh == 3))
                k.dve.op(lambda: nc.vector.tensor_tensor(out=attnT[:, :], in0=ps_at[:, :], in1=mask4[:, :], op=ALU.mult),
                         reads=[ps_at, mask4], writes=[attnT])
                for pr in range(2):
                    po = ps_o[pr]
                    for hl in range(2):
                        hh = pr * 2 + hl
                        osl = po[hl * 64:(hl + 1) * 64, :]
                        k.pe.op(lambda hh=hh, osl=osl, c=c: nc.tensor.matmul(
                            osl, lhsT=vT[:, c, hh * 64:(hh + 1) * 64], rhs=attnT[:, hh * C:(hh + 1) * C],
                            start=True, stop=False), reads=[vT, attnT], writes=[po], inc=False)
                        k.pe.op(lambda hh=hh, osl=osl, cs=cs: nc.tensor.matmul(
                            osl, lhsT=Sb[:, hh * 64:(hh + 1) * 64], rhs=qth[hh][:, cs], start=False, stop=True),
                            reads=[Sb, qth[hh]], writes=[po], inc=True)
                    k.act.op(lambda pr=pr, po=po, cs=cs: nc.scalar.activation(out=oacc[pr][:, cs], in_=po[:, :],
                                                                              func=AF.Copy), reads=[po], writes=[oacc[pr]])
                mm(k, ps_kv, ps_kv[:, :], [(kendT[:, c, :], vT[:, c, :])], [kendT, vT])
                k.dve.op(lambda: nc.vector.tensor_tensor(out=kvm[:, :], in0=ps_kv[:, :], in1=bd[:, :], op=ALU.mult),
                         reads=[ps_kv, bd], writes=[kvm])
                k.dve.op(lambda c=c: nc.vector.scalar_tensor_tensor(out=S4[:, :], in0=S4[:, :], scalar=dec[:, c:c + 1],
                                                                    in1=kvm[:, :], op0=ALU.mult, op1=ALU.add),
                         reads=[S4, dec, kvm], writes=[S4])
                k.act.op(lambda: nc.scalar.activation(out=Sb[:, :], in_=S4[:, :], func=AF.Copy), reads=[S4], writes=[Sb])
            for pr in range(2):
                rstd = nrm.stats([oacc[pr][:, :]], [oacc[pr]], GDV, EPS)
                k.dve.op(lambda pr=pr: nc.vector.tensor_tensor(out=tmp[:, :], in0=oacc[pr][:, :], in1=rstd[:, :],
                                                               op=ALU.mult), reads=[oacc[pr], rstd], writes=[tmp])
                k.dve.op(lambda pr=pr: nc.vector.scalar_tensor_tensor(
                    out=yo[pr][:, :], in0=tmp[:, :], scalar=vec[:, o_ng:o_ng + 1], in1=sgo[pr][:, :],
                    op0=ALU.mult, op1=ALU.mult), reads=[tmp, vec, sgo[pr]], writes=[yo[pr]])
                k.sp.dma(dr["yT"][:, 4 + pr, t0:t0 + TB], yo[pr][:, :], yo[pr], reads=[yo[pr]], writes=[dr["yT_buf"]])


def TT(e, out, a, b, op, R, W):
    e.op(lambda: e.eng.tensor_tensor(out=out, in0=a, in1=b, op=op), reads=R, writes=W)


def TS(e, out, a, s1, s2, op0, op1, R, W):
    e.op(lambda: e.eng.tensor_scalar(out=out, in0=a, scalar1=s1, scalar2=s2, op0=op0, op1=op1), reads=R, writes=W)


def STT(k, out, a, sc, b, op0, op1, R, W):
    k.dve.op(lambda: k.nc.vector.scalar_tensor_tensor(out=out, in0=a, scalar=sc, in1=b, op0=op0, op1=op1),
             reads=R, writes=W)


def ACTF(k, out, in_, func, R, W, scale=1.0, bias=0.0):
    k.act.op(lambda: k.nc.scalar.activation(out=out, in_=in_, func=func, scale=scale, bias=bias), reads=R, writes=W)


def MM(k, out, lhsT, rhs, R, W, start=True, stop=True, inc=True):
    k.pe.op(lambda: k.nc.tensor.matmul(out, lhsT=lhsT, rhs=rhs, start=start, stop=stop), reads=R, writes=W, inc=inc)


class _Stop(Exception):
    pass


STOP = [None]


def chk(n):
    if STOP[0] == n:
        raise _Stop()


def phase_rwkv(k, T, l, dr, cst):
    try:
        _phase_rwkv(k, T, l, dr, cst)
    except _Stop:
        pass


NS = 3


def _phase_rwkv(k, T, l, dr, cst):
    nc = k.nc
    C = 128
    NCH = TB // C
    R0 = 704 + 784
    DEC = float(np.exp(-0.5))
    with k.phase() as es:
        vec = k.sb(es, "r_vec", [128, NV], F32)
        k.sp.dma(vec[:, :], dr["vecs"][l], vec, writes=[vec])
        cb = load_misc(k, es, "r", cst, ["ones2", "ident"], BF16)
        cf = load_misc(k, es, "rf", cst, ["cmask", "smask", "mask4", "imaskT", "ident"], F32)
        ones2, ident, cmask = cb["ones2"], cb["ident"], cf["cmask"]
        m_si = k.sb(es, "r_msi", [128, 2, 256], BF16)
        m_tt = k.sb(es, "r_mtt", [128, 2, 128], BF16)
        for hl in range(2):
            k.pool.op(lambda hl=hl: nc.gpsimd.tensor_copy(out=m_si[:, hl, 0:128], in_=cf["smask"][:, :]),
                      reads=[cf["smask"]], writes=[m_si])
            k.pool.op(lambda hl=hl: nc.gpsimd.tensor_copy(out=m_si[:, hl, 128:256], in_=cf["mask4"][:, 0:128]),
                      reads=[cf["mask4"]], writes=[m_si])
            k.pool.op(lambda hl=hl: nc.gpsimd.tensor_copy(out=m_tt[:, hl, :], in_=cf["imaskT"][:, :]),
                      reads=[cf["imaskT"]], writes=[m_tt])
        I2 = k.sb(es, "r_I2", [128, 64], F32)
        k.pool.op(lambda: nc.gpsimd.tensor_copy(out=I2[0:64, :], in_=cf["ident"][0:64, 0:64]), reads=[cf["ident"]], writes=[I2])
        k.pool.op(lambda: nc.gpsimd.tensor_copy(out=I2[64:128, :], in_=cf["ident"][64:128, 64:128]), reads=[cf["ident"]], writes=[I2])
        wR = k.sb(es, "r_wR", [128, 8, 1024], BF16)
        load_cast3(k, wR, wR[:, :, :], dr["w_in"][l], R0, R0 + 1024)
        wl_w = k.sb(es, "r_wlw", [128, 256], BF16)
        wl_a = k.sb(es, "r_wla", [128, 256], BF16)
        k.pool.op(lambda: nc.gpsimd.memset(wl_w[:, :], 0.0), writes=[wl_w])
        k.pool.op(lambda: nc.gpsimd.memset(wl_a[:, :], 0.0), writes=[wl_a])
        load_cast(k, wl_w, wl_w[0:64, :], dr["rwkv_w2"][l])
        load_cast(k, wl_a, wl_a[64:128, :], dr["rwkv_a2"][l])
        hm2 = k.sb(es, "r_hm2", [128, 2], F32)
        nhm2 = k.sb(es, "r_nhm2", [128, 2], F32)
        I2h = [k.sb(es, f"r_I2h{i}", [128, 64], F32) for i in range(2)]
        for i in range(2):
            k.pool.op(lambda i=i: nc.gpsimd.memset(I2h[i][:, :], 0.0), writes=[I2h[i]])
            k.pool.op(lambda i=i: nc.gpsimd.tensor_copy(out=I2h[i][i * 64:(i + 1) * 64, :],
                                                        in_=cf["ident"][i * 64:(i + 1) * 64, i * 64:(i + 1) * 64]),
                      reads=[cf["ident"]], writes=[I2h[i]])
        k.pool.op(lambda: nc.gpsimd.memset(hm2[:, :], 0.0), writes=[hm2])
        k.pool.op(lambda: nc.gpsimd.memset(hm2[0:64, 0:1], 1.0), writes=[hm2])
        k.pool.op(lambda: nc.gpsimd.memset(hm2[64:128, 1:2], 1.0), writes=[hm2])
        TS(k.dve, nhm2[:, :], hm2[:, :], -1.0, None, ALU.mult, ALU.bypass, [hm2], [nhm2])
        wg2 = k.sb(es, "r_wg2", [128, 256], BF16)
        load_cast(k, wg2, wg2[:, :], dr["rwkv_g2"][l])
        o_mu, o_w0, o_a0, o_kk, o_ka, o_rk, o_lg, o_lb = (VOFF[n][0] for n in (
            "rw_mu", "rw_w0", "rw_a0", "rw_kk", "rw_ka", "rw_rk", "rw_lng", "rw_lnb"))
        omm = k.sb(es, "r_omm", [128, 8], F32)
        omka = k.sb(es, "r_omka", [128, 2], F32)
        TS(k.dve, omm[:, :], vec[:, o_mu:o_mu + 8], -1.0, 1.0, ALU.mult, ALU.add, [vec], [omm])
        TS(k.dve, omka[:, :], vec[:, o_ka:o_ka + 2], -1.0, 1.0, ALU.mult, ALU.add, [vec], [omka])

        hTs = [k.sb(es, f"r_hT{i}", [128, 8, TB + 1], BF16) for i in range(2)]
        def f32t(n): return k.sb(es, "r_" + n, [128, TB], F32)
        def bft(n): return k.sb(es, "r_" + n, [128, TB], BF16)
        tprev = f32t("tprev")
        xr, xk, xv = [f32t(f"xr{i}") for i in range(2)], [f32t(f"xk{i}") for i in range(2)], [f32t(f"xv{i}") for i in range(2)]
        xlo = bft("xlo")
        sgl = bft("sgl")
        gate = [f32t(f"gate{i}") for i in range(2)]
        bon = [f32t(f"bon{i}") for i in range(2)]
        aa, kkt, kmod, cc, t1, t2, t3 = f32t("aa"), f32t("kk"), f32t("kmod"), f32t("cc"), f32t("t1"), f32t("t2"), f32t("t3")
        tb1 = bft("tb1")
        ar = [[k.sb(es, f"r_ar{i}{j}", [128, NCH, 256], BF16) for j in range(2)] for i in range(2)]
        Xw_ = [[k.sb(es, f"r_Xw{p}{i}", [128, 128], BF16) for i in range(2)] for p in range(NS)]
        for p_ in range(NS):
            for i in range(2):
                k.pool.op(lambda i=i, p_=p_: nc.gpsimd.memset(Xw_[p_][i][:, :], 0.0), writes=[Xw_[p_][i]])
        bt = [bft(f"bt{i}") for i in range(2)]
        ktl = [bft(f"ktl{i}") for i in range(2)]
        Bp = [bft(f"Bp{i}") for i in range(2)]
        Kp = [bft(f"Kp{i}") for i in range(2)]
        vb = [bft(f"vb{i}") for i in range(2)]
        nbl = k.sb(es, "r_nbl", [128, NCH], F32)
        pC = [k.sb(es, f"r_pC{i}", [128, NCH], F32) for i in range(2)]
        TM_ = [k.sb(es, f"r_TM{p}", [128, 4, 128], BF16) for p in range(NS)]
        AbRb_ = [k.sb(es, f"r_AbRb{p}", [128, 2, 256], BF16) for p in range(NS)]
        AkRk_ = [k.sb(es, f"r_AkRk{p}", [128, 2, 256], BF16) for p in range(NS)]
        PP_ = [[k.sb(es, f"r_PP{p}{i}", [128, 2, 256], BF16) for i in range(2)] for p in range(NS)]
        X_ = [k.sb(es, f"r_X{p}", [128, 2, 128], BF16) for p in range(NS)]
        QpT = [[bft(f"QpT{i}{j}") for j in range(2)] for i in range(2)]
        Y0T = [f32t(f"Y0T{i}") for i in range(2)]
        MT = [[k.sb(es, f"r_MT{i}{j}", [128, NCH, 64], BF16) for j in range(2)] for i in range(2)]
        Gm = [k.sb(es, f"r_G{i}", [128, NCH, 64], F32) for i in range(2)]
        Hb = [k.sb(es, f"r_H{i}", [128, 64], BF16) for i in range(2)]
        yacc = [f32t(f"yacc{i}") for i in range(2)]
        yo = [bft(f"yo{i}") for i in range(2)]
        o1, ob1 = f32t("o1"), bft("ob1")
        sets = [(gate, bon, vb, ar, bt, ktl, pC, Bp, Kp)]
        sets.append(([f32t(f"gateB{i}") for i in range(2)], [f32t(f"bonB{i}") for i in range(2)],
                     [bft(f"vbB{i}") for i in range(2)],
                     [[k.sb(es, f"r_arB{i}{j}", [128, NCH, 256], BF16) for j in range(2)] for i in range(2)],
                     [bft(f"btB{i}") for i in range(2)], [bft(f"ktlB{i}") for i in range(2)],
                     [k.sb(es, f"r_pCB{i}", [128, NCH], F32) for i in range(2)],
                     [bft(f"BpB{i}") for i in range(2)], [bft(f"KpB{i}") for i in range(2)]))
        for i in range(2):
            k.pool.op(lambda i=i: nc.gpsimd.memset(Hb[i][:, :], 0.0), writes=[Hb[i]])
        ps_p = [k.ps(es, f"r_psp{i}", [128, TB]) for i in range(2)]
        nrm = Norm(k, es, "r_n", TB, ones2, ps=ps_p[0])
        ps_x_ = [k.ps(es, f"r_bx{p}", [128, TB]) for p in range(NS)]
        ps_pp_ = [k.ps(es, f"r_pspp{p}", [128, 2, 256]) for p in range(NS)]
        banks = [ps_p[0], ps_p[1]]
        ip = 0
        isx = 0

        def bank():
            nonlocal isx
            isx += 1
            return banks[isx % 2]

        def proj(pairs, reads, n=TB):
            nonlocal ip
            p = ps_p[ip % 2]
            ip += 1
            mm(k, p, p[:, 0:n], pairs, reads)
            return p

        def prepR(b):
            t0 = b * TB
            h = hTs[b % 2]
            gate, bon, vb, ar, bt, ktl, pC, Bp, Kp = sets[b % 2]
            if b == 0:
                k.pool.op(lambda h=h: nc.gpsimd.memset(h[:, :, 0:1], 0.0), writes=[h])
                k.sp.dma(h[:, :, 1:TB + 1], dr["hT"][:, :, 0:TB], h, reads=[dr["hT_buf"]], writes=[h])
            else:
                k.sp.dma(h[:, :, :], dr["hT"][:, :, t0 - 1:t0 + TB], h, reads=[dr["hT_buf"]], writes=[h])

            def mixed(j, out, func=AF.Identity, out_bufs=None):
                cols = slice(j * 128, (j + 1) * 128)
                pp = proj([(wR[:, c, cols], h[:, c, 0:TB]) for c in range(8)], [wR, h])
                ACTF(k, tprev[:, :], pp[:, :], AF.Copy if False else AF.Identity, [pp, vec], [tprev],
                     scale=vec[:, o_mu + j:o_mu + j + 1])
                pc_ = proj([(wR[:, c, cols], h[:, c, 1:TB + 1]) for c in range(8)], [wR, h])
                STT(k, t1[:, :], pc_[:, :], omm[:, j:j + 1], tprev[:, :], ALU.mult, ALU.add, [pc_, omm, tprev], [t1])
                return t1

            yield
            mixed(6, None)
            ACTF(k, xlo[0:64, :], t1[0:64, :], AF.Tanh, [t1], [xlo])
            ACTF(k, xlo[64:128, :], t1[64:128, :], AF.Copy, [t1], [xlo])
            yield
            mixed(7, None)
            ACTF(k, sgl[:, :], t1[:, :], AF.Sigmoid, [t1], [sgl])
            yield
            for j in range(2):
                mixed(j, None)
                ACTF(k, xr[j][:, :], t1[:, :], AF.Copy, [t1], [xr[j]])
                yield
                mixed(2 + j, None)
                ACTF(k, xk[j][:, :], t1[:, :], AF.Copy, [t1], [xk[j]])
                yield
                mixed(4 + j, None)
                ACTF(k, xv[j][:, :], t1[:, :], AF.Copy, [t1], [xv[j]])
                yield
            for pr in range(2):
                pcols = slice(pr * 128, (pr + 1) * 128)
                r_, k_, v_ = xr[pr], xk[pr], xv[pr]
                p = proj([(wg2[:, pcols], sgl[:, :])], [wg2, sgl])
                ACTF(k, gate[pr][:, :], p[:, :], AF.Copy, [p], [gate[pr]])
                yield
                p = proj([(wl_a[:, pcols], xlo[:, :])], [wl_a, xlo])
                ACTF(k, aa[:, :], p[:, :], AF.Sigmoid, [p, vec], [aa], bias=vec[:, o_a0 + pr:o_a0 + pr + 1])
                yield
                p = proj([(wl_w[:, pcols], xlo[:, :])], [wl_w, xlo])
                ACTF(k, t2[:, :], p[:, :], AF.Sigmoid, [p, vec], [t2], bias=vec[:, o_w0 + pr:o_w0 + pr + 1])
                TS(k.dve, t2[:, :], t2[:, :], DEC, None, ALU.mult, ALU.bypass, [t2], [t2])
                k.dve.op(lambda: nc.vector.tensor_tensor_scan(out=cc[:, :], data0=cmask[:, :], data1=t2[:, :], initial=0.0,
                                                              op0=ALU.mult, op1=ALU.add), reads=[cmask, t2], writes=[cc])
                yield
                TS(k.dve, kkt[:, :], k_[:, :], vec[:, o_kk + pr:o_kk + pr + 1], None, ALU.mult, ALU.bypass, [k_, vec], [kkt])
                rstd = nrm.stats([kkt[:, :]], [kkt], 1.0, 1e-24)
                TT(k.dve, kkt[:, :], kkt[:, :], rstd[:, :], ALU.mult, [kkt, rstd], [kkt])
                yield
                TS(k.dve, t3[:, :], aa[:, :], vec[:, o_ka + pr:o_ka + pr + 1], omka[:, pr:pr + 1], ALU.mult, ALU.add,
                   [aa, vec, omka], [t3])
                TT(k.dve, kmod[:, :], k_[:, :], t3[:, :], ALU.mult, [k_, t3], [kmod])
                yield
                STT(k, tb1[:, :], r_[:, :], vec[:, o_rk + pr:o_rk + pr + 1], kmod[:, :], ALU.mult, ALU.mult,
                    [r_, vec, kmod], [tb1])
                p = proj([(ones2[:, :], tb1[:, :])], [ones2, tb1])
                TT(k.dve, bon[pr][:, :], p[:, :], v_[:, :], ALU.mult, [p, v_], [bon[pr]])
                ACTF(k, vb[pr][:, :], v_[:, :], AF.Copy, [v_], [vb[pr]])
                yield
                ACTF(k, t3[:, :], cc[:, :], AF.Exp, [cc], [t3], scale=-1.0)
                for c in range(NCH):
                    for hl in range(2):
                        STT(k, ar[pr][hl][:, c, 128:256], r_[:, c * C:(c + 1) * C], hm2[:, hl:hl + 1],
                            t3[:, c * C:(c + 1) * C], ALU.mult, ALU.mult, [r_, t3, hm2], [ar[pr][hl]])
                    yield
                TT(k.dve, t3[:, :], cc[:, :], t2[:, :], ALU.subtract, [cc, t2], [t3])
                ACTF(k, t3[:, :], t3[:, :], AF.Exp, [t3], [t3], scale=-1.0)
                for c in range(NCH):
                    for hl in range(2):
                        STT(k, ar[pr][hl][:, c, 0:128], kkt[:, c * C:(c + 1) * C], nhm2[:, hl:hl + 1],
                            t3[:, c * C:(c + 1) * C], ALU.mult, ALU.mult, [kkt, t3, nhm2], [ar[pr][hl]])
                    yield
                TT(k.dve, aa[:, :], aa[:, :], kkt[:, :], ALU.mult, [aa, kkt], [aa])
                ACTF(k, t3[:, :], cc[:, :], AF.Exp, [cc], [t3], scale=1.0)
                TT(k.dve, bt[pr][:, :], aa[:, :], t3[:, :], ALU.mult, [aa, t3], [bt[pr]])
                TT(k.dve, ktl[pr][:, :], kmod[:, :], t3[:, :], ALU.mult, [kmod, t3], [ktl[pr]])
                yield
                TS(k.dve, nbl[:, :], cc[:, C - 1::C], -1.0, None, ALU.mult, ALU.bypass, [cc], [nbl])
                ACTF(k, pC[pr][:, :], nbl[:, :], AF.Exp, [nbl], [pC[pr]])
                for c in range(NCH):
                    ACTF(k, t3[:, c * C:(c + 1) * C], cc[:, c * C:(c + 1) * C], AF.Exp, [cc, nbl], [t3], scale=1.0,
                         bias=nbl[:, c:c + 1])
                TT(k.dve, Bp[pr][:, :], aa[:, :], t3[:, :], ALU.mult, [aa, t3], [Bp[pr]])
                TT(k.dve, Kp[pr][:, :], kmod[:, :], t3[:, :], ALU.mult, [kmod, t3], [Kp[pr]])

                yield

        def drain(g):
            if g is not None:
                for _ in g:
                    pass

        drain(prepR(0))
        for b in range(T // TB):
            t0 = b * TB
            gate, bon, vb, ar, bt, ktl, pC, Bp, Kp = sets[b % 2]
            gp = prepR(b + 1) if (b + 1) * TB < T else None
            hs = [slice(0, 64), slice(64, 128)]

            def stageA(pr, c, sid):
                cs = slice(c * C, (c + 1) * C)
                TM, AbRb, AkRk, PP, X, Xw = TM_[sid], AbRb_[sid], AkRk_[sid], PP_[sid], X_[sid], Xw_[sid]
                ps_x, ps_pp = ps_x_[sid], ps_pp_[sid]
                s_ = bank()
                MM(k, s_[:, 0:128], ar[pr][0][:, c, 0:128], ident[:, :], [ar[pr][0], ident], [s_], stop=False, inc=False)
                MM(k, s_[:, 0:128], ar[pr][1][:, c, 0:128], ident[:, :], [ar[pr][1], ident], [s_], start=False, inc=False)
                for n_, src in enumerate((Bp[pr][:, cs], Kp[pr][:, cs], vb[pr][:, cs])):
                    MM(k, s_[:, (n_ + 1) * 128:(n_ + 2) * 128], src, ident[:, :], [Bp[pr], Kp[pr], vb[pr], ident],
                       [s_], inc=(n_ == 2))
                ACTF(k, TM[:, :, :], s_[:, :], AF.Copy, [s_], [TM])
                yield
                s1 = bank()
                for hl in range(2):
                    MM(k, s1[:, hl * 256:(hl + 1) * 256], bt[pr][:, cs], ar[pr][hl][:, c, :], [bt[pr], ar[pr][hl]],
                       [s1], inc=(hl == 1))
                TT(k.dve, AbRb[:, :, :], s1[:, :], m_si[:, :, :], ALU.mult, [s1, m_si], [AbRb])
                yield
                s2 = bank()
                for hl in range(2):
                    MM(k, s2[:, hl * 256:(hl + 1) * 256], ktl[pr][:, cs], ar[pr][hl][:, c, :], [ktl[pr], ar[pr][hl]],
                       [s2], inc=(hl == 1))
                TT(k.dve, AkRk[:, :, :], s2[:, :], m_si[:, :, :], ALU.mult, [s2, m_si], [AkRk])
                yield
                s3 = bank()
                for hl in range(2):
                    MM(k, s3[:, hl * 128:(hl + 1) * 128], ar[pr][hl][:, c, 0:128], bt[pr][:, cs], [bt[pr], ar[pr][hl]],
                       [s3], inc=(hl == 1))
                P0 = PP[0]
                TT(k.dve, P0[:, :, 0:128], s3[:, 0:256], m_tt[:, :, :], ALU.mult, [s3, m_tt], [P0])
                k.pool.op(lambda P0=P0: nc.gpsimd.tensor_copy(out=P0[:, :, 128:256], in_=AbRb[:, :, 0:128]),
                          reads=[AbRb], writes=[P0])
                yield
                for hl in range(2):
                    MM(k, ps_x[:, hl * 128 + 64:(hl + 1) * 128], AkRk[:, hl, 0:128], TM[:, 3, hs[hl]], [AkRk, TM],
                       [ps_x], inc=(hl == 1))
                for hl in range(2):
                    ACTF(k, X[:, hl, 0:64], TM[:, 0, hs[hl]], AF.Copy, [TM], [X])
                    ACTF(k, X[:, hl, 64:128], ps_x[:, hl * 128 + 64:(hl + 1) * 128], AF.Copy, [ps_x], [X])
                yield
                for lv in range(7):
                    Pc = PP[lv % 2]
                    for hl in range(2):
                        MM(k, ps_x[:, hl * 128:(hl + 1) * 128], Pc[:, hl, 128:256], X[:, hl, :], [Pc, X], [ps_x],
                           inc=(hl == 1))
                    if lv < 6:
                        Pn = PP[(lv + 1) % 2]
                        for hl in range(2):
                            MM(k, ps_pp[:, hl, 0:128], Pc[:, hl, 128:256], Pc[:, hl, 0:128], [Pc], [ps_pp], inc=False)
                            MM(k, ps_pp[:, hl, 128:256], Pc[:, hl, 0:128], Pc[:, hl, 128:256], [Pc], [ps_pp],
                               inc=(hl == 1))
                        ACTF(k, Pn[:, :, :], ps_pp[:, :, :], AF.Copy, [ps_pp], [Pn])
                    TT(k.dve, X[:, :, :], ps_x[:, 0:256], X[:, :, :], ALU.add, [ps_x, X], [X])
                    yield
                for hl in range(2):
                    ACTF(k, Xw[hl][:, hs[hl]], X[:, hl, 0:64], AF.Copy, [X], [Xw[hl]])
                for hl in range(2):
                    pq = bank()
                    MM(k, pq[:, 0:128], Xw[hl][:, :], AbRb[:, hl, 128:256], [Xw[hl], AbRb], [pq])
                    TT(k.dve, QpT[pr][hl][:, cs], pq[:, 0:128], ar[pr][hl][:, c, 128:256], ALU.add, [pq, ar[pr][hl]],
                       [QpT[pr][hl]])
                yield
                py0 = bank()
                for hl in range(2):
                    MM(k, py0[hs[hl], 0:128], X[:, hl, 64:128], AbRb[:, hl, 128:256], [X, AbRb], [py0], stop=False,
                       inc=False)
                    MM(k, py0[hs[hl], 0:128], TM[:, 3, hs[hl]], AkRk[:, hl, 128:256], [TM, AkRk], [py0], start=False,
                       inc=(hl == 1))
                ACTF(k, Y0T[pr][:, cs], py0[:, 0:128], AF.Copy, [py0], [Y0T[pr]])
                yield
                for hl in range(2):
                    pm = bank()
                    MM(k, pm[:, 0:64], Xw[hl][:, :], TM[:, 1, hs[hl]], [Xw[hl], TM], [pm])
                    STT(k, MT[pr][hl][:, c, :], I2h[hl][:, :], pC[pr][:, c:c + 1], pm[:, 0:64], ALU.mult, ALU.add,
                        [I2h[hl], pC[pr], pm], [MT[pr][hl]])
                yield
                pg = bank()
                for hl in range(2):
                    MM(k, pg[hs[hl], 0:64], TM[:, 1, hs[hl]], X[:, hl, 64:128], [X, TM], [pg], stop=False, inc=False)
                    MM(k, pg[hs[hl], 0:64], TM[:, 2, hs[hl]], TM[:, 3, hs[hl]], [TM], [pg], start=False,
                       inc=(hl == 1))
                ACTF(k, Gm[pr][:, c, :], pg[:, 0:64], AF.Copy, [pg], [Gm[pr]])

            pending = [(pr, c) for c in range(NCH) for pr in range(2)]
            active = {}
            while pending or active:
                for sid in range(NS):
                    if sid not in active and pending:
                        pr_, c_ = pending.pop(0)
                        active[sid] = stageA(pr_, c_, sid)
                for sid in list(active):
                    try:
                        next(active[sid])
                    except StopIteration:
                        del active[sid]
                if gp is not None:
                    try:
                        next(gp)
                    except StopIteration:
                        gp = None
            for c in range(NCH):
                cs = slice(c * C, (c + 1) * C)
                for pr in range(2):
                    py = bank()
                    for hl in range(2):
                        MM(k, py[hs[hl], 0:128], Hb[pr][:, :], QpT[pr][hl][:, cs], [Hb[pr], QpT[pr][hl]], [py], inc=(hl == 1))
                    TT(k.dve, yacc[pr][:, cs], py[:, 0:128], Y0T[pr][:, cs], ALU.add, [py, Y0T[pr]], [yacc[pr]])
                    ph = bank()
                    for hl in range(2):
                        MM(k, ph[hs[hl], 0:64], MT[pr][hl][:, c, :], Hb[pr][:, :], [MT[pr][hl], Hb[pr]], [ph], inc=(hl == 1))
                    TT(k.dve, Hb[pr][:, :], ph[:, 0:64], Gm[pr][:, c, :], ALU.add, [ph, Gm[pr]], [Hb[pr]])
            for pr in range(2):
                ACTF(k, ob1[:, :], yacc[pr][:, :], AF.Copy, [yacc[pr]], [ob1])
                p = proj([(ones2[:, :], ob1[:, :])], [ones2, ob1])
                STT(k, o1[:, :], p[:, :], -1.0 / RW_N, yacc[pr][:, :], ALU.mult, ALU.add, [p, yacc[pr]], [o1])
                rstd = nrm.stats([o1[:, :]], [o1], RW_N, RW_EPS)
                TT(k.dve, o1[:, :], o1[:, :], rstd[:, :], ALU.mult, [o1, rstd], [o1])
                TS(k.dve, o1[:, :], o1[:, :], vec[:, o_lg + pr:o_lg + pr + 1], vec[:, o_lb + pr:o_lb + pr + 1], ALU.mult,
                   ALU.add, [o1, vec], [o1])
                TT(k.dve, o1[:, :], o1[:, :], bon[pr][:, :], ALU.add, [o1, bon[pr]], [o1])
                TT(k.dve, yo[pr][:, :], o1[:, :], gate[pr][:, :], ALU.mult, [o1, gate[pr]], [yo[pr]])
                k.sp.dma(dr["yT"][:, 6 + pr, t0:t0 + TB], yo[pr][:, :], yo[pr], reads=[yo[pr]], writes=[dr["yT_buf"]])
            drain(gp)


def phase_out(k, T, l, dr, cst):
    nc = k.nc
    with k.phase() as es:
        vec = k.sb(es, "o_vec", [128, NV], F32)
        k.sp.dma(vec[:, :], dr["vecs"][l], vec, writes=[vec])
        ones = k.sb(es, "o_ones", [128, 128], BF16)
        load_cast(k, ones, ones[:, :], cst["ones"])
        wo = k.sb(es, "o_wo", [128, 8, D], BF16)
        load_cast3(k, wo, wo[:, :, :], dr["w_out"][l], 0, D)
        nrm = Norm(k, es, "o_n", TB, ones)
        xb = [k.sb(es, f"o_xb{i}", [128, 8, TB], F32) for i in range(2)]
        yb = [k.sb(es, f"o_yb{i}", [128, 8, TB], BF16) for i in range(2)]
        yn = k.sb(es, "o_yn", [128, 4, TB], BF16)
        ps = [k.ps(es, f"o_ps{i}", [128, TB]) for i in range(3)]
        o_g = VOFF["on_g"][0]
        xsrc, xsrc_buf = (dr["x_in"], Buf(None, "xin")) if l == 0 else (dr["xT"], dr["xT_buf"])
        for b in range(T // TB):
            t0 = b * TB
            x, y = xb[b % 2], yb[b % 2]
            k.sp.dma(x[:, :, :], xsrc[:, :, t0:t0 + TB], x, reads=[xsrc_buf], writes=[x])
            k.sp.dma(y[:, :, :], dr["yT"][:, :, t0:t0 + TB], y, reads=[dr["yT_buf"]], writes=[y])
            rstd = nrm.stats([y[:, c, :] for c in range(4)], [y] * 4, MLA_H * DV, EPS)
            for c in range(4):
                k.dve.op(lambda c=c, y=y: nc.vector.scalar_tensor_tensor(
                    out=yn[:, c, :], in0=y[:, c, :], scalar=vec[:, o_g + c:o_g + c + 1], in1=rstd[:, :],
                    op0=ALU.mult, op1=ALU.mult), reads=[y, vec, rstd], writes=[yn])
            for m in range(8):
                p = ps[m % 3]
                mm(k, p, p[:, :], [(wo[:, c, m * 128:(m + 1) * 128], yn[:, c, :] if c < 4 else y[:, c, :])
                                   for c in range(8)], [wo, yn, y])
                k.dve.op(lambda m=m, p=p, x=x: nc.vector.tensor_tensor(out=x[:, m, :], in0=p[:, :], in1=x[:, m, :],
                                                                       op=ALU.add), reads=[p, x], writes=[x])
            k.sp.dma(dr["xT"][:, :, t0:t0 + TB], x[:, :, :], x, reads=[x], writes=[dr["xT_buf"]])


def build(T, shapes, phases=("h0", "mla", "gla", "rwkv", "out", "ffn"), depth=DEPTH, debug=()):
    nc = bass.Bass("TRN2", target_bir_lowering=False)
    dr = {}
    for n, shp in shapes.items():
        dr[n] = nc.dram_tensor(n, list(shp), F32, kind="ExternalInput").ap()
    dr["x_in"] = dr["xT_in"].rearrange("(c p) t -> p c t", p=128)

    def scratch(name, dtype):
        t = nc.dram_tensor(name, [D, T], dtype, kind="ExternalOutput" if name in debug else "Internal").ap()
        dr[name] = t.rearrange("(c p) t -> p c t", p=128)
        dr[name + "_buf"] = Buf(None, name)

    outT = nc.dram_tensor("outT", [D, T], F32, kind="ExternalOutput").ap()
    dr["outT"] = outT.rearrange("(c p) t -> p c t", p=128)
    dr["outT_buf"] = Buf(None, "outT")
    scratch("xT", F32)
    scratch("hT", BF16)
    scratch("yT", BF16)
    cst = {"ones": dr["c_ones"], "amask": dr["c_amask"], "cos4": dr["c_cos4"], "sin4": dr["c_sin4"],
           "misc": dr["c_misc"]}
    with contextlib.ExitStack() as es:
        k = K(nc, es)
        if "h0" not in phases:
            with k.phase() as pes:
                t = k.sb(pes, "cp", [128, 8, TB], F32)
                for b in range(T // TB):
                    k.sp.dma(t[:, :, :], dr["x_in"][:, :, b * TB:(b + 1) * TB], t, writes=[t])
                    k.sp.dma(dr["xT"][:, :, b * TB:(b + 1) * TB], t[:, :, :], t, reads=[t], writes=[dr["xT_buf"]])
        else:
            phase_h0(k, T, dr, cst)
        for l in range(depth):
            if "mla" in phases:
                phase_mla(k, T, l, dr, cst, (0, 1))
                phase_mla(k, T, l, dr, cst, (2, 3))
            if "gla" in phases:
                phase_gla(k, T, l, dr, cst)
            if "rwkv" in phases:
                phase_rwkv(k, T, l, dr, cst)
            if "out" in phases:
                phase_out(k, T, l, dr, cst)
            if "ffn" in phases:
                phase_ffn(k, T, l, dr, cst, last=(l == depth - 1))
        k.barrier()
    return nc


W_NAMES = ["w_in", "mla_w_uq", "mla_w_ukv", "gla_w_gk", "rwkv_w2", "rwkv_a2", "rwkv_g2", "w_out",
           "ffn_w_up", "ffn_w_down"]


def pack_vecs(inp, l):
    v = np.zeros((128, NV), np.float32)

    def put(name, arr, n):
        o, c = VOFF[name]
        assert c == n
        v[:, o:o + n] = _cols(arr, n)

    put("ln1_g", inp["ln1_g"][l], 8)
    put("ln2_g", inp["ln2_g"][l], 8)
    put("final_g", inp["final_g"], 8)
    put("qn_g", inp["mla_q_norm_g"][l], 3)
    put("kvn_g", inp["mla_kv_norm_g"][l], 2)
    put("on_g", inp["mla_out_norm_g"][l], 4)
    put("gla_nb", inp["gla_b_gk"][l], 1)
    put("gla_ng", np.tile(inp["gla_norm_g"][l], 2), 1)
    put("rw_mu", inp["rwkv_mu"][l], 8)
    put("rw_w0", inp["rwkv_w0"][l], 2)
    put("rw_a0", inp["rwkv_a0"][l], 2)
    put("rw_kk", inp["rwkv_k_k"][l], 2)
    put("rw_ka", inp["rwkv_k_a"][l], 2)
    put("rw_rk", inp["rwkv_r_k"][l].reshape(-1), 2)
    put("rw_lng", inp["rwkv_ln_g"][l], 2)
    put("rw_lnb", inp["rwkv_ln_b"][l], 2)
    cw = inp["ffn_conv_w"][l]
    put("cw0", cw[0], 44)
    put("cw1", cw[1], 44)
    put("cw2", cw[2], 44)
    put("cb", inp["ffn_conv_b"][l], 44)
    return v


def host_inputs(inp, b, T):
    m = {}
    m["xT_in"] = np.ascontiguousarray(np.asarray(inp["x"][b, :T], np.float32).T)
    m["vecs"] = np.stack([pack_vecs(inp, l) for l in range(DEPTH)])
    m["c_ones"] = np.ones((128, 128), np.float32)
    m["c_misc"] = make_misc()
    kk = np.arange(128)[:, None]
    qq = np.arange(TB)[None, :]
    m["c_amask"] = np.stack([np.where(kk + 128 * d <= qq, 0.0, -30000.0).astype(np.float32) for d in range(4)])
    inv = (np.float32(1.0) / (np.float32(10000.0) ** (np.arange(0, ROPE, 2, dtype=np.float32) / np.float32(ROPE)))
           ).astype(np.float32)
    ang = (np.arange(T, dtype=np.float32)[None, :] * inv[:, None]).astype(np.float32)
    cs, sn = np.cos(ang).astype(np.float32), np.sin(ang).astype(np.float32)
    m["c_cos4"] = np.ascontiguousarray(np.concatenate([cs, cs, cs, cs], 0))
    m["c_sin4"] = np.ascontiguousarray(np.concatenate([-sn, sn, -sn, sn], 0))
    for n in W_NAMES:
        m[n] = np.ascontiguousarray(np.asarray(inp[n], np.float32))
    return m


def run(inp, T, nb, **bk):
    maps = [host_inputs(inp, b, T) for b in range(nb)]
    shapes = {n: a.shape for n, a in maps[0].items()}
    nc = build(T, shapes, **bk)
    res = run_bass_kernel_spmd(nc, maps, core_ids=list(range(nb)))
    return res


def kernel(**inputs):
    inp = {n: np.asarray(a) for n, a in inputs.items()}
    B, T = inp["x"].shape[0], inp["x"].shape[1]
    ncores = 8
    maps = [host_inputs(inp, b % B, T) for b in range(ncores)]
    shapes = {n: a.shape for n, a in maps[0].items()}
    nc = build(T, shapes)
    res = run_bass_kernel_spmd(nc, maps, core_ids=list(range(ncores)))
    out = np.stack([np.ascontiguousarray(res.results[b]["outT"].T) for b in range(B)])
    return out.astype(np.float32)


def phase_h0(k, T, dr, cst):
    nc = k.nc
    with k.phase() as es:
        vec = k.sb(es, "h_vec", [128, NV], F32)
        k.sp.dma(vec[:, :], dr["vecs"][0], vec, writes=[vec])
        ones = k.sb(es, "h_ones", [128, 128], BF16)
        load_cast(k, ones, ones[:, :], cst["ones"])
        nrm = Norm(k, es, "h_n", TB, ones)
        xb = [k.sb(es, f"h_xb{i}", [128, 8, TB], F32) for i in range(2)]
        hT = [k.sb(es, f"h_hT{i}", [128, 8, TB], BF16) for i in range(2)]
        o_g = VOFF["ln1_g"][0]
        for b in range(T // TB):
            x, h = xb[b % 2], hT[b % 2]
            t0 = b * TB
            k.sp.dma(x[:, :, :], dr["x_in"][:, :, t0:t0 + TB], x, writes=[x])
            rstd = nrm.stats([x[:, c, :] for c in range(8)], [x] * 8, D, EPS)
            for c in range(8):
                k.dve.op(lambda c=c, x=x, h=h: nc.vector.scalar_tensor_tensor(
                    out=h[:, c, :], in0=x[:, c, :], scalar=vec[:, o_g + c:o_g + c + 1], in1=rstd[:, :],
                    op0=ALU.mult, op1=ALU.mult), reads=[x, vec, rstd], writes=[h])
            k.sp.dma(dr["hT"][:, :, t0:t0 + TB], h[:, :, :], h, reads=[h], writes=[dr["hT_buf"]])


def phase_mla(k, T, l, dr, cst, heads):
    nc = k.nc
    nh = len(heads)
    nblk = T // TB
    NKT = T // 128
    scale = float((NOPE + ROPE) ** -0.5)
    w_in, w_uq, w_ukv = dr["w_in"], dr["mla_w_uq"], dr["mla_w_ukv"]
    with k.phase() as es:
        vec = k.sb(es, "a_vec", [128, NV], F32)
        k.sp.dma(vec[:, :], dr["vecs"][l], vec, writes=[vec])
        ones = k.sb(es, "a_ones", [128, 128], BF16)
        load_cast(k, ones, ones[:, :], cst["ones"])
        masks = k.sb(es, "a_mask", [128, 4, TB], BF16)
        k.pool.dma(masks[:, :, :], cst["amask"].rearrange("d p n -> p d n"), masks, writes=[masks])
        ident = k.sb(es, "a_ident", [128, 128], BF16)
        load_cast(k, ident, ident[:, :], cst["misc"][:, MOFF["ident"][0]:MOFF["ident"][1]])
        wA = k.sb(es, "a_wA", [128, 8, 896], BF16)
        load_cast3(k, wA, wA[:, :, 0:640], w_in[l], 0, 640)
        for r in range(2):
            load_cast3(k, wA, wA[:, :, 640 + 64 * r:704 + 64 * r], w_in[l], 640, 704)
            load_cast3(k, wA, wA[:, :, 768 + 64 * r:800 + 64 * r], w_in[l], 672, 704)
            load_cast3(k, wA, wA[:, :, 800 + 64 * r:832 + 64 * r], w_in[l], 640, 672)
        wq = k.sb(es, "a_wq", [128, 3, 3 * nh * 128], BF16)
        ro = nh * 128
        so = 2 * nh * 128
        k.pool.op(lambda: nc.gpsimd.memset(wq[:, :, :], 0.0), writes=[wq])
        for i, h in enumerate(heads):
            b0 = h * 192
            load_cast3(k, wq, wq[:, :, i * 128:(i + 1) * 128], w_uq[l], b0, b0 + 128)
            load_cast3(k, wq, wq[:, :, ro + i * 128:ro + i * 128 + 64], w_uq[l], b0 + 128, b0 + 192)
            load_cast3(k, wq, wq[:, :, so + i * 128:so + i * 128 + 32], w_uq[l], b0 + 160, b0 + 192)
            load_cast3(k, wq, wq[:, :, so + i * 128 + 32:so + i * 128 + 64], w_uq[l], b0 + 128, b0 + 160)
        wk = k.sb(es, "a_wk", [128, 2, nh * 128], BF16)
        wv = k.sb(es, "a_wv", [128, 2, nh * 128], BF16)
        for i, h in enumerate(heads):
            load_cast3(k, wk, wk[:, :, i * 128:(i + 1) * 128], w_ukv[l], h * 256, h * 256 + 128)
            load_cast3(k, wv, wv[:, :, i * 128:(i + 1) * 128], w_ukv[l], h * 256 + 128, h * 256 + 256)
        kn = [k.sb(es, f"a_kn{i}", [128, T], BF16) for i in range(nh)]
        kr = k.sb(es, "a_kr", [128, T], BF16)
        vc = k.sb(es, "a_vc", [128, NKT, nh * 128], BF16)
        hT = [k.sb(es, f"a_hT{i}", [128, 8, TB], BF16) for i in range(2)]
        cq = k.sb(es, "a_cq", [128, 3, TB], F32)
        ckv = k.sb(es, "a_ckv", [128, 2, TB], F32)
        cqn = k.sb(es, "a_cqn", [128, 3, TB], BF16)
        ckvn = k.sb(es, "a_ckvn", [128, 2, TB], BF16)
        qn = [k.sb(es, f"a_qn{i}", [128, TB], BF16) for i in range(nh)]
        qr = [k.sb(es, f"a_qr{i}", [128, TB], BF16) for i in range(nh)]
        cos = k.sb(es, "a_cos", [128, TB], F32)
        sin = k.sb(es, "a_sin", [128, TB], F32)
        t1 = k.sb(es, "a_t1", [128, TB], F32)
        t2 = k.sb(es, "a_t2", [128, TB], F32)
        pT = [k.sb(es, f"a_pT{i}", [128, TB], BF16) for i in range(3)]
        oT = [k.sb(es, f"a_oT{i}", [128, TB], BF16) for i in range(2)]
        rcp = k.sb(es, "a_rcp", [128, TB], F32)
        paccs = [k.sb(es, f"a_pacc{i}", [128, TB], F32) for i in range(2)]
        oraw = k.sb(es, "a_oraw", [128, TB], F32)
        ones_f = k.sb(es, "a_onesf", [128, 128], F32)
        k.sp.dma(ones_f[:, :], cst["ones"], ones_f, writes=[ones_f])
        nrm = Norm(k, es, "a_n", TB, ones)
        ps_p = [k.ps(es, f"a_psp{i}", [128, TB]) for i in range(2)]
        ps_s = [k.ps(es, f"a_pss{i}", [128, TB]) for i in range(3)]
        ps_o = k.ps(es, "a_pso", [128, TB])
        ps_l = k.ps(es, "a_psl", [128, TB])
        o_qg, o_kg = VOFF["qn_g"][0], VOFF["kvn_g"][0]
        ip = 0
        ipt = 0

        def proj(lhs_list, rhs_list, reads, m=128, n=TB):
            nonlocal ip
            p = ps_p[ip % 2]
            ip += 1
            mm(k, p, p[0:m, 0:n], list(zip(lhs_list, rhs_list)), reads)
            return p

        kn_b = [[Buf(kn[i].ap[:, bb * TB:(bb + 1) * TB], f"kn{i}_{bb}") for bb in range(nblk)] for i in range(nh)]
        kr_b = [Buf(kr.ap[:, bb * TB:(bb + 1) * TB], f"kr_{bb}") for bb in range(nblk)]
        vc_b = [Buf(vc.ap[:, bb * 4:(bb + 1) * 4, :], f"vc_{bb}") for bb in range(nblk)]
        qn2 = [qn, [k.sb(es, f"a_qnB{i}", [128, TB], BF16) for i in range(nh)]]
        qr2 = [qr, [k.sb(es, f"a_qrB{i}", [128, TB], BF16) for i in range(nh)]]

        def prep(b):
            t0 = b * TB
            h = hT[b % 2]
            qn_, qr_ = qn2[b % 2], qr2[b % 2]
            k.sp.dma(h[:, :, :], dr["hT"][:, :, t0:t0 + TB], h, reads=[dr["hT_buf"]], writes=[h])
            k.sp.dma(cos[:, :], cst["cos4"][:, t0:t0 + TB], cos, writes=[cos])
            k.sp.dma(sin[:, :], cst["sin4"][:, t0:t0 + TB], sin, writes=[sin])
            yield
            for j in range(3):
                p = proj([wA[:, c, j * 128:(j + 1) * 128] for c in range(8)], [h[:, c, :] for c in range(8)], [wA, h])
                k.act.op(lambda p=p, j=j: nc.scalar.activation(out=cq[:, j, :], in_=p[:, :], func=AF.Copy),
                         reads=[p], writes=[cq])
                yield
            for j in range(2):
                p = proj([wA[:, c, 384 + j * 128:384 + (j + 1) * 128] for c in range(8)],
                         [h[:, c, :] for c in range(8)], [wA, h])
                k.act.op(lambda p=p, j=j: nc.scalar.activation(out=ckv[:, j, :], in_=p[:, :], func=AF.Copy),
                         reads=[p], writes=[ckv])
                yield
            rstd = nrm.stats([cq[:, j, :] for j in range(3)], [cq] * 3, QLORA, EPS)
            yield
            for j in range(3):
                k.dve.op(lambda j=j: nc.vector.scalar_tensor_tensor(
                    out=cqn[:, j, :], in0=cq[:, j, :], scalar=vec[:, o_qg + j:o_qg + j + 1], in1=rstd[:, :],
                    op0=ALU.mult, op1=ALU.mult), reads=[cq, vec, rstd], writes=[cqn])
            yield
            rstd = nrm.stats([ckv[:, j, :] for j in range(2)], [ckv] * 2, KVLORA, EPS)
            yield
            for j in range(2):
                k.dve.op(lambda j=j: nc.vector.scalar_tensor_tensor(
                    out=ckvn[:, j, :], in0=ckv[:, j, :], scalar=vec[:, o_kg + j:o_kg + j + 1], in1=rstd[:, :],
                    op0=ALU.mult, op1=ALU.mult), reads=[ckv, vec, rstd], writes=[ckvn])
            yield
            pa = proj([wA[:, c, 640:768] for c in range(8)], [h[:, c, :] for c in range(8)], [wA, h])
            k.dve.op(lambda pa=pa: nc.vector.tensor_tensor(out=t1[:, :], in0=pa[:, :], in1=cos[:, :], op=ALU.mult),
                     reads=[pa, cos], writes=[t1])
            yield
            pb = proj([wA[:, c, 768:896] for c in range(8)], [h[:, c, :] for c in range(8)], [wA, h])
            k.dve.op(lambda pb=pb: nc.vector.tensor_tensor(out=t2[:, :], in0=pb[:, :], in1=sin[:, :], op=ALU.mult),
                     reads=[pb, sin], writes=[t2])
            k.pool.op(lambda: nc.gpsimd.tensor_tensor(out=kr_b[b][:, :], in0=t1[:, :], in1=t2[:, :], op=ALU.add),
                      reads=[t1, t2], writes=[kr_b[b]])
            yield
            for i in range(nh):
                p = proj([wk[:, c, i * 128:(i + 1) * 128] for c in range(2)], [ckvn[:, c, :] for c in range(2)],
                         [wk, ckvn])
                k.act.op(lambda p=p, i=i: nc.scalar.activation(out=kn_b[i][b][:, :], in_=p[:, :], func=AF.Copy),
                         reads=[p], writes=[kn_b[i][b]])
                yield
            for s_ in range(TB // 128):
                p = proj([ckvn[:, c, s_ * 128:(s_ + 1) * 128] for c in range(2)], [wv[:, c, :] for c in range(2)],
                         [wv, ckvn], n=nh * 128)
                k.act.op(lambda p=p, s_=s_: nc.scalar.activation(out=vc_b[b][:, s_, :], in_=p[:, 0:nh * 128],
                                                                 func=AF.Copy), reads=[p], writes=[vc_b[b]])
                yield
            for i in range(nh):
                p = proj([wq[:, c, i * 128:(i + 1) * 128] for c in range(3)], [cqn[:, c, :] for c in range(3)],
                         [wq, cqn])
                k.act.op(lambda p=p, i=i: nc.scalar.activation(out=qn_[i][:, :], in_=p[:, :], func=AF.Copy),
                         reads=[p], writes=[qn_[i]])
                yield
            for pr in range(nh):
                pa = proj([wq[:, c, ro + pr * 128:ro + (pr + 1) * 128] for c in range(3)],
                          [cqn[:, c, :] for c in range(3)], [wq, cqn])
                k.dve.op(lambda pa=pa: nc.vector.tensor_tensor(out=t1[:, :], in0=pa[:, :], in1=cos[:, :],
                                                               op=ALU.mult), reads=[pa, cos], writes=[t1])
                yield
                pb = proj([wq[:, c, so + pr * 128:so + (pr + 1) * 128] for c in range(3)],
                          [cqn[:, c, :] for c in range(3)], [wq, cqn])
                k.dve.op(lambda pb=pb: nc.vector.tensor_tensor(out=t2[:, :], in0=pb[:, :], in1=sin[:, :],
                                                               op=ALU.mult), reads=[pb, sin], writes=[t2])
                k.pool.op(lambda pr=pr: nc.gpsimd.tensor_tensor(out=qr_[pr][:, :], in0=t1[:, :], in1=t2[:, :],
                                                                op=ALU.add), reads=[t1, t2], writes=[qr_[pr]])
                yield

        def drain(g):
            if g is not None:
                for _ in g:
                    pass

        drain(prep(0))
        for b in range(nblk):
            t0 = b * TB
            qn_, qr_ = qn2[b % 2], qr2[b % 2]
            gnext = prep(b + 1) if b + 1 < nblk else None
            nkt = (t0 + TB) // 128
            its = [(i, kt) for i in range(nh) for kt in range(nkt)]
            sb_of = {}

            def issue_s(n):
                nonlocal ipt
                i, kt = its[n]
                s = ps_s[ipt % 3]
                ipt += 1
                bb, ks = kt // 4, slice((kt % 4) * 128, (kt % 4 + 1) * 128)
                pairs = [(kn_b[i][bb][:, ks], qn_[i][:, :]), (kr_b[bb][:, ks], qr_[i][:, :])]
                d_ = kt - (t0 // 128)
                if d_ >= 0:
                    pairs.append((ident[:, :], masks[:, d_, :]))
                mm(k, s, s[:, :], pairs, [kn_b[i][bb], qn_[i], kr_b[bb], qr_[i], ident, masks])
                sb_of[n] = s

            for n in range(min(2, len(its))):
                issue_s(n)
            for n, (i, kt) in enumerate(its):
                if n + 2 < len(its):
                    issue_s(n + 2)
                if gnext is not None:
                    try:
                        next(gnext)
                    except StopIteration:
                        gnext = None
                s = sb_of.pop(n)
                pt = pT[n % 3]
                k.act.op(lambda s=s, pt=pt: nc.scalar.activation(out=pt[:, :], in_=s[:, :], func=AF.Exp,
                                                                 scale=scale), reads=[s], writes=[pt])
                bb = kt // 4
                k.pe.op(lambda pt=pt, kt=kt, i=i, bb=bb: nc.tensor.matmul(
                    ps_o[:, :], lhsT=vc_b[bb][:, kt % 4, i * 128:(i + 1) * 128], rhs=pt[:, :], start=(kt == 0),
                    stop=(kt == nkt - 1)), reads=[vc_b[bb], pt], writes=[ps_o], inc=True)
                pacc = paccs[kt % 2]
                e_ = k.dve if kt % 2 == 0 else k.pool
                if kt < 2:
                    e_.op(lambda pt=pt, e_=e_, pacc=pacc: e_.eng.tensor_copy(out=pacc[:, :], in_=pt[:, :]),
                          reads=[pt], writes=[pacc])
                else:
                    e_.op(lambda pt=pt, e_=e_, pacc=pacc: e_.eng.tensor_tensor(out=pacc[:, :], in0=pacc[:, :],
                                                                               in1=pt[:, :], op=ALU.add),
                          reads=[pt, pacc], writes=[pacc])
                if kt == nkt - 1:
                    k.act.op(lambda: nc.scalar.activation(out=oraw[:, :], in_=ps_o[:, :], func=AF.Copy),
                             reads=[ps_o], writes=[oraw])
                    k.pe.op(lambda: nc.tensor.matmul(ps_l[:, :], lhsT=ones_f[:, :], rhs=paccs[0][:, :], start=True,
                                                     stop=False), reads=[ones_f, paccs[0]], writes=[ps_l], inc=False)
                    k.pe.op(lambda: nc.tensor.matmul(ps_l[:, :], lhsT=ones_f[:, :], rhs=paccs[1][:, :], start=False,
                                                     stop=True), reads=[ones_f, paccs[1]], writes=[ps_l], inc=True)
                    k.dve.op(lambda: nc.vector.reciprocal(out=rcp[:, :], in_=ps_l[:, :]), reads=[ps_l], writes=[rcp])
                    o = oT[i % 2]
                    k.dve.op(lambda o=o: nc.vector.tensor_tensor(out=o[:, :], in0=oraw[:, :], in1=rcp[:, :],
                                                                 op=ALU.mult), reads=[oraw, rcp], writes=[o])
                    k.sp.dma(dr["yT"][:, heads[i], t0:t0 + TB], o[:, :], o, reads=[o], writes=[dr["yT_buf"]])
            drain(gnext)
```

```python
import contextlib
import numpy as np
import concourse.bass as bass
import concourse.mybir as mybir
from concourse.bass_utils import run_bass_kernel_spmd

F32 = mybir.dt.float32
BF16 = mybir.dt.bfloat16
AF = mybir.ActivationFunctionType
ALU = mybir.AluOpType

D = 1024
DEPTH = 2
MLA_H, NOPE, ROPE, DV = 4, 128, 64, 128
QLORA, KVLORA = 384, 256
GLA_H, GDK, GDV, GRANK = 4, 32, 64, 16
RW_H, RW_N = 4, 64
DFF = 2816
NFT = DFF // 128
EPS = 1e-6
RW_EPS = 64e-5
TB = 512
REPORT_SBUF = False
NO_SELF_WAIT = False


class Buf:
    def __init__(self, ap=None, name=""):
        self.ap = ap
        self.name = name
        self.w = None
        self.r = {}
        self.d = None

    def __getitem__(self, idx):
        return self.ap[idx]


class Eng:
    def __init__(self, k, name, eng, sem):
        self.k, self.name, self.eng, self.sem = k, name, eng, sem
        self.count = 0
        self.known = {}
        self.pending = False

    def wait(self, tok):
        if tok is None:
            return
        sem, val = tok
        if sem is self.sem and (self.name == "pe" or val > self.count or NO_SELF_WAIT):
            return
        if self.known.get(sem, 0) >= val:
            return
        self.eng.wait_ge(sem, val)
        self.known[sem] = val

    def deps(self, reads, writes):
        for b in reads:
            self.wait(b.w)
        for b in writes:
            self.wait(b.w)
            for s, v in list(b.r.items()):
                self.wait((s, v))

    def mark(self, tok, reads, writes):
        s, v = tok
        for b in reads:
            if b.r.get(s, 0) < v:
                b.r[s] = v
        for b in writes:
            b.w = tok
            b.r = {}

    def op(self, fn, reads=(), writes=(), inc=True):
        self.deps(reads, writes)
        inst = fn()
        tok = (self.sem, self.count + 1)
        if inc:
            inst.then_inc(self.sem, 1)
            self.count += 1
            self.pending = False
        else:
            self.pending = True
        self.mark(tok, reads, writes)
        return inst

    def dma(self, out, in_, sb, reads=(), writes=()):
        self.deps(reads, writes)
        if sb.d is None:
            sb.d = {}
        if self.name not in sb.d:
            sb.d[self.name] = self.k.get_dsem(self.name)
            self.k.phase_dma.append((sb, self.name))
        e = sb.d[self.name]
        e[1] += 16
        self.eng.dma_start(out=out, in_=in_).then_inc(e[0], 16)
        self.mark((e[0], e[1]), reads, writes)


class K:
    def __init__(self, nc, es):
        self.nc, self.es = nc, es
        self.nsem = 0
        self.pe = Eng(self, "pe", nc.tensor, self.new_sem("pe"))
        self.act = Eng(self, "act", nc.scalar, self.new_sem("act"))
        self.dve = Eng(self, "dve", nc.vector, self.new_sem("dve"))
        self.pool = Eng(self, "pool", nc.gpsimd, self.new_sem("pool"))
        self.sp = Eng(self, "sp", nc.sync, self.new_sem("sp"))
        self.engs = [self.pe, self.act, self.dve, self.pool, self.sp]
        self.dsem_free = {"sp": [], "pool": []}
        self.phase_dma = []

    def get_dsem(self, q):
        if self.dsem_free[q]:
            return self.dsem_free[q].pop()
        return [self.new_sem("dma" + q), 0]

    @contextlib.contextmanager
    def phase(self):
        with contextlib.ExitStack() as es:
            yield es
            if REPORT_SBUF:
                print("sbuf remaining at phase end:", self.nc.sbuf_bytes_remaining)
            self.barrier()
            for b, q in self.phase_dma:
                self.dsem_free[q].append(b.d.pop(q))
            self.phase_dma = []

    def new_sem(self, name):
        self.nsem += 1
        return self.es.enter_context(self.nc.semaphore(f"{name}_{self.nsem}"))

    def sb(self, es, name, shape, dtype):
        self.nsem += 1
        t = es.enter_context(self.nc.sbuf_tensor(f"{name}_{self.nsem}", list(shape), dtype))
        return Buf(t, name)

    def ps(self, es, name, shape, dtype=F32):
        self.nsem += 1
        t = es.enter_context(self.nc.psum_tensor(f"{name}_{self.nsem}", list(shape), dtype))
        return Buf(t, name)

    def barrier(self):
        toks = []
        for e in self.engs:
            assert not e.pending, e.name
            if e.count:
                toks.append((e.sem, e.count))
        for b, q in self.phase_dma:
            toks.append((b.d[q][0], b.d[q][1]))
        for e in self.engs:
            for t in toks:
                e.wait(t)


def mm(k, ps_buf, ps_ap, pairs, reads):
    n = len(pairs)
    for i, (l, r) in enumerate(pairs):
        k.pe.op(lambda l=l, r=r, i=i: k.nc.tensor.matmul(ps_ap, lhsT=l, rhs=r, start=(i == 0),
                                                         stop=(i == n - 1)),
                reads=reads, writes=[ps_buf], inc=(i == n - 1))


def load_cast(k, wbuf, dst_ap, src_ap, dram_reads=()):
    n = src_ap.shape[-1]
    for c0 in range(0, n, 2048):
        c1 = min(n, c0 + 2048)
        k.pool.dma(dst_ap[:, c0:c1], src_ap[:, c0:c1], wbuf, reads=list(dram_reads), writes=[wbuf])


def load_cast3(k, wbuf, dst3, src2, c0, c1):
    src3 = src2.rearrange("(c p) n -> p c n", p=128)
    n = c1 - c0
    for o in range(0, n, 2048):
        e = min(n, o + 2048)
        k.pool.dma(dst3[:, :, o:e], src3[:, :, c0 + o:c0 + e], wbuf, writes=[wbuf])


class Norm:
    def __init__(self, k, es, name, n, ones_bf, ps=None):
        self.k, self.n, self.ones = k, n, ones_bf
        self.sq = [k.sb(es, f"{name}_sq{i}", [128, n], BF16) for i in range(2)]
        self.ps = k.ps(es, f"{name}_ps", [128, n]) if ps is None else ps
        self.sd = k.sb(es, f"{name}_sd", [128, n], F32)
        self.rstd = self.sd
        self.i = 0

    def stats(self, srcs, src_bufs, dim, eps, ones_ap=None, np_=128):
        k, n = self.k, self.n
        ones_ap = self.ones[0:np_, 0:np_] if ones_ap is None else ones_ap
        nchunk = len(srcs)
        for c, (s, sbuf) in enumerate(zip(srcs, src_bufs)):
            sq = self.sq[self.i % 2]
            self.i += 1
            k.act.op(lambda s=s, sq=sq: k.nc.scalar.activation(out=sq[0:np_, :], in_=s, func=AF.Square),
                     reads=[sbuf], writes=[sq])
            k.pe.op(lambda sq=sq, c=c: k.nc.tensor.matmul(self.ps[0:np_, :], lhsT=ones_ap, rhs=sq[0:np_, :],
                                                          start=(c == 0), stop=(c == nchunk - 1)),
                    reads=[sq, self.ones], writes=[self.ps], inc=True)
        k.act.op(lambda: k.nc.scalar.activation(out=self.sd[0:np_, :], in_=self.ps[0:np_, :], func=AF.Ln,
                                                scale=1.0 / dim, bias=float(eps)),
                 reads=[self.ps], writes=[self.sd])
        k.act.op(lambda: k.nc.scalar.activation(out=self.sd[0:np_, :], in_=self.sd[0:np_, :], func=AF.Exp,
                                                scale=-0.5),
                 reads=[self.sd], writes=[self.sd])
        return self.rstd


VEC_LAYOUT = [
    ("ln1_g", 8), ("ln2_g", 8), ("final_g", 8), ("qn_g", 3), ("kvn_g", 2), ("on_g", 4),
    ("cw0", 44), ("cw1", 44), ("cw2", 44), ("cb", 44),
    ("gla_nb", 1), ("gla_ng", 1),
    ("rw_mu", 8), ("rw_1mmu", 8), ("rw_w0", 2), ("rw_a0", 2), ("rw_kk", 2), ("rw_ka", 2), ("rw_rk", 2),
    ("rw_lng", 2), ("rw_lnb", 2),
]
VOFF = {}
_o = 0
for _n, _c in VEC_LAYOUT:
    VOFF[_n] = (_o, _c)
    _o += _c
NV = _o


def _cols(v, n):
    return np.ascontiguousarray(np.asarray(v, np.float32).reshape(n, 128).T)


def phase_ffn(k, T, l, dr, cst, last):
    nc = k.nc
    nblk = T // TB
    with k.phase() as es:
        vec = k.sb(es, "f_vec", [128, NV], F32)
        k.sp.dma(vec[:, :], dr["vecs"][l], vec, writes=[vec])
        vnext = k.sb(es, "f_vnext", [128, 8], F32)
        if not last:
            o1 = VOFF["ln1_g"][0]
            k.sp.dma(vnext[:, :], dr["vecs"][l + 1][:, o1:o1 + 8], vnext, writes=[vnext])
        ones = k.sb(es, "f_ones", [128, 128], BF16)
        load_cast(k, ones, ones[:, :], cst["ones"])
        wup = k.sb(es, "f_wup", [128, 8, 2 * DFF], BF16)
        load_cast3(k, wup, wup[:, :, :], dr["ffn_w_up"][l], 0, 2 * DFF)
        wdn = k.sb(es, "f_wdn", [128, NFT, D], BF16)
        load_cast3(k, wdn, wdn[:, :, :], dr["ffn_w_down"][l], 0, D)
        xbs = [k.sb(es, f"f_xb{i}", [128, 8, TB], F32) for i in range(2)]
        hT = k.sb(es, "f_hT", [128, 8, TB], BF16)
        gT = [k.sb(es, f"f_gT{j}", [128, TB], BF16) for j in range(NFT)]
        halo = k.sb(es, "f_halo", [128, 2 * NFT, 2], F32)
        k.pool.op(lambda: nc.gpsimd.memset(halo[:, :, :], 0.0), writes=[halo])
        ug = [k.sb(es, f"f_ug{i}", [128, TB + 2], F32) for i in range(2)]
        ugh = [Buf(ug[i].ap[:, 0:2], f"ugh{i}") for i in range(2)]
        ugm = [Buf(ug[i].ap[:, 2:TB + 2], f"ugm{i}") for i in range(2)]
        a1 = [k.sb(es, f"f_a1{i}", [128, TB], F32) for i in range(2)]
        psu = [k.ps(es, f"f_psu{i}", [128, TB]) for i in range(4)]
        psd = [k.ps(es, f"f_psd{i}", [128, TB]) for i in range(2)]
        nrm = Norm(k, es, "f_n", TB, ones)
        o_g = VOFF["ln2_g"][0]
        o_w0, o_w1, o_w2, o_cb = (VOFF[n][0] for n in ("cw0", "cw1", "cw2", "cb"))
        o_fg = VOFF["final_g"][0]
        xT = dr["xT"]
        xTb = dr["xT_buf"]
        it = 0
        def norm_in(xb):
            rstd = nrm.stats([xb[:, c, :] for c in range(8)], [xb] * 8, D, EPS)
            for c in range(8):
                k.dve.op(lambda c=c, xb=xb: nc.vector.scalar_tensor_tensor(
                    out=hT[:, c, :], in0=xb[:, c, :], scalar=vec[:, o_g + c:o_g + c + 1], in1=rstd[:, :],
                    op0=ALU.mult, op1=ALU.mult), reads=[xb, vec, rstd], writes=[hT])

        k.sp.dma(xbs[0][:, :, :], xT[:, :, 0:TB], xbs[0], reads=[xTb], writes=[xbs[0]])
        norm_in(xbs[0])
        for b in range(nblk):
            t0 = b * TB
            xb = xbs[b % 2]
            if b + 1 < nblk:
                xn = xbs[(b + 1) % 2]
                k.sp.dma(xn[:, :, :], xT[:, :, t0 + TB:t0 + 2 * TB], xn, reads=[xTb], writes=[xn])
            for j in range(NFT):
                res = []
                for half in range(2):
                    jt = j + half * NFT
                    u = half
                    pu = psu[it % 4]
                    it += 1
                    col0 = half * DFF + j * 128
                    mm(k, pu, pu[:, :],
                       [(wup[:, c, col0:col0 + 128], hT[:, c, :]) for c in range(8)], [wup, hT])
                    k.pool.op(lambda u=u, jt=jt: nc.gpsimd.tensor_copy(out=ug[u][:, 0:2], in_=halo[:, jt, :]),
                              reads=[halo], writes=[ugh[u]])
                    k.act.op(lambda u=u, pu=pu: nc.scalar.activation(out=ug[u][:, 2:TB + 2], in_=pu[:, :],
                                                                     func=AF.Copy), reads=[pu], writes=[ugm[u]])
                    k.act.op(lambda u=u, jt=jt, pu=pu: nc.scalar.activation(
                        out=a1[u][:, :], in_=pu[:, :], func=AF.Identity,
                        scale=vec[:, o_w2 + jt:o_w2 + jt + 1], bias=vec[:, o_cb + jt:o_cb + jt + 1]),
                        reads=[pu, vec], writes=[a1[u]])
                    k.pool.op(lambda u=u, jt=jt: nc.gpsimd.tensor_copy(out=halo[:, jt, :], in_=ug[u][:, TB:TB + 2]),
                              reads=[ugm[u]], writes=[halo])
                    k.dve.op(lambda u=u, jt=jt: nc.vector.scalar_tensor_tensor(
                        out=a1[u][:, :], in0=ug[u][:, 1:TB + 1], scalar=vec[:, o_w1 + jt:o_w1 + jt + 1],
                        in1=a1[u][:, :], op0=ALU.mult, op1=ALU.add), reads=[ugm[u], ugh[u], vec, a1[u]], writes=[a1[u]])
                    k.dve.op(lambda u=u, jt=jt: nc.vector.scalar_tensor_tensor(
                        out=a1[u][:, :], in0=ug[u][:, 0:TB], scalar=vec[:, o_w0 + jt:o_w0 + jt + 1],
                        in1=a1[u][:, :], op0=ALU.mult, op1=ALU.add), reads=[ugm[u], ugh[u], vec, a1[u]], writes=[a1[u]])
                    res.append(a1[u])
                s = res[0]
                k.act.op(lambda s=s: nc.scalar.activation(out=s[:, :], in_=s[:, :], func=AF.Silu),
                         reads=[s], writes=[s])
                k.pool.op(lambda s=s, v=res[1], j=j: nc.gpsimd.tensor_tensor(
                    out=gT[j][:, :], in0=s[:, :], in1=v[:, :], op=ALU.mult), reads=[s, res[1]], writes=[gT[j]])
            if b + 1 < nblk:
                norm_in(xbs[(b + 1) % 2])
            for m in range(8):
                pd = psd[m % 2]
                mm(k, pd, pd[:, :], [(wdn[:, j, m * 128:(m + 1) * 128], gT[j][:, :]) for j in range(NFT)],
                   [wdn] + gT)
                k.dve.op(lambda m=m, pd=pd, xb=xb: nc.vector.tensor_tensor(out=xb[:, m, :], in0=pd[:, :],
                                                                           in1=xb[:, m, :], op=ALU.add),
                         reads=[pd, xb], writes=[xb])
            if not last:
                k.sp.dma(xT[:, :, t0:t0 + TB], xb[:, :, :], xb, reads=[xb], writes=[xTb])
                rstd = nrm.stats([xb[:, c, :] for c in range(8)], [xb] * 8, D, EPS)
                for c in range(8):
                    hn = gT[NFT - 8 + c]
                    k.dve.op(lambda c=c, hn=hn, xb=xb: nc.vector.scalar_tensor_tensor(
                        out=hn[:, :], in0=xb[:, c, :], scalar=vnext[:, c:c + 1], in1=rstd[:, :],
                        op0=ALU.mult, op1=ALU.mult), reads=[xb, vnext, rstd], writes=[hn])
                    k.sp.dma(dr["hT"][:, c, t0:t0 + TB], hn[:, :], hn, reads=[hn], writes=[dr["hT_buf"]])
            else:
                rstd = nrm.stats([xb[:, c, :] for c in range(8)], [xb] * 8, D, EPS)
                for c in range(8):
                    k.dve.op(lambda c=c, xb=xb: nc.vector.scalar_tensor_tensor(
                        out=xb[:, c, :], in0=xb[:, c, :], scalar=vec[:, o_fg + c:o_fg + c + 1], in1=rstd[:, :],
                        op0=ALU.mult, op1=ALU.mult), reads=[xb, vec, rstd], writes=[xb])
                k.sp.dma(dr["outT"][:, :, t0:t0 + TB], xb[:, :, :], xb, reads=[xb], writes=[dr["outT_buf"]])


MISC_LAYOUT = [("ones", 128), ("ones2", 128), ("ident", 128), ("cmask", 512), ("mask4", 512), ("bd", 256),
               ("hm", 4), ("smask", 128), ("imaskT", 128)]
MOFF = {}
_o = 0
for _n, _c in MISC_LAYOUT:
    MOFF[_n] = (_o, _o + _c)
    _o += _c
NMISC = _o


def make_misc():
    m = np.zeros((128, NMISC), np.float32)
    p = np.arange(128)[:, None]
    f = lambda n: np.arange(n)[None, :]
    m[:, slice(*MOFF["ones"])] = 1.0
    m[:, slice(*MOFF["ones2"])] = (p // 64 == f(128) // 64)
    m[:, slice(*MOFF["ident"])] = (p == f(128))
    m[:, slice(*MOFF["cmask"])] = (f(512) % 128 != 0)
    m[:, slice(*MOFF["mask4"])] = (p <= f(512) % 128)
    m[:, slice(*MOFF["bd"])] = (p // 32 == f(256) // 64)
    m[:, slice(*MOFF["hm"])] = (p // 32 == f(4))
    m[:, slice(*MOFF["smask"])] = (p < f(128))
    m[:, slice(*MOFF["imaskT"])] = (p > f(128))
    return m


def load_misc(k, es, name, cst, names, dtype):
    out = {}
    for n in names:
        a, b = MOFF[n]
        t = k.sb(es, f"{name}_{n}", [128, b - a], dtype)
        if dtype == BF16:
            load_cast(k, t, t[:, :], cst["misc"][:, a:b])
        else:
            k.sp.dma(t[:, :], cst["misc"][:, a:b], t, writes=[t])
        out[n] = t
    return out


def phase_gla(k, T, l, dr, cst):
    nc = k.nc
    C = 128
    NCH = TB // C
    w_in = dr["w_in"]
    G0 = 704
    with k.phase() as es:
        vec = k.sb(es, "g_vec", [128, NV], F32)
        k.sp.dma(vec[:, :], dr["vecs"][l], vec, writes=[vec])
        cb = load_misc(k, es, "g", cst, ["ones2", "ident", "mask4", "bd"], BF16)
        cf = load_misc(k, es, "gf", cst, ["cmask", "hm"], F32)
        ones2, ident, mask4, bd, cmask, hm = cb["ones2"], cb["ident"], cb["mask4"], cb["bd"], cf["cmask"], cf["hm"]
        wG = k.sb(es, "g_wG", [128, 8, 784], BF16)
        load_cast3(k, wG, wG[:, :, 0:256], w_in[l], G0, G0 + 256)
        load_cast3(k, wG, wG[:, :, 256:512], w_in[l], G0 + 528, G0 + 784)
        load_cast3(k, wG, wG[:, :, 512:768], w_in[l], G0 + 256, G0 + 512)
        load_cast3(k, wG, wG[:, :, 768:784], w_in[l], G0 + 512, G0 + 528)
        wgk = k.sb(es, "g_wgk", [16, 128], BF16)
        load_cast(k, wgk, wgk[:, :], dr["gla_w_gk"][l])
        nb = k.sb(es, "g_nb", [128, 1], F32)
        o_nb, o_ng = VOFF["gla_nb"][0], VOFF["gla_ng"][0]
        k.dve.op(lambda: nc.vector.tensor_scalar(out=nb[:, :], in0=vec[:, o_nb:o_nb + 1], scalar1=-1.0, scalar2=None,
                                                 op0=ALU.mult), reads=[vec], writes=[nb])
        hT = [k.sb(es, f"g_hT{i}", [128, 8, TB], BF16) for i in range(2)]
        q32 = k.sb(es, "g_q32", [128, TB], F32)
        k32 = k.sb(es, "g_k32", [128, TB], F32)
        glo = k.sb(es, "g_glo", [16, TB], BF16)
        Lt = k.sb(es, "g_L", [128, TB], F32)
        Bc = k.sb(es, "g_Bc", [128, TB], F32)
        eb = k.sb(es, "g_eb", [128, TB], F32)
        enb = k.sb(es, "g_enb", [128, TB], F32)
        kef = k.sb(es, "g_kef", [128, TB], F32)
        nbl = k.sb(es, "g_nbl", [128, NCH], F32)
        dec = k.sb(es, "g_dec", [128, NCH], F32)
        qth = [k.sb(es, f"g_qth{i}", [128, TB], BF16) for i in range(4)]
        kt = k.sb(es, "g_kt", [128, TB], BF16)
        kend = k.sb(es, "g_kend", [128, TB], BF16)
        kendT = k.sb(es, "g_kendT", [128, NCH, 128], BF16)
        vT = k.sb(es, "g_vT", [128, NCH, 256], BF16)
        sgo = [k.sb(es, f"g_sgo{i}", [128, TB], F32) for i in range(2)]
        attnT = k.sb(es, "g_attnT", [128, 4 * C], BF16)
        S4 = k.sb(es, "g_S4", [128, 256], F32)
        Sb = k.sb(es, "g_Sb", [128, 256], BF16)
        kvm = k.sb(es, "g_kvm", [128, 256], F32)
        oacc = [k.sb(es, f"g_oacc{i}", [128, TB], F32) for i in range(2)]
        yo = [k.sb(es, f"g_yo{i}", [128, TB], BF16) for i in range(2)]
        tmp = k.sb(es, "g_tmp", [128, TB], F32)
        tmp2 = k.sb(es, "g_tmp2", [128, TB], F32)
        sets = [(qth, kt, kendT, vT, dec, sgo),
                ([k.sb(es, f"g_qthB{i}", [128, TB], BF16) for i in range(4)], k.sb(es, "g_ktB", [128, TB], BF16),
                 k.sb(es, "g_kendTB", [128, NCH, 128], BF16), k.sb(es, "g_vTB", [128, NCH, 256], BF16),
                 k.sb(es, "g_decB", [128, NCH], F32), [k.sb(es, f"g_sgoB{i}", [128, TB], F32) for i in range(2)])]
        k.pool.op(lambda: nc.gpsimd.memset(S4[:, :], 0.0), writes=[S4])
        k.pool.op(lambda: nc.gpsimd.memset(Sb[:, :], 0.0), writes=[Sb])
        nrm = Norm(k, es, "g_n", TB, ones2)
        ps_p = [k.ps(es, f"g_psp{i}", [128, TB]) for i in range(2)]
        ps_at = k.ps(es, "g_psat", [128, 4 * C])
        ps_o = [k.ps(es, f"g_pso{i}", [128, C]) for i in range(2)]
        ps_kv = k.ps(es, "g_pskv", [128, 256])
        ps_tr = k.ps(es, "g_pstr", [128, 128])
        ip = 0

        def proj(pairs, reads, m=128, n=TB):
            nonlocal ip
            p = ps_p[ip % 2]
            ip += 1
            mm(k, p, p[0:m, 0:n], pairs, reads)
            return p

        qs = float(GDK ** -0.5)
        def prepG(b):
            t0 = b * TB
            h = hT[b % 2]
            qth, kt, kendT, vT, dec, sgo = sets[b % 2]
            k.sp.dma(h[:, :, :], dr["hT"][:, :, t0:t0 + TB], h, reads=[dr["hT_buf"]], writes=[h])
            yield
            p = proj([(wG[:, c, 0:128], h[:, c, :]) for c in range(8)], [wG, h])
            k.act.op(lambda p=p: nc.scalar.activation(out=q32[:, :], in_=p[:, :], func=AF.Copy), reads=[p], writes=[q32])
            yield
            p = proj([(wG[:, c, 128:256], h[:, c, :]) for c in range(8)], [wG, h])
            k.act.op(lambda p=p: nc.scalar.activation(out=k32[:, :], in_=p[:, :], func=AF.Copy), reads=[p], writes=[k32])
            yield
            p = proj([(wG[:, c, 768:784], h[:, c, :]) for c in range(8)], [wG, h], m=16)
            k.act.op(lambda p=p: nc.scalar.activation(out=glo[:, :], in_=p[0:16, :], func=AF.Copy), reads=[p], writes=[glo])
            yield
            for i in range(2):
                p = proj([(wG[:, c, 256 + i * 128:384 + i * 128], h[:, c, :]) for c in range(8)], [wG, h])
                k.act.op(lambda p=p, i=i: nc.scalar.activation(out=sgo[i][:, :], in_=p[:, :], func=AF.Silu),
                         reads=[p], writes=[sgo[i]])
                yield
            for c in range(NCH):
                p = proj([(h[:, c8, c * C:(c + 1) * C], wG[:, c8, 512:768]) for c8 in range(8)], [wG, h], n=256)
                k.act.op(lambda p=p, c=c: nc.scalar.activation(out=vT[:, c, :], in_=p[:, 0:256], func=AF.Copy),
                         reads=[p], writes=[vT])
                yield
            p = proj([(wgk[:, :], glo[:, :])], [wgk, glo])
            k.act.op(lambda p=p: nc.scalar.activation(out=Lt[:, :], in_=p[:, :], func=AF.Exp, scale=-1.0, bias=nb[:, 0:1]),
                     reads=[p, nb], writes=[Lt])
            k.act.op(lambda: nc.scalar.activation(out=Lt[:, :], in_=Lt[:, :], func=AF.Ln, bias=1.0), reads=[Lt], writes=[Lt])
            yield
            k.dve.op(lambda: nc.vector.tensor_tensor_scan(out=Bc[:, :], data0=cmask[:, :], data1=Lt[:, :], initial=0.0,
                                                          op0=ALU.mult, op1=ALU.add), reads=[cmask, Lt], writes=[Bc])
            yield
            k.act.op(lambda: nc.scalar.activation(out=eb[:, :], in_=Bc[:, :], func=AF.Exp, scale=-1.0 / 16), reads=[Bc], writes=[eb])
            k.act.op(lambda: nc.scalar.activation(out=enb[:, :], in_=Bc[:, :], func=AF.Exp, scale=1.0 / 16), reads=[Bc], writes=[enb])
            yield
            k.dve.op(lambda: nc.vector.tensor_scalar(out=nbl[:, :], in0=Bc[:, C - 1::C], scalar1=-1.0 / 16, scalar2=None,
                                                     op0=ALU.mult), reads=[Bc], writes=[nbl])
            k.act.op(lambda: nc.scalar.activation(out=dec[:, :], in_=nbl[:, :], func=AF.Exp), reads=[nbl], writes=[dec])
            yield
            for c in range(NCH):
                k.act.op(lambda c=c: nc.scalar.activation(out=kef[:, c * C:(c + 1) * C], in_=Bc[:, c * C:(c + 1) * C],
                                                          func=AF.Exp, scale=1.0 / 16, bias=nbl[:, c:c + 1]),
                         reads=[Bc, nbl], writes=[kef])
                yield
            k.dve.op(lambda: nc.vector.tensor_tensor(out=tmp[:, :], in0=q32[:, :], in1=eb[:, :], op=ALU.mult),
                     reads=[q32, eb], writes=[tmp])
            for hh in range(4):
                k.dve.op(lambda hh=hh: nc.vector.tensor_scalar(out=qth[hh][:, :], in0=tmp[:, :], scalar1=hm[:, hh:hh + 1],
                                                               scalar2=qs, op0=ALU.mult, op1=ALU.mult),
                         reads=[tmp, hm], writes=[qth[hh]])
                yield
            k.dve.op(lambda: nc.vector.tensor_tensor(out=kt[:, :], in0=k32[:, :], in1=enb[:, :], op=ALU.mult),
                     reads=[k32, enb], writes=[kt])
            yield
            k.dve.op(lambda: nc.vector.tensor_tensor(out=kend[:, :], in0=k32[:, :], in1=kef[:, :], op=ALU.mult),
                     reads=[k32, kef], writes=[kend])
            yield
            for c in range(NCH):
                cs = slice(c * C, (c + 1) * C)
                mm(k, ps_tr, ps_tr[:, :], [(kend[:, cs], ident[:, :])], [kend, ident])
                k.act.op(lambda c=c: nc.scalar.activation(out=kendT[:, c, :], in_=ps_tr[:, :], func=AF.Copy),
                         reads=[ps_tr], writes=[kendT])
                yield
        def drain(g):
            if g is not None:
                for _ in g:
                    pass

        def step():
            nonlocal gp
            if gp is not None:
                try:
                    next(gp)
                except StopIteration:
                    gp = None

        gp = None
        drain(prepG(0))
        for b in range(T // TB):
            t0 = b * TB
            qth, kt, kendT, vT, dec, sgo = sets[b % 2]
            gp = prepG(b + 1) if (b + 1) * TB < T else None
            for c in range(NCH):
                cs = slice(c * C, (c + 1) * C)
                step()
                for hh in range(4):
                    k.pe.op(lambda hh=hh, cs=cs: nc.tensor.matmul(ps_at[:, hh * C:(hh + 1) * C], lhsT=kt[:, cs],
                                                                  rhs=qth[hh][:, cs], start=True, stop=True),
                            reads=[kt, qth[hh]], writes=[ps_at], inc=(hh == 3))
                k.dve.op(lambda: nc.vector.tensor_tensor(out=attnT[:, :], in0=ps_at[:, :], in1=mask4[:, :], op=ALU.mult),
                         reads=[ps_at, mask4], writes=[attnT])
                step()
                for pr in range(2):
                    po = ps_o[pr]
                    for hl in range(2):
                        hh = pr * 2 + hl
                        osl = po[hl * 64:(hl + 1) * 64, :]
                        k.pe.op(lambda hh=hh, osl=osl, c=c: nc.tensor.matmul(
                            osl, lhsT=vT[:, c, hh * 64:(hh + 1) * 64], rhs=attnT[:, hh * C:(hh + 1) * C],
                            start=True, stop=False), reads=[vT, attnT], writes=[po], inc=False)
                        k.pe.op(lambda hh=hh, osl=osl, cs=cs: nc.tensor.matmul(
                            osl, lhsT=Sb[:, hh * 64:(hh + 1) * 64], rhs=qth[hh][:, cs], start=False, stop=True),
                            reads=[Sb, qth[hh]], writes=[po], inc=True)
                    k.act.op(lambda pr=pr, po=po, cs=cs: nc.scalar.activation(out=oacc[pr][:, cs], in_=po[:, :],
                                                                              func=AF.Copy), reads=[po], writes=[oacc[pr]])
                step()
                mm(k, ps_kv, ps_kv[:, :], [(kendT[:, c, :], vT[:, c, :])], [kendT, vT])
                k.dve.op(lambda: nc.vector.tensor_tensor(out=kvm[:, :], in0=ps_kv[:, :], in1=bd[:, :], op=ALU.mult),
                         reads=[ps_kv, bd], writes=[kvm])
                k.dve.op(lambda c=c: nc.vector.scalar_tensor_tensor(out=S4[:, :], in0=S4[:, :], scalar=dec[:, c:c + 1],
                                                                    in1=kvm[:, :], op0=ALU.mult, op1=ALU.add),
                         reads=[S4, dec, kvm], writes=[S4])
                k.act.op(lambda: nc.scalar.activation(out=Sb[:, :], in_=S4[:, :], func=AF.Copy), reads=[S4], writes=[Sb])
                step()
                step()
            for pr in range(2):
                rstd = nrm.stats([oacc[pr][:, :]], [oacc[pr]], GDV, EPS)
                k.dve.op(lambda pr=pr: nc.vector.tensor_tensor(out=tmp2[:, :], in0=oacc[pr][:, :], in1=rstd[:, :],
                                                               op=ALU.mult), reads=[oacc[pr], rstd], writes=[tmp2])
                k.dve.op(lambda pr=pr: nc.vector.scalar_tensor_tensor(
                    out=yo[pr][:, :], in0=tmp2[:, :], scalar=vec[:, o_ng:o_ng + 1], in1=sgo[pr][:, :],
                    op0=ALU.mult, op1=ALU.mult), reads=[tmp2, vec, sgo[pr]], writes=[yo[pr]])
                k.sp.dma(dr["yT"][:, 4 + pr, t0:t0 + TB], yo[pr][:, :], yo[pr], reads=[yo[pr]], writes=[dr["yT_buf"]])
            drain(gp)
            gp = None


def TT(e, out, a, b, op, R, W):
    e.op(lambda: e.eng.tensor_tensor(out=out, in0=a, in1=b, op=op), reads=R, writes=W)


def TS(e, out, a, s1, s2, op0, op1, R, W):
    e.op(lambda: e.eng.tensor_scalar(out=out, in0=a, scalar1=s1, scalar2=s2, op0=op0, op1=op1), reads=R, writes=W)


def STT(k, out, a, sc, b, op0, op1, R, W):
    k.dve.op(lambda: k.nc.vector.scalar_tensor_tensor(out=out, in0=a, scalar=sc, in1=b, op0=op0, op1=op1),
             reads=R, writes=W)


def ACTF(k, out, in_, func, R, W, scale=1.0, bias=0.0):
    k.act.op(lambda: k.nc.scalar.activation(out=out, in_=in_, func=func, scale=scale, bias=bias), reads=R, writes=W)


def MM(k, out, lhsT, rhs, R, W, start=True, stop=True, inc=True):
    k.pe.op(lambda: k.nc.tensor.matmul(out, lhsT=lhsT, rhs=rhs, start=start, stop=stop), reads=R, writes=W, inc=inc)


class _Stop(Exception):
    pass


STOP = [None]


def chk(n):
    if STOP[0] == n:
        raise _Stop()


def phase_rwkv(k, T, l, dr, cst):
    try:
        _phase_rwkv(k, T, l, dr, cst)
    except _Stop:
        pass


NS = 3


def _phase_rwkv(k, T, l, dr, cst):
    nc = k.nc
    C = 128
    NCH = TB // C
    R0 = 704 + 784
    DEC = float(np.exp(-0.5))
    with k.phase() as es:
        vec = k.sb(es, "r_vec", [128, NV], F32)
        k.sp.dma(vec[:, :], dr["vecs"][l], vec, writes=[vec])
        cb = load_misc(k, es, "r", cst, ["ones2", "ident"], BF16)
        cf = load_misc(k, es, "rf", cst, ["cmask", "smask", "mask4", "imaskT", "ident"], F32)
        ones2, ident, cmask = cb["ones2"], cb["ident"], cf["cmask"]
        m_si = k.sb(es, "r_msi", [128, 2, 256], BF16)
        m_tt = k.sb(es, "r_mtt", [128, 2, 128], BF16)
        for hl in range(2):
            k.pool.op(lambda hl=hl: nc.gpsimd.tensor_copy(out=m_si[:, hl, 0:128], in_=cf["smask"][:, :]),
                      reads=[cf["smask"]], writes=[m_si])
            k.pool.op(lambda hl=hl: nc.gpsimd.tensor_copy(out=m_si[:, hl, 128:256], in_=cf["mask4"][:, 0:128]),
                      reads=[cf["mask4"]], writes=[m_si])
            k.pool.op(lambda hl=hl: nc.gpsimd.tensor_copy(out=m_tt[:, hl, :], in_=cf["imaskT"][:, :]),
                      reads=[cf["imaskT"]], writes=[m_tt])
        I2 = k.sb(es, "r_I2", [128, 64], F32)
        k.pool.op(lambda: nc.gpsimd.tensor_copy(out=I2[0:64, :], in_=cf["ident"][0:64, 0:64]), reads=[cf["ident"]], writes=[I2])
        k.pool.op(lambda: nc.gpsimd.tensor_copy(out=I2[64:128, :], in_=cf["ident"][64:128, 64:128]), reads=[cf["ident"]], writes=[I2])
        wR = k.sb(es, "r_wR", [128, 8, 1024], BF16)
        load_cast3(k, wR, wR[:, :, :], dr["w_in"][l], R0, R0 + 1024)
        wl_w = k.sb(es, "r_wlw", [128, 256], BF16)
        wl_a = k.sb(es, "r_wla", [128, 256], BF16)
        k.pool.op(lambda: nc.gpsimd.memset(wl_w[:, :], 0.0), writes=[wl_w])
        k.pool.op(lambda: nc.gpsimd.memset(wl_a[:, :], 0.0), writes=[wl_a])
        load_cast(k, wl_w, wl_w[0:64, :], dr["rwkv_w2"][l])
        load_cast(k, wl_a, wl_a[64:128, :], dr["rwkv_a2"][l])
        hm2 = k.sb(es, "r_hm2", [128, 2], F32)
        nhm2 = k.sb(es, "r_nhm2", [128, 2], F32)
        I2h = [k.sb(es, f"r_I2h{i}", [128, 64], F32) for i in range(2)]
        for i in range(2):
            k.pool.op(lambda i=i: nc.gpsimd.memset(I2h[i][:, :], 0.0), writes=[I2h[i]])
            k.pool.op(lambda i=i: nc.gpsimd.tensor_copy(out=I2h[i][i * 64:(i + 1) * 64, :],
                                                        in_=cf["ident"][i * 64:(i + 1) * 64, i * 64:(i + 1) * 64]),
                      reads=[cf["ident"]], writes=[I2h[i]])
        k.pool.op(lambda: nc.gpsimd.memset(hm2[:, :], 0.0), writes=[hm2])
        k.pool.op(lambda: nc.gpsimd.memset(hm2[0:64, 0:1], 1.0), writes=[hm2])
        k.pool.op(lambda: nc.gpsimd.memset(hm2[64:128, 1:2], 1.0), writes=[hm2])
        TS(k.dve, nhm2[:, :], hm2[:, :], -1.0, None, ALU.mult, ALU.bypass, [hm2], [nhm2])
        wg2 = k.sb(es, "r_wg2", [128, 256], BF16)
        load_cast(k, wg2, wg2[:, :], dr["rwkv_g2"][l])
        o_mu, o_w0, o_a0, o_kk, o_ka, o_rk, o_lg, o_lb = (VOFF[n][0] for n in (
            "rw_mu", "rw_w0", "rw_a0", "rw_kk", "rw_ka", "rw_rk", "rw_lng", "rw_lnb"))
        omm = k.sb(es, "r_omm", [128, 8], F32)
        omka = k.sb(es, "r_omka", [128, 2], F32)
        TS(k.dve, omm[:, :], vec[:, o_mu:o_mu + 8], -1.0, 1.0, ALU.mult, ALU.add, [vec], [omm])
        TS(k.dve, omka[:, :], vec[:, o_ka:o_ka + 2], -1.0, 1.0, ALU.mult, ALU.add, [vec], [omka])

        hTs = [k.sb(es, f"r_hT{i}", [128, 8, TB + 1], BF16) for i in range(2)]
        def f32t(n): return k.sb(es, "r_" + n, [128, TB], F32)
        def bft(n): return k.sb(es, "r_" + n, [128, TB], BF16)
        tprev = f32t("tprev")
        xr, xk, xv = [f32t(f"xr{i}") for i in range(2)], [f32t(f"xk{i}") for i in range(2)], [f32t(f"xv{i}") for i in range(2)]
        xlo = bft("xlo")
        sgl = bft("sgl")
        gate = [f32t(f"gate{i}") for i in range(2)]
        bon = [f32t(f"bon{i}") for i in range(2)]
        aa, kkt, kmod, cc, t1, t2, t3 = f32t("aa"), f32t("kk"), f32t("kmod"), f32t("cc"), f32t("t1"), f32t("t2"), f32t("t3")
        tb1 = bft("tb1")
        ar = [[k.sb(es, f"r_ar{i}{j}", [128, NCH, 256], BF16) for j in range(2)] for i in range(2)]
        Xw_ = [[k.sb(es, f"r_Xw{p}{i}", [128, 128], BF16) for i in range(2)] for p in range(NS)]
        for p_ in range(NS):
            for i in range(2):
                k.pool.op(lambda i=i, p_=p_: nc.gpsimd.memset(Xw_[p_][i][:, :], 0.0), writes=[Xw_[p_][i]])
        bt = [bft(f"bt{i}") for i in range(2)]
        ktl = [bft(f"ktl{i}") for i in range(2)]
        Bp = [bft(f"Bp{i}") for i in range(2)]
        Kp = [bft(f"Kp{i}") for i in range(2)]
        vb = [bft(f"vb{i}") for i in range(2)]
        nbl = k.sb(es, "r_nbl", [128, NCH], F32)
        pC = [k.sb(es, f"r_pC{i}", [128, NCH], F32) for i in range(2)]
        TM_ = [k.sb(es, f"r_TM{p}", [128, 4, 128], BF16) for p in range(NS)]
        AbRb_ = [k.sb(es, f"r_AbRb{p}", [128, 2, 256], BF16) for p in range(NS)]
        AkRk_ = [k.sb(es, f"r_AkRk{p}", [128, 2, 256], BF16) for p in range(NS)]
        PP_ = [[k.sb(es, f"r_PP{p}{i}", [128, 2, 256], BF16) for i in range(2)] for p in range(NS)]
        X_ = [k.sb(es, f"r_X{p}", [128, 2, 128], BF16) for p in range(NS)]
        QpT = [[bft(f"QpT{i}{j}") for j in range(2)] for i in range(2)]
        Y0T = [f32t(f"Y0T{i}") for i in range(2)]
        MT = [[k.sb(es, f"r_MT{i}{j}", [128, NCH, 64], BF16) for j in range(2)] for i in range(2)]
        Gm = [k.sb(es, f"r_G{i}", [128, NCH, 64], F32) for i in range(2)]
        Hb = [k.sb(es, f"r_H{i}", [128, 64], BF16) for i in range(2)]
        yacc = [f32t(f"yacc{i}") for i in range(2)]
        yo = [bft(f"yo{i}") for i in range(2)]
        o1, ob1 = f32t("o1"), bft("ob1")
        sets = [(gate, bon, vb, ar, bt, ktl, pC, Bp, Kp)]
        sets.append(([f32t(f"gateB{i}") for i in range(2)], [f32t(f"bonB{i}") for i in range(2)],
                     [bft(f"vbB{i}") for i in range(2)],
                     [[k.sb(es, f"r_arB{i}{j}", [128, NCH, 256], BF16) for j in range(2)] for i in range(2)],
                     [bft(f"btB{i}") for i in range(2)], [bft(f"ktlB{i}") for i in range(2)],
                     [k.sb(es, f"r_pCB{i}", [128, NCH], F32) for i in range(2)],
                     [bft(f"BpB{i}") for i in range(2)], [bft(f"KpB{i}") for i in range(2)]))
        for i in range(2):
            k.pool.op(lambda i=i: nc.gpsimd.memset(Hb[i][:, :], 0.0), writes=[Hb[i]])
        ps_p = [k.ps(es, f"r_psp{i}", [128, TB]) for i in range(2)]
        nrm = Norm(k, es, "r_n", TB, ones2, ps=ps_p[0])
        ps_x_ = [k.ps(es, f"r_bx{p}", [128, TB]) for p in range(NS)]
        ps_pp_ = [k.ps(es, f"r_pspp{p}", [128, 2, 256]) for p in range(NS)]
        banks = [ps_p[0], ps_p[1]]
        ip = 0
        isx = 0

        def bank():
            nonlocal isx
            isx += 1
            return banks[isx % 2]

        def proj(pairs, reads, n=TB):
            nonlocal ip
            p = ps_p[ip % 2]
            ip += 1
            mm(k, p, p[:, 0:n], pairs, reads)
            return p

        def prepR(b):
            t0 = b * TB
            h = hTs[b % 2]
            gate, bon, vb, ar, bt, ktl, pC, Bp, Kp = sets[b % 2]
            if b == 0:
                k.pool.op(lambda h=h: nc.gpsimd.memset(h[:, :, 0:1], 0.0), writes=[h])
                k.sp.dma(h[:, :, 1:TB + 1], dr["hT"][:, :, 0:TB], h, reads=[dr["hT_buf"]], writes=[h])
            else:
                k.sp.dma(h[:, :, :], dr["hT"][:, :, t0 - 1:t0 + TB], h, reads=[dr["hT_buf"]], writes=[h])

            def mixed(j, out, func=AF.Identity, out_bufs=None):
                cols = slice(j * 128, (j + 1) * 128)
                pp = proj([(wR[:, c, cols], h[:, c, 0:TB]) for c in range(8)], [wR, h])
                ACTF(k, tprev[:, :], pp[:, :], AF.Copy if False else AF.Identity, [pp, vec], [tprev],
                     scale=vec[:, o_mu + j:o_mu + j + 1])
                pc_ = proj([(wR[:, c, cols], h[:, c, 1:TB + 1]) for c in range(8)], [wR, h])
                STT(k, t1[:, :], pc_[:, :], omm[:, j:j + 1], tprev[:, :], ALU.mult, ALU.add, [pc_, omm, tprev], [t1])
                return t1

            yield
            mixed(6, None)
            ACTF(k, xlo[0:64, :], t1[0:64, :], AF.Tanh, [t1], [xlo])
            ACTF(k, xlo[64:128, :], t1[64:128, :], AF.Copy, [t1], [xlo])
            yield
            mixed(7, None)
            ACTF(k, sgl[:, :], t1[:, :], AF.Sigmoid, [t1], [sgl])
            yield
            for j in range(2):
                mixed(j, None)
                ACTF(k, xr[j][:, :], t1[:, :], AF.Copy, [t1], [xr[j]])
                yield
                mixed(2 + j, None)
                ACTF(k, xk[j][:, :], t1[:, :], AF.Copy, [t1], [xk[j]])
                yield
                mixed(4 + j, None)
                ACTF(k, xv[j][:, :], t1[:, :], AF.Copy, [t1], [xv[j]])
                yield
            for pr in range(2):
                pcols = slice(pr * 128, (pr + 1) * 128)
                r_, k_, v_ = xr[pr], xk[pr], xv[pr]
                p = proj([(wg2[:, pcols], sgl[:, :])], [wg2, sgl])
                ACTF(k, gate[pr][:, :], p[:, :], AF.Copy, [p], [gate[pr]])
                yield
                p = proj([(wl_a[:, pcols], xlo[:, :])], [wl_a, xlo])
                ACTF(k, aa[:, :], p[:, :], AF.Sigmoid, [p, vec], [aa], bias=vec[:, o_a0 + pr:o_a0 + pr + 1])
                yield
                p = proj([(wl_w[:, pcols], xlo[:, :])], [wl_w, xlo])
                ACTF(k, t2[:, :], p[:, :], AF.Sigmoid, [p, vec], [t2], bias=vec[:, o_w0 + pr:o_w0 + pr + 1])
                TS(k.dve, t2[:, :], t2[:, :], DEC, None, ALU.mult, ALU.bypass, [t2], [t2])
                k.dve.op(lambda: nc.vector.tensor_tensor_scan(out=cc[:, :], data0=cmask[:, :], data1=t2[:, :], initial=0.0,
                                                              op0=ALU.mult, op1=ALU.add), reads=[cmask, t2], writes=[cc])
                yield
                TS(k.dve, kkt[:, :], k_[:, :], vec[:, o_kk + pr:o_kk + pr + 1], None, ALU.mult, ALU.bypass, [k_, vec], [kkt])
                rstd = nrm.stats([kkt[:, :]], [kkt], 1.0, 1e-24)
                TT(k.dve, kkt[:, :], kkt[:, :], rstd[:, :], ALU.mult, [kkt, rstd], [kkt])
                yield
                TS(k.dve, t3[:, :], aa[:, :], vec[:, o_ka + pr:o_ka + pr + 1], omka[:, pr:pr + 1], ALU.mult, ALU.add,
                   [aa, vec, omka], [t3])
                TT(k.dve, kmod[:, :], k_[:, :], t3[:, :], ALU.mult, [k_, t3], [kmod])
                yield
                STT(k, tb1[:, :], r_[:, :], vec[:, o_rk + pr:o_rk + pr + 1], kmod[:, :], ALU.mult, ALU.mult,
                    [r_, vec, kmod], [tb1])
                p = proj([(ones2[:, :], tb1[:, :])], [ones2, tb1])
                TT(k.dve, bon[pr][:, :], p[:, :], v_[:, :], ALU.mult, [p, v_], [bon[pr]])
                ACTF(k, vb[pr][:, :], v_[:, :], AF.Copy, [v_], [vb[pr]])
                yield
                ACTF(k, t3[:, :], cc[:, :], AF.Exp, [cc], [t3], scale=-1.0)
                for c in range(NCH):
                    for hl in range(2):
                        STT(k, ar[pr][hl][:, c, 128:256], r_[:, c * C:(c + 1) * C], hm2[:, hl:hl + 1],
                            t3[:, c * C:(c + 1) * C], ALU.mult, ALU.mult, [r_, t3, hm2], [ar[pr][hl]])
                    yield
                TT(k.dve, t3[:, :], cc[:, :], t2[:, :], ALU.subtract, [cc, t2], [t3])
                ACTF(k, t3[:, :], t3[:, :], AF.Exp, [t3], [t3], scale=-1.0)
                for c in range(NCH):
                    for hl in range(2):
                        STT(k, ar[pr][hl][:, c, 0:128], kkt[:, c * C:(c + 1) * C], nhm2[:, hl:hl + 1],
                            t3[:, c * C:(c + 1) * C], ALU.mult, ALU.mult, [kkt, t3, nhm2], [ar[pr][hl]])
                    yield
                TT(k.dve, aa[:, :], aa[:, :], kkt[:, :], ALU.mult, [aa, kkt], [aa])
                ACTF(k, t3[:, :], cc[:, :], AF.Exp, [cc], [t3], scale=1.0)
                TT(k.dve, bt[pr][:, :], aa[:, :], t3[:, :], ALU.mult, [aa, t3], [bt[pr]])
                TT(k.dve, ktl[pr][:, :], kmod[:, :], t3[:, :], ALU.mult, [kmod, t3], [ktl[pr]])
                yield
                TS(k.dve, nbl[:, :], cc[:, C - 1::C], -1.0, None, ALU.mult, ALU.bypass, [cc], [nbl])
                ACTF(k, pC[pr][:, :], nbl[:, :], AF.Exp, [nbl], [pC[pr]])
                for c in range(NCH):
                    ACTF(k, t3[:, c * C:(c + 1) * C], cc[:, c * C:(c + 1) * C], AF.Exp, [cc, nbl], [t3], scale=1.0,
                         bias=nbl[:, c:c + 1])
                TT(k.dve, Bp[pr][:, :], aa[:, :], t3[:, :], ALU.mult, [aa, t3], [Bp[pr]])
                TT(k.dve, Kp[pr][:, :], kmod[:, :], t3[:, :], ALU.mult, [kmod, t3], [Kp[pr]])

                yield

        def drain(g):
            if g is not None:
                for _ in g:
                    pass

        drain(prepR(0))
        for b in range(T // TB):
            t0 = b * TB
            gate, bon, vb, ar, bt, ktl, pC, Bp, Kp = sets[b % 2]
            gp = prepR(b + 1) if (b + 1) * TB < T else None
            hs = [slice(0, 64), slice(64, 128)]

            def stageA(pr, c, sid):
                cs = slice(c * C, (c + 1) * C)
                TM, AbRb, AkRk, PP, X, Xw = TM_[sid], AbRb_[sid], AkRk_[sid], PP_[sid], X_[sid], Xw_[sid]
                ps_x, ps_pp = ps_x_[sid], ps_pp_[sid]
                s_ = bank()
                MM(k, s_[:, 0:128], ar[pr][0][:, c, 0:128], ident[:, :], [ar[pr][0], ident], [s_], stop=False, inc=False)
                MM(k, s_[:, 0:128], ar[pr][1][:, c, 0:128], ident[:, :], [ar[pr][1], ident], [s_], start=False, inc=False)
                for n_, src in enumerate((Bp[pr][:, cs], Kp[pr][:, cs], vb[pr][:, cs])):
                    MM(k, s_[:, (n_ + 1) * 128:(n_ + 2) * 128], src, ident[:, :], [Bp[pr], Kp[pr], vb[pr], ident],
                       [s_], inc=(n_ == 2))
                ACTF(k, TM[:, :, :], s_[:, :], AF.Copy, [s_], [TM])
                yield
                s1 = bank()
                for hl in range(2):
                    MM(k, s1[:, hl * 256:(hl + 1) * 256], bt[pr][:, cs], ar[pr][hl][:, c, :], [bt[pr], ar[pr][hl]],
                       [s1], inc=(hl == 1))
                TT(k.dve, AbRb[:, :, :], s1[:, :], m_si[:, :, :], ALU.mult, [s1, m_si], [AbRb])
                yield
                s2 = bank()
                for hl in range(2):
                    MM(k, s2[:, hl * 256:(hl + 1) * 256], ktl[pr][:, cs], ar[pr][hl][:, c, :], [ktl[pr], ar[pr][hl]],
                       [s2], inc=(hl == 1))
                TT(k.dve, AkRk[:, :, :], s2[:, :], m_si[:, :, :], ALU.mult, [s2, m_si], [AkRk])
                yield
                s3 = bank()
                for hl in range(2):
                    MM(k, s3[:, hl * 128:(hl + 1) * 128], ar[pr][hl][:, c, 0:128], bt[pr][:, cs], [bt[pr], ar[pr][hl]],
                       [s3], inc=(hl == 1))
                P0 = PP[0]
                TT(k.dve, P0[:, :, 0:128], s3[:, 0:256], m_tt[:, :, :], ALU.mult, [s3, m_tt], [P0])
                k.pool.op(lambda P0=P0: nc.gpsimd.tensor_copy(out=P0[:, :, 128:256], in_=AbRb[:, :, 0:128]),
                          reads=[AbRb], writes=[P0])
                yield
                for hl in range(2):
                    MM(k, ps_x[:, hl * 128 + 64:(hl + 1) * 128], AkRk[:, hl, 0:128], TM[:, 3, hs[hl]], [AkRk, TM],
                       [ps_x], inc=(hl == 1))
                for hl in range(2):
                    ACTF(k, X[:, hl, 0:64], TM[:, 0, hs[hl]], AF.Copy, [TM], [X])
                    ACTF(k, X[:, hl, 64:128], ps_x[:, hl * 128 + 64:(hl + 1) * 128], AF.Copy, [ps_x], [X])
                yield
                for lv in range(7):
                    Pc = PP[lv % 2]
                    for hl in range(2):
                        MM(k, ps_x[:, hl * 128:(hl + 1) * 128], Pc[:, hl, 128:256], X[:, hl, :], [Pc, X], [ps_x],
                           inc=(hl == 1))
                    if lv < 6:
                        Pn = PP[(lv + 1) % 2]
                        for hl in range(2):
                            MM(k, ps_pp[:, hl, 0:128], Pc[:, hl, 128:256], Pc[:, hl, 0:128], [Pc], [ps_pp], inc=False)
                            MM(k, ps_pp[:, hl, 128:256], Pc[:, hl, 0:128], Pc[:, hl, 128:256], [Pc], [ps_pp],
                               inc=(hl == 1))
                        ACTF(k, Pn[:, :, :], ps_pp[:, :, :], AF.Copy, [ps_pp], [Pn])
                    TT(k.dve, X[:, :, :], ps_x[:, 0:256], X[:, :, :], ALU.add, [ps_x, X], [X])
                    yield
                for hl in range(2):
                    ACTF(k, Xw[hl][:, hs[hl]], X[:, hl, 0:64], AF.Copy, [X], [Xw[hl]])
                for hl in range(2):
                    pq = bank()
                    MM(k, pq[:, 0:128], Xw[hl][:, :], AbRb[:, hl, 128:256], [Xw[hl], AbRb], [pq])
                    TT(k.dve, QpT[pr][hl][:, cs], pq[:, 0:128], ar[pr][hl][:, c, 128:256], ALU.add, [pq, ar[pr][hl]],
                       [QpT[pr][hl]])
                yield
                py0 = bank()
                for hl in range(2):
                    MM(k, py0[hs[hl], 0:128], X[:, hl, 64:128], AbRb[:, hl, 128:256], [X, AbRb], [py0], stop=False,
                       inc=False)
                    MM(k, py0[hs[hl], 0:128], TM[:, 3, hs[hl]], AkRk[:, hl, 128:256], [TM, AkRk], [py0], start=False,
                       inc=(hl == 1))
                ACTF(k, Y0T[pr][:, cs], py0[:, 0:128], AF.Copy, [py0], [Y0T[pr]])
                yield
                for hl in range(2):
                    pm = bank()
                    MM(k, pm[:, 0:64], Xw[hl][:, :], TM[:, 1, hs[hl]], [Xw[hl], TM], [pm])
                    STT(k, MT[pr][hl][:, c, :], I2h[hl][:, :], pC[pr][:, c:c + 1], pm[:, 0:64], ALU.mult, ALU.add,
                        [I2h[hl], pC[pr], pm], [MT[pr][hl]])
                yield
                pg = bank()
                for hl in range(2):
                    MM(k, pg[hs[hl], 0:64], TM[:, 1, hs[hl]], X[:, hl, 64:128], [X, TM], [pg], stop=False, inc=False)
                    MM(k, pg[hs[hl], 0:64], TM[:, 2, hs[hl]], TM[:, 3, hs[hl]], [TM], [pg], start=False,
                       inc=(hl == 1))
                ACTF(k, Gm[pr][:, c, :], pg[:, 0:64], AF.Copy, [pg], [Gm[pr]])

            pending = [(pr, c) for c in range(NCH) for pr in range(2)]
            active = {}
            while pending or active:
                for sid in range(NS):
                    if sid not in active and pending:
                        pr_, c_ = pending.pop(0)
                        active[sid] = stageA(pr_, c_, sid)
                for sid in list(active):
                    try:
                        next(active[sid])
                    except StopIteration:
                        del active[sid]
                if gp is not None:
                    try:
                        next(gp)
                    except StopIteration:
                        gp = None
            for c in range(NCH):
                cs = slice(c * C, (c + 1) * C)
                for pr in range(2):
                    py = bank()
                    for hl in range(2):
                        MM(k, py[hs[hl], 0:128], Hb[pr][:, :], QpT[pr][hl][:, cs], [Hb[pr], QpT[pr][hl]], [py], inc=(hl == 1))
                    TT(k.dve, yacc[pr][:, cs], py[:, 0:128], Y0T[pr][:, cs], ALU.add, [py, Y0T[pr]], [yacc[pr]])
                    ph = bank()
                    for hl in range(2):
                        MM(k, ph[hs[hl], 0:64], MT[pr][hl][:, c, :], Hb[pr][:, :], [MT[pr][hl], Hb[pr]], [ph], inc=(hl == 1))
                    TT(k.dve, Hb[pr][:, :], ph[:, 0:64], Gm[pr][:, c, :], ALU.add, [ph, Gm[pr]], [Hb[pr]])
            for pr in range(2):
                ACTF(k, ob1[:, :], yacc[pr][:, :], AF.Copy, [yacc[pr]], [ob1])
                p = proj([(ones2[:, :], ob1[:, :])], [ones2, ob1])
                STT(k, o1[:, :], p[:, :], -1.0 / RW_N, yacc[pr][:, :], ALU.mult, ALU.add, [p, yacc[pr]], [o1])
                rstd = nrm.stats([o1[:, :]], [o1], RW_N, RW_EPS)
                TT(k.dve, o1[:, :], o1[:, :], rstd[:, :], ALU.mult, [o1, rstd], [o1])
                TS(k.dve, o1[:, :], o1[:, :], vec[:, o_lg + pr:o_lg + pr + 1], vec[:, o_lb + pr:o_lb + pr + 1], ALU.mult,
                   ALU.add, [o1, vec], [o1])
                TT(k.dve, o1[:, :], o1[:, :], bon[pr][:, :], ALU.add, [o1, bon[pr]], [o1])
                TT(k.dve, yo[pr][:, :], o1[:, :], gate[pr][:, :], ALU.mult, [o1, gate[pr]], [yo[pr]])
                k.sp.dma(dr["yT"][:, 6 + pr, t0:t0 + TB], yo[pr][:, :], yo[pr], reads=[yo[pr]], writes=[dr["yT_buf"]])
            drain(gp)


def phase_out(k, T, l, dr, cst):
    nc = k.nc
    with k.phase() as es:
        vec = k.sb(es, "o_vec", [128, NV], F32)
        k.sp.dma(vec[:, :], dr["vecs"][l], vec, writes=[vec])
        ones = k.sb(es, "o_ones", [128, 128], BF16)
        load_cast(k, ones, ones[:, :], cst["ones"])
        wo = k.sb(es, "o_wo", [128, 8, D], BF16)
        load_cast3(k, wo, wo[:, :, :], dr["w_out"][l], 0, D)
        nrm = Norm(k, es, "o_n", TB, ones)
        xb = [k.sb(es, f"o_xb{i}", [128, 8, TB], F32) for i in range(2)]
        yb = [k.sb(es, f"o_yb{i}", [128, 8, TB], BF16) for i in range(2)]
        yn = k.sb(es, "o_yn", [128, 4, TB], BF16)
        ps = [k.ps(es, f"o_ps{i}", [128, TB]) for i in range(3)]
        o_g = VOFF["on_g"][0]
        xsrc, xsrc_buf = (dr["x_in"], Buf(None, "xin")) if l == 0 else (dr["xT"], dr["xT_buf"])
        for b in range(T // TB):
            t0 = b * TB
            x, y = xb[b % 2], yb[b % 2]
            k.sp.dma(x[:, :, :], xsrc[:, :, t0:t0 + TB], x, reads=[xsrc_buf], writes=[x])
            k.sp.dma(y[:, :, :], dr["yT"][:, :, t0:t0 + TB], y, reads=[dr["yT_buf"]], writes=[y])
            rstd = nrm.stats([y[:, c, :] for c in range(4)], [y] * 4, MLA_H * DV, EPS)
            for c in range(4):
                k.dve.op(lambda c=c, y=y: nc.vector.scalar_tensor_tensor(
                    out=yn[:, c, :], in0=y[:, c, :], scalar=vec[:, o_g + c:o_g + c + 1], in1=rstd[:, :],
                    op0=ALU.mult, op1=ALU.mult), reads=[y, vec, rstd], writes=[yn])
            for m in range(8):
                p = ps[m % 3]
                mm(k, p, p[:, :], [(wo[:, c, m * 128:(m + 1) * 128], yn[:, c, :] if c < 4 else y[:, c, :])
                                   for c in range(8)], [wo, yn, y])
                k.dve.op(lambda m=m, p=p, x=x: nc.vector.tensor_tensor(out=x[:, m, :], in0=p[:, :], in1=x[:, m, :],
                                                                       op=ALU.add), reads=[p, x], writes=[x])
            k.sp.dma(dr["xT"][:, :, t0:t0 + TB], x[:, :, :], x, reads=[x], writes=[dr["xT_buf"]])


def build(T, shapes, phases=("h0", "mla", "gla", "rwkv", "out", "ffn"), depth=DEPTH, debug=()):
    nc = bass.Bass("TRN2", target_bir_lowering=False)
    dr = {}
    for n, shp in shapes.items():
        dr[n] = nc.dram_tensor(n, list(shp), F32, kind="ExternalInput").ap()
    dr["x_in"] = dr["xT_in"].rearrange("(c p) t -> p c t", p=128)

    def scratch(name, dtype):
        t = nc.dram_tensor(name, [D, T], dtype, kind="ExternalOutput" if name in debug else "Internal").ap()
        dr[name] = t.rearrange("(c p) t -> p c t", p=128)
        dr[name + "_buf"] = Buf(None, name)

    outT = nc.dram_tensor("outT", [D, T], F32, kind="ExternalOutput").ap()
    dr["outT"] = outT.rearrange("(c p) t -> p c t", p=128)
    dr["outT_buf"] = Buf(None, "outT")
    scratch("xT", F32)
    scratch("hT", BF16)
    scratch("yT", BF16)
    cst = {"ones": dr["c_ones"], "amask": dr["c_amask"], "cos4": dr["c_cos4"], "sin4": dr["c_sin4"],
           "misc": dr["c_misc"]}
    with contextlib.ExitStack() as es:
        k = K(nc, es)
        if "h0" not in phases:
            with k.phase() as pes:
                t = k.sb(pes, "cp", [128, 8, TB], F32)
                for b in range(T // TB):
                    k.sp.dma(t[:, :, :], dr["x_in"][:, :, b * TB:(b + 1) * TB], t, writes=[t])
                    k.sp.dma(dr["xT"][:, :, b * TB:(b + 1) * TB], t[:, :, :], t, reads=[t], writes=[dr["xT_buf"]])
        else:
            phase_h0(k, T, dr, cst)
        for l in range(depth):
            if "mla" in phases:
                phase_mla(k, T, l, dr, cst, (0, 1))
                phase_mla(k, T, l, dr, cst, (2, 3))
            if "gla" in phases:
                phase_gla(k, T, l, dr, cst)
            if "rwkv" in phases:
                phase_rwkv(k, T, l, dr, cst)
            if "out" in phases:
                phase_out(k, T, l, dr, cst)
            if "ffn" in phases:
                phase_ffn(k, T, l, dr, cst, last=(l == depth - 1))
        k.barrier()
    return nc


W_NAMES = ["w_in", "mla_w_uq", "mla_w_ukv", "gla_w_gk", "rwkv_w2", "rwkv_a2", "rwkv_g2", "w_out",
           "ffn_w_up", "ffn_w_down"]


def pack_vecs(inp, l):
    v = np.zeros((128, NV), np.float32)

    def put(name, arr, n):
        o, c = VOFF[name]
        assert c == n
        v[:, o:o + n] = _cols(arr, n)

    put("ln1_g", inp["ln1_g"][l], 8)
    put("ln2_g", inp["ln2_g"][l], 8)
    put("final_g", inp["final_g"], 8)
    put("qn_g", inp["mla_q_norm_g"][l], 3)
    put("kvn_g", inp["mla_kv_norm_g"][l], 2)
    put("on_g", inp["mla_out_norm_g"][l], 4)
    put("gla_nb", inp["gla_b_gk"][l], 1)
    put("gla_ng", np.tile(inp["gla_norm_g"][l], 2), 1)
    put("rw_mu", inp["rwkv_mu"][l], 8)
    put("rw_w0", inp["rwkv_w0"][l], 2)
    put("rw_a0", inp["rwkv_a0"][l], 2)
    put("rw_kk", inp["rwkv_k_k"][l], 2)
    put("rw_ka", inp["rwkv_k_a"][l], 2)
    put("rw_rk", inp["rwkv_r_k"][l].reshape(-1), 2)
    put("rw_lng", inp["rwkv_ln_g"][l], 2)
    put("rw_lnb", inp["rwkv_ln_b"][l], 2)
    cw = inp["ffn_conv_w"][l]
    put("cw0", cw[0], 44)
    put("cw1", cw[1], 44)
    put("cw2", cw[2], 44)
    put("cb", inp["ffn_conv_b"][l], 44)
    return v


def host_inputs(inp, b, T):
    m = {}
    m["xT_in"] = np.ascontiguousarray(np.asarray(inp["x"][b, :T], np.float32).T)
    m["vecs"] = np.stack([pack_vecs(inp, l) for l in range(DEPTH)])
    m["c_ones"] = np.ones((128, 128), np.float32)
    m["c_misc"] = make_misc()
    kk = np.arange(128)[:, None]
    qq = np.arange(TB)[None, :]
    m["c_amask"] = np.stack([np.where(kk + 128 * d <= qq, 0.0, -30000.0).astype(np.float32) for d in range(4)])
    inv = (np.float32(1.0) / (np.float32(10000.0) ** (np.arange(0, ROPE, 2, dtype=np.float32) / np.float32(ROPE)))
           ).astype(np.float32)
    ang = (np.arange(T, dtype=np.float32)[None, :] * inv[:, None]).astype(np.float32)
    cs, sn = np.cos(ang).astype(np.float32), np.sin(ang).astype(np.float32)
    m["c_cos4"] = np.ascontiguousarray(np.concatenate([cs, cs, cs, cs], 0))
    m["c_sin4"] = np.ascontiguousarray(np.concatenate([-sn, sn, -sn, sn], 0))
    for n in W_NAMES:
        m[n] = np.ascontiguousarray(np.asarray(inp[n], np.float32))
    return m


def run(inp, T, nb, **bk):
    maps = [host_inputs(inp, b, T) for b in range(nb)]
    shapes = {n: a.shape for n, a in maps[0].items()}
    nc = build(T, shapes, **bk)
    res = run_bass_kernel_spmd(nc, maps, core_ids=list(range(nb)))
    return res


def kernel(**inputs):
    inp = {n: np.asarray(a) for n, a in inputs.items()}
    B, T = inp["x"].shape[0], inp["x"].shape[1]
    ncores = 8
    maps = [host_inputs(inp, b % B, T) for b in range(ncores)]
    shapes = {n: a.shape for n, a in maps[0].items()}
    nc = build(T, shapes)
    res = run_bass_kernel_spmd(nc, maps, core_ids=list(range(ncores)))
    out = np.stack([np.ascontiguousarray(res.results[b]["outT"].T) for b in range(B)])
    return out.astype(np.float32)


def phase_h0(k, T, dr, cst):
    nc = k.nc
    with k.phase() as es:
        vec = k.sb(es, "h_vec", [128, NV], F32)
        k.sp.dma(vec[:, :], dr["vecs"][0], vec, writes=[vec])
        ones = k.sb(es, "h_ones", [128, 128], BF16)
        load_cast(k, ones, ones[:, :], cst["ones"])
        nrm = Norm(k, es, "h_n", TB, ones)
        xb = [k.sb(es, f"h_xb{i}", [128, 8, TB], F32) for i in range(2)]
        hT = [k.sb(es, f"h_hT{i}", [128, 8, TB], BF16) for i in range(2)]
        o_g = VOFF["ln1_g"][0]
        for b in range(T // TB):
            x, h = xb[b % 2], hT[b % 2]
            t0 = b * TB
            k.sp.dma(x[:, :, :], dr["x_in"][:, :, t0:t0 + TB], x, writes=[x])
            rstd = nrm.stats([x[:, c, :] for c in range(8)], [x] * 8, D, EPS)
            for c in range(8):
                k.dve.op(lambda c=c, x=x, h=h: nc.vector.scalar_tensor_tensor(
                    out=h[:, c, :], in0=x[:, c, :], scalar=vec[:, o_g + c:o_g + c + 1], in1=rstd[:, :],
                    op0=ALU.mult, op1=ALU.mult), reads=[x, vec, rstd], writes=[h])
            k.sp.dma(dr["hT"][:, :, t0:t0 + TB], h[:, :, :], h, reads=[h], writes=[dr["hT_buf"]])


def phase_mla(k, T, l, dr, cst, heads):
    nc = k.nc
    nh = len(heads)
    nblk = T // TB
    NKT = T // 128
    scale = float((NOPE + ROPE) ** -0.5)
    w_in, w_uq, w_ukv = dr["w_in"], dr["mla_w_uq"], dr["mla_w_ukv"]
    with k.phase() as es:
        vec = k.sb(es, "a_vec", [128, NV], F32)
        k.sp.dma(vec[:, :], dr["vecs"][l], vec, writes=[vec])
        ones = k.sb(es, "a_ones", [128, 128], BF16)
        load_cast(k, ones, ones[:, :], cst["ones"])
        masks = k.sb(es, "a_mask", [128, 4, TB], BF16)
        k.pool.dma(masks[:, :, :], cst["amask"].rearrange("d p n -> p d n"), masks, writes=[masks])
        ident = k.sb(es, "a_ident", [128, 128], BF16)
        load_cast(k, ident, ident[:, :], cst["misc"][:, MOFF["ident"][0]:MOFF["ident"][1]])
        wA = k.sb(es, "a_wA", [128, 8, 896], BF16)
        load_cast3(k, wA, wA[:, :, 0:640], w_in[l], 0, 640)
        for r in range(2):
            load_cast3(k, wA, wA[:, :, 640 + 64 * r:704 + 64 * r], w_in[l], 640, 704)
            load_cast3(k, wA, wA[:, :, 768 + 64 * r:800 + 64 * r], w_in[l], 672, 704)
            load_cast3(k, wA, wA[:, :, 800 + 64 * r:832 + 64 * r], w_in[l], 640, 672)
        wq = k.sb(es, "a_wq", [128, 3, 3 * nh * 128], BF16)
        ro = nh * 128
        so = 2 * nh * 128
        k.pool.op(lambda: nc.gpsimd.memset(wq[:, :, :], 0.0), writes=[wq])
        for i, h in enumerate(heads):
            b0 = h * 192
            load_cast3(k, wq, wq[:, :, i * 128:(i + 1) * 128], w_uq[l], b0, b0 + 128)
            load_cast3(k, wq, wq[:, :, ro + i * 128:ro + i * 128 + 64], w_uq[l], b0 + 128, b0 + 192)
            load_cast3(k, wq, wq[:, :, so + i * 128:so + i * 128 + 32], w_uq[l], b0 + 160, b0 + 192)
            load_cast3(k, wq, wq[:, :, so + i * 128 + 32:so + i * 128 + 64], w_uq[l], b0 + 128, b0 + 160)
        wk = k.sb(es, "a_wk", [128, 2, nh * 128], BF16)
        wv = k.sb(es, "a_wv", [128, 2, nh * 128], BF16)
        for i, h in enumerate(heads):
            load_cast3(k, wk, wk[:, :, i * 128:(i + 1) * 128], w_ukv[l], h * 256, h * 256 + 128)
            load_cast3(k, wv, wv[:, :, i * 128:(i + 1) * 128], w_ukv[l], h * 256 + 128, h * 256 + 256)
        kn = [k.sb(es, f"a_kn{i}", [128, T], BF16) for i in range(nh)]
        kr = k.sb(es, "a_kr", [128, T], BF16)
        vc = k.sb(es, "a_vc", [128, NKT, nh * 128], BF16)
        hT = [k.sb(es, f"a_hT{i}", [128, 8, TB], BF16) for i in range(2)]
        cq = k.sb(es, "a_cq", [128, 3, TB], F32)
        ckv = k.sb(es, "a_ckv", [128, 2, TB], F32)
        cqn = k.sb(es, "a_cqn", [128, 3, TB], BF16)
        ckvn = k.sb(es, "a_ckvn", [128, 2, TB], BF16)
        qn = [k.sb(es, f"a_qn{i}", [128, TB], BF16) for i in range(nh)]
        qr = [k.sb(es, f"a_qr{i}", [128, TB], BF16) for i in range(nh)]
        cos = k.sb(es, "a_cos", [128, TB], F32)
        sin = k.sb(es, "a_sin", [128, TB], F32)
        t1 = k.sb(es, "a_t1", [128, TB], F32)
        t2 = k.sb(es, "a_t2", [128, TB], F32)
        pT = [k.sb(es, f"a_pT{i}", [128, TB], BF16) for i in range(3)]
        oT = [k.sb(es, f"a_oT{i}", [128, TB], BF16) for i in range(2)]
        rcp = k.sb(es, "a_rcp", [128, TB], F32)
        paccs = [k.sb(es, f"a_pacc{i}", [128, TB], F32) for i in range(2)]
        oraw = k.sb(es, "a_oraw", [128, TB], F32)
        ones_f = k.sb(es, "a_onesf", [128, 128], F32)
        k.sp.dma(ones_f[:, :], cst["ones"], ones_f, writes=[ones_f])
        nrm = Norm(k, es, "a_n", TB, ones)
        ps_p = [k.ps(es, f"a_psp{i}", [128, TB]) for i in range(2)]
        ps_s = [k.ps(es, f"a_pss{i}", [128, TB]) for i in range(3)]
        ps_o = k.ps(es, "a_pso", [128, TB])
        ps_l = k.ps(es, "a_psl", [128, TB])
        o_qg, o_kg = VOFF["qn_g"][0], VOFF["kvn_g"][0]
        ip = 0
        ipt = 0

        def proj(lhs_list, rhs_list, reads, m=128, n=TB):
            nonlocal ip
            p = ps_p[ip % 2]
            ip += 1
            mm(k, p, p[0:m, 0:n], list(zip(lhs_list, rhs_list)), reads)
            return p

        kn_b = [[Buf(kn[i].ap[:, bb * TB:(bb + 1) * TB], f"kn{i}_{bb}") for bb in range(nblk)] for i in range(nh)]
        kr_b = [Buf(kr.ap[:, bb * TB:(bb + 1) * TB], f"kr_{bb}") for bb in range(nblk)]
        vc_b = [Buf(vc.ap[:, bb * 4:(bb + 1) * 4, :], f"vc_{bb}") for bb in range(nblk)]
        qn2 = [qn, [k.sb(es, f"a_qnB{i}", [128, TB], BF16) for i in range(nh)]]
        qr2 = [qr, [k.sb(es, f"a_qrB{i}", [128, TB], BF16) for i in range(nh)]]

        def prep(b):
            t0 = b * TB
            h = hT[b % 2]
            qn_, qr_ = qn2[b % 2], qr2[b % 2]
            k.sp.dma(h[:, :, :], dr["hT"][:, :, t0:t0 + TB], h, reads=[dr["hT_buf"]], writes=[h])
            k.sp.dma(cos[:, :], cst["cos4"][:, t0:t0 + TB], cos, writes=[cos])
            k.sp.dma(sin[:, :], cst["sin4"][:, t0:t0 + TB], sin, writes=[sin])
            yield
            for j in range(3):
                p = proj([wA[:, c, j * 128:(j + 1) * 128] for c in range(8)], [h[:, c, :] for c in range(8)], [wA, h])
                k.act.op(lambda p=p, j=j: nc.scalar.activation(out=cq[:, j, :], in_=p[:, :], func=AF.Copy),
                         reads=[p], writes=[cq])
                yield
            for j in range(2):
                p = proj([wA[:, c, 384 + j * 128:384 + (j + 1) * 128] for c in range(8)],
                         [h[:, c, :] for c in range(8)], [wA, h])
                k.act.op(lambda p=p, j=j: nc.scalar.activation(out=ckv[:, j, :], in_=p[:, :], func=AF.Copy),
                         reads=[p], writes=[ckv])
                yield
            rstd = nrm.stats([cq[:, j, :] for j in range(3)], [cq] * 3, QLORA, EPS)
            yield
            for j in range(3):
                k.dve.op(lambda j=j: nc.vector.scalar_tensor_tensor(
                    out=cqn[:, j, :], in0=cq[:, j, :], scalar=vec[:, o_qg + j:o_qg + j + 1], in1=rstd[:, :],
                    op0=ALU.mult, op1=ALU.mult), reads=[cq, vec, rstd], writes=[cqn])
            yield
            rstd = nrm.stats([ckv[:, j, :] for j in range(2)], [ckv] * 2, KVLORA, EPS)
            yield
            for j in range(2):
                k.dve.op(lambda j=j: nc.vector.scalar_tensor_tensor(
                    out=ckvn[:, j, :], in0=ckv[:, j, :], scalar=vec[:, o_kg + j:o_kg + j + 1], in1=rstd[:, :],
                    op0=ALU.mult, op1=ALU.mult), reads=[ckv, vec, rstd], writes=[ckvn])
            yield
            pa = proj([wA[:, c, 640:768] for c in range(8)], [h[:, c, :] for c in range(8)], [wA, h])
            k.dve.op(lambda pa=pa: nc.vector.tensor_tensor(out=t1[:, :], in0=pa[:, :], in1=cos[:, :], op=ALU.mult),
                     reads=[pa, cos], writes=[t1])
            yield
            pb = proj([wA[:, c, 768:896] for c in range(8)], [h[:, c, :] for c in range(8)], [wA, h])
            k.dve.op(lambda pb=pb: nc.vector.tensor_tensor(out=t2[:, :], in0=pb[:, :], in1=sin[:, :], op=ALU.mult),
                     reads=[pb, sin], writes=[t2])
            k.pool.op(lambda: nc.gpsimd.tensor_tensor(out=kr_b[b][:, :], in0=t1[:, :], in1=t2[:, :], op=ALU.add),
                      reads=[t1, t2], writes=[kr_b[b]])
            yield
            for i in range(nh):
                p = proj([wk[:, c, i * 128:(i + 1) * 128] for c in range(2)], [ckvn[:, c, :] for c in range(2)],
                         [wk, ckvn])
                k.act.op(lambda p=p, i=i: nc.scalar.activation(out=kn_b[i][b][:, :], in_=p[:, :], func=AF.Copy),
                         reads=[p], writes=[kn_b[i][b]])
                yield
            for s_ in range(TB // 128):
                p = proj([ckvn[:, c, s_ * 128:(s_ + 1) * 128] for c in range(2)], [wv[:, c, :] for c in range(2)],
                         [wv, ckvn], n=nh * 128)
                k.act.op(lambda p=p, s_=s_: nc.scalar.activation(out=vc_b[b][:, s_, :], in_=p[:, 0:nh * 128],
                                                                 func=AF.Copy), reads=[p], writes=[vc_b[b]])
                yield
            for i in range(nh):
                p = proj([wq[:, c, i * 128:(i + 1) * 128] for c in range(3)], [cqn[:, c, :] for c in range(3)],
                         [wq, cqn])
                k.act.op(lambda p=p, i=i: nc.scalar.activation(out=qn_[i][:, :], in_=p[:, :], func=AF.Copy),
                         reads=[p], writes=[qn_[i]])
                yield
            for pr in range(nh):
                pa = proj([wq[:, c, ro + pr * 128:ro + (pr + 1) * 128] for c in range(3)],
                          [cqn[:, c, :] for c in range(3)], [wq, cqn])
                k.dve.op(lambda pa=pa: nc.vector.tensor_tensor(out=t1[:, :], in0=pa[:, :], in1=cos[:, :],
                                                               op=ALU.mult), reads=[pa, cos], writes=[t1])
                yield
                pb = proj([wq[:, c, so + pr * 128:so + (pr + 1) * 128] for c in range(3)],
                          [cqn[:, c, :] for c in range(3)], [wq, cqn])
                k.dve.op(lambda pb=pb: nc.vector.tensor_tensor(out=t2[:, :], in0=pb[:, :], in1=sin[:, :],
                                                               op=ALU.mult), reads=[pb, sin], writes=[t2])
                k.pool.op(lambda pr=pr: nc.gpsimd.tensor_tensor(out=qr_[pr][:, :], in0=t1[:, :], in1=t2[:, :],
                                                                op=ALU.add), reads=[t1, t2], writes=[qr_[pr]])
                yield

        def drain(g):
            if g is not None:
                for _ in g:
                    pass

        drain(prep(0))
        for b in range(nblk):
            t0 = b * TB
            qn_, qr_ = qn2[b % 2], qr2[b % 2]
            gnext = prep(b + 1) if b + 1 < nblk else None
            nkt = (t0 + TB) // 128
            its = [(i, kt) for i in range(nh) for kt in range(nkt)]
            sb_of = {}

            def issue_s(n):
                nonlocal ipt
                i, kt = its[n]
                s = ps_s[ipt % 3]
                ipt += 1
                bb, ks = kt // 4, slice((kt % 4) * 128, (kt % 4 + 1) * 128)
                pairs = [(kn_b[i][bb][:, ks], qn_[i][:, :]), (kr_b[bb][:, ks], qr_[i][:, :])]
                d_ = kt - (t0 // 128)
                if d_ >= 0:
                    pairs.append((ident[:, :], masks[:, d_, :]))
                mm(k, s, s[:, :], pairs, [kn_b[i][bb], qn_[i], kr_b[bb], qr_[i], ident, masks])
                sb_of[n] = s

            for n in range(min(2, len(its))):
                issue_s(n)
            for n, (i, kt) in enumerate(its):
                if n + 2 < len(its):
                    issue_s(n + 2)
                if gnext is not None:
                    try:
                        next(gnext)
                    except StopIteration:
                        gnext = None
                s = sb_of.pop(n)
                pt = pT[n % 3]
                k.act.op(lambda s=s, pt=pt: nc.scalar.activation(out=pt[:, :], in_=s[:, :], func=AF.Exp,
                                                                 scale=scale), reads=[s], writes=[pt])
                bb = kt // 4
                k.pe.op(lambda pt=pt, kt=kt, i=i, bb=bb: nc.tensor.matmul(
                    ps_o[:, :], lhsT=vc_b[bb][:, kt % 4, i * 128:(i + 1) * 128], rhs=pt[:, :], start=(kt == 0),
                    stop=(kt == nkt - 1)), reads=[vc_b[bb], pt], writes=[ps_o], inc=True)
                pacc = paccs[kt % 2]
                e_ = k.dve if kt % 2 == 0 else k.pool
                if kt < 2:
                    e_.op(lambda pt=pt, e_=e_, pacc=pacc: e_.eng.tensor_copy(out=pacc[:, :], in_=pt[:, :]),
                          reads=[pt], writes=[pacc])
                else:
                    e_.op(lambda pt=pt, e_=e_, pacc=pacc: e_.eng.tensor_tensor(out=pacc[:, :], in0=pacc[:, :],
                                                                               in1=pt[:, :], op=ALU.add),
                          reads=[pt, pacc], writes=[pacc])
                if kt == nkt - 1:
                    k.act.op(lambda: nc.scalar.activation(out=oraw[:, :], in_=ps_o[:, :], func=AF.Copy),
                             reads=[ps_o], writes=[oraw])
                    k.pe.op(lambda: nc.tensor.matmul(ps_l[:, :], lhsT=ones_f[:, :], rhs=paccs[0][:, :], start=True,
                                                     stop=False), reads=[ones_f, paccs[0]], writes=[ps_l], inc=False)
                    k.pe.op(lambda: nc.tensor.matmul(ps_l[:, :], lhsT=ones_f[:, :], rhs=paccs[1][:, :], start=False,
                                                     stop=True), reads=[ones_f, paccs[1]], writes=[ps_l], inc=True)
                    k.dve.op(lambda: nc.vector.reciprocal(out=rcp[:, :], in_=ps_l[:, :]), reads=[ps_l], writes=[rcp])
                    o = oT[i % 2]
                    k.dve.op(lambda o=o: nc.vector.tensor_tensor(out=o[:, :], in0=oraw[:, :], in1=rcp[:, :],
                                                                 op=ALU.mult), reads=[oraw, rcp], writes=[o])
                    k.sp.dma(dr["yT"][:, heads[i], t0:t0 + TB], o[:, :], o, reads=[o], writes=[dr["yT_buf"]])
            drain(gnext)
```

```python
import contextlib
import numpy as np
import concourse.bass as bass
import concourse.mybir as mybir
from concourse.bass_utils import run_bass_kernel_spmd

F32 = mybir.dt.float32
BF16 = mybir.dt.bfloat16
AF = mybir.ActivationFunctionType
ALU = mybir.AluOpType

D = 1024
DEPTH = 2
MLA_H, NOPE, ROPE, DV = 4, 128, 64, 128
QLORA, KVLORA = 384, 256
GLA_H, GDK, GDV, GRANK = 4, 32, 64, 16
RW_H, RW_N = 4, 64
DFF = 2816
NFT = DFF // 128
EPS = 1e-6
RW_EPS = 64e-5
TB = 512
REPORT_SBUF = False
NO_SELF_WAIT = False


class Buf:
    def __init__(self, ap=None, name=""):
        self.ap = ap
        self.name = name
        self.w = None
        self.r = {}
        self.d = None

    def __getitem__(self, idx):
        return self.ap[idx]


class Eng:
    def __init__(self, k, name, eng, sem):
        self.k, self.name, self.eng, self.sem = k, name, eng, sem
        self.count = 0
        self.known = {}
        self.pending = False

    def wait(self, tok):
        if tok is None:
            return
        sem, val = tok
        if sem is self.sem and (self.name == "pe" or val > self.count or NO_SELF_WAIT):
            return
        if self.known.get(sem, 0) >= val:
            return
        self.eng.wait_ge(sem, val)
        self.known[sem] = val

    def deps(self, reads, writes):
        for b in reads:
            self.wait(b.w)
        for b in writes:
            self.wait(b.w)
            for s, v in list(b.r.items()):
                self.wait((s, v))

    def mark(self, tok, reads, writes):
        s, v = tok
        for b in reads:
            if b.r.get(s, 0) < v:
                b.r[s] = v
        for b in writes:
            b.w = tok
            b.r = {}

    def op(self, fn, reads=(), writes=(), inc=True):
        self.deps(reads, writes)
        inst = fn()
        tok = (self.sem, self.count + 1)
        if inc:
            inst.then_inc(self.sem, 1)
            self.count += 1
            self.pending = False
        else:
            self.pending = True
        self.mark(tok, reads, writes)
        return inst

    def dma(self, out, in_, sb, reads=(), writes=()):
        self.deps(reads, writes)
        if sb.d is None:
            sb.d = {}
        if self.name not in sb.d:
            sb.d[self.name] = self.k.get_dsem(self.name)
            self.k.phase_dma.append((sb, self.name))
        e = sb.d[self.name]
        e[1] += 16
        self.eng.dma_start(out=out, in_=in_).then_inc(e[0], 16)
        self.mark((e[0], e[1]), reads, writes)


class K:
    def __init__(self, nc, es):
        self.nc, self.es = nc, es
        self.nsem = 0
        self.pe = Eng(self, "pe", nc.tensor, self.new_sem("pe"))
        self.act = Eng(self, "act", nc.scalar, self.new_sem("act"))
        self.dve = Eng(self, "dve", nc.vector, self.new_sem("dve"))
        self.pool = Eng(self, "pool", nc.gpsimd, self.new_sem("pool"))
        self.sp = Eng(self, "sp", nc.sync, self.new_sem("sp"))
        self.engs = [self.pe, self.act, self.dve, self.pool, self.sp]
        self.dsem_free = {"sp": [], "pool": []}
        self.phase_dma = []

    def get_dsem(self, q):
        if self.dsem_free[q]:
            return self.dsem_free[q].pop()
        return [self.new_sem("dma" + q), 0]

    @contextlib.contextmanager
    def phase(self):
        with contextlib.ExitStack() as es:
            yield es
            if REPORT_SBUF:
                print("sbuf remaining at phase end:", self.nc.sbuf_bytes_remaining)
            self.barrier()
            for b, q in self.phase_dma:
                self.dsem_free[q].append(b.d.pop(q))
            self.phase_dma = []

    def new_sem(self, name):
        self.nsem += 1
        return self.es.enter_context(self.nc.semaphore(f"{name}_{self.nsem}"))

    def sb(self, es, name, shape, dtype):
        self.nsem += 1
        t = es.enter_context(self.nc.sbuf_tensor(f"{name}_{self.nsem}", list(shape), dtype))
        return Buf(t, name)

    def ps(self, es, name, shape, dtype=F32):
        self.nsem += 1
        t = es.enter_context(self.nc.psum_tensor(f"{name}_{self.nsem}", list(shape), dtype))
        return Buf(t, name)

    def barrier(self):
        toks = []
        for e in self.engs:
            assert not e.pending, e.name
            if e.count:
                toks.append((e.sem, e.count))
        for b, q in self.phase_dma:
            toks.append((b.d[q][0], b.d[q][1]))
        for e in self.engs:
            for t in toks:
                e.wait(t)


def mm(k, ps_buf, ps_ap, pairs, reads):
    n = len(pairs)
    for i, (l, r) in enumerate(pairs):
        k.pe.op(lambda l=l, r=r, i=i: k.nc.tensor.matmul(ps_ap, lhsT=l, rhs=r, start=(i == 0),
                                                         stop=(i == n - 1)),
                reads=reads, writes=[ps_buf], inc=(i == n - 1))


def load_cast(k, wbuf, dst_ap, src_ap, dram_reads=()):
    n = src_ap.shape[-1]
    for c0 in range(0, n, 2048):
        c1 = min(n, c0 + 2048)
        k.pool.dma(dst_ap[:, c0:c1], src_ap[:, c0:c1], wbuf, reads=list(dram_reads), writes=[wbuf])


def load_cast3(k, wbuf, dst3, src2, c0, c1):
    src3 = src2.rearrange("(c p) n -> p c n", p=128)
    n = c1 - c0
    for o in range(0, n, 2048):
        e = min(n, o + 2048)
        k.pool.dma(dst3[:, :, o:e], src3[:, :, c0 + o:c0 + e], wbuf, writes=[wbuf])


class Norm:
    def __init__(self, k, es, name, n, ones_bf, ps=None):
        self.k, self.n, self.ones = k, n, ones_bf
        self.sq = [k.sb(es, f"{name}_sq{i}", [128, n], BF16) for i in range(2)]
        self.ps = k.ps(es, f"{name}_ps", [128, n]) if ps is None else ps
        self.sd = k.sb(es, f"{name}_sd", [128, n], F32)
        self.rstd = self.sd
        self.i = 0

    def stats(self, srcs, src_bufs, dim, eps, ones_ap=None, np_=128):
        k, n = self.k, self.n
        ones_ap = self.ones[0:np_, 0:np_] if ones_ap is None else ones_ap
        nchunk = len(srcs)
        for c, (s, sbuf) in enumerate(zip(srcs, src_bufs)):
            sq = self.sq[self.i % 2]
            self.i += 1
            k.act.op(lambda s=s, sq=sq: k.nc.scalar.activation(out=sq[0:np_, :], in_=s, func=AF.Square),
                     reads=[sbuf], writes=[sq])
            k.pe.op(lambda sq=sq, c=c: k.nc.tensor.matmul(self.ps[0:np_, :], lhsT=ones_ap, rhs=sq[0:np_, :],
                                                          start=(c == 0), stop=(c == nchunk - 1)),
                    reads=[sq, self.ones], writes=[self.ps], inc=True)
        k.act.op(lambda: k.nc.scalar.activation(out=self.sd[0:np_, :], in_=self.ps[0:np_, :], func=AF.Ln,
                                                scale=1.0 / dim, bias=float(eps)),
                 reads=[self.ps], writes=[self.sd])
        k.act.op(lambda: k.nc.scalar.activation(out=self.sd[0:np_, :], in_=self.sd[0:np_, :], func=AF.Exp,
                                                scale=-0.5),
                 reads=[self.sd], writes=[self.sd])
        return self.rstd


VEC_LAYOUT = [
    ("ln1_g", 8), ("ln2_g", 8), ("final_g", 8), ("qn_g", 3), ("kvn_g", 2), ("on_g", 4),
    ("cw0", 44), ("cw1", 44), ("cw2", 44), ("cb", 44),
    ("gla_nb", 1), ("gla_ng", 1),
    ("rw_mu", 8), ("rw_1mmu", 8), ("rw_w0", 2), ("rw_a0", 2), ("rw_kk", 2), ("rw_ka", 2), ("rw_rk", 2),
    ("rw_lng", 2), ("rw_lnb", 2),
]
VOFF = {}
_o = 0
for _n, _c in VEC_LAYOUT:
    VOFF[_n] = (_o, _c)
    _o += _c
NV = _o


def _cols(v, n):
    return np.ascontiguousarray(np.asarray(v, np.float32).reshape(n, 128).T)


def phase_ffn(k, T, l, dr, cst, last):
    nc = k.nc
    nblk = T // TB
    with k.phase() as es:
        vec = k.sb(es, "f_vec", [128, NV], F32)
        k.sp.dma(vec[:, :], dr["vecs"][l], vec, writes=[vec])
        vnext = k.sb(es, "f_vnext", [128, 8], F32)
        if not last:
            o1 = VOFF["ln1_g"][0]
            k.sp.dma(vnext[:, :], dr["vecs"][l + 1][:, o1:o1 + 8], vnext, writes=[vnext])
        ones = k.sb(es, "f_ones", [128, 128], BF16)
        load_cast(k, ones, ones[:, :], cst["ones"])
        wup = k.sb(es, "f_wup", [128, 8, 2 * DFF], BF16)
        load_cast3(k, wup, wup[:, :, :], dr["ffn_w_up"][l], 0, 2 * DFF)
        wdn = k.sb(es, "f_wdn", [128, NFT, D], BF16)
        load_cast3(k, wdn, wdn[:, :, :], dr["ffn_w_down"][l], 0, D)
        xbs = [k.sb(es, f"f_xb{i}", [128, 8, TB], F32) for i in range(2)]
        hT = k.sb(es, "f_hT", [128, 8, TB], BF16)
        gT = [k.sb(es, f"f_gT{j}", [128, TB], BF16) for j in range(NFT)]
        halo = k.sb(es, "f_halo", [128, 2 * NFT, 2], F32)
        k.pool.op(lambda: nc.gpsimd.memset(halo[:, :, :], 0.0), writes=[halo])
        ug = [k.sb(es, f"f_ug{i}", [128, TB + 2], F32) for i in range(2)]
        ugh = [Buf(ug[i].ap[:, 0:2], f"ugh{i}") for i in range(2)]
        ugm = [Buf(ug[i].ap[:, 2:TB + 2], f"ugm{i}") for i in range(2)]
        a1 = [k.sb(es, f"f_a1{i}", [128, TB], F32) for i in range(2)]
        psu = [k.ps(es, f"f_psu{i}", [128, TB]) for i in range(4)]
        psd = [k.ps(es, f"f_psd{i}", [128, TB]) for i in range(2)]
        nrm = Norm(k, es, "f_n", TB, ones)
        o_g = VOFF["ln2_g"][0]
        o_w0, o_w1, o_w2, o_cb = (VOFF[n][0] for n in ("cw0", "cw1", "cw2", "cb"))
        o_fg = VOFF["final_g"][0]
        xT = dr["xT"]
        xTb = dr["xT_buf"]
        it = 0
        def norm_in(xb):
            rstd = nrm.stats([xb[:, c, :] for c in range(8)], [xb] * 8, D, EPS)
            for c in range(8):
                k.dve.op(lambda c=c, xb=xb: nc.vector.scalar_tensor_tensor(
                    out=hT[:, c, :], in0=xb[:, c, :], scalar=vec[:, o_g + c:o_g + c + 1], in1=rstd[:, :],
                    op0=ALU.mult, op1=ALU.mult), reads=[xb, vec, rstd], writes=[hT])

        k.sp.dma(xbs[0][:, :, :], xT[:, :, 0:TB], xbs[0], reads=[xTb], writes=[xbs[0]])
        norm_in(xbs[0])
        for b in range(nblk):
            t0 = b * TB
            xb = xbs[b % 2]
            if b + 1 < nblk:
                xn = xbs[(b + 1) % 2]
                k.sp.dma(xn[:, :, :], xT[:, :, t0 + TB:t0 + 2 * TB], xn, reads=[xTb], writes=[xn])
            for j in range(NFT):
                res = []
                for half in range(2):
                    jt = j + half * NFT
                    u = half
                    pu = psu[it % 4]
                    it += 1
                    col0 = half * DFF + j * 128
                    mm(k, pu, pu[:, :],
                       [(wup[:, c, col0:col0 + 128], hT[:, c, :]) for c in range(8)], [wup, hT])
                    k.pool.op(lambda u=u, jt=jt: nc.gpsimd.tensor_copy(out=ug[u][:, 0:2], in_=halo[:, jt, :]),
                              reads=[halo], writes=[ugh[u]])
                    k.act.op(lambda u=u, pu=pu: nc.scalar.activation(out=ug[u][:, 2:TB + 2], in_=pu[:, :],
                                                                     func=AF.Copy), reads=[pu], writes=[ugm[u]])
                    k.act.op(lambda u=u, jt=jt, pu=pu: nc.scalar.activation(
                        out=a1[u][:, :], in_=pu[:, :], func=AF.Identity,
                        scale=vec[:, o_w2 + jt:o_w2 + jt + 1], bias=vec[:, o_cb + jt:o_cb + jt + 1]),
                        reads=[pu, vec], writes=[a1[u]])
                    k.pool.op(lambda u=u, jt=jt: nc.gpsimd.tensor_copy(out=halo[:, jt, :], in_=ug[u][:, TB:TB + 2]),
                              reads=[ugm[u]], writes=[halo])
                    k.dve.op(lambda u=u, jt=jt: nc.vector.scalar_tensor_tensor(
                        out=a1[u][:, :], in0=ug[u][:, 1:TB + 1], scalar=vec[:, o_w1 + jt:o_w1 + jt + 1],
                        in1=a1[u][:, :], op0=ALU.mult, op1=ALU.add), reads=[ugm[u], ugh[u], vec, a1[u]], writes=[a1[u]])
                    k.dve.op(lambda u=u, jt=jt: nc.vector.scalar_tensor_tensor(
                        out=a1[u][:, :], in0=ug[u][:, 0:TB], scalar=vec[:, o_w0 + jt:o_w0 + jt + 1],
                        in1=a1[u][:, :], op0=ALU.mult, op1=ALU.add), reads=[ugm[u], ugh[u], vec, a1[u]], writes=[a1[u]])
                    res.append(a1[u])
                s = res[0]
                k.act.op(lambda s=s: nc.scalar.activation(out=s[:, :], in_=s[:, :], func=AF.Silu),
                         reads=[s], writes=[s])
                k.pool.op(lambda s=s, v=res[1], j=j: nc.gpsimd.tensor_tensor(
                    out=gT[j][:, :], in0=s[:, :], in1=v[:, :], op=ALU.mult), reads=[s, res[1]], writes=[gT[j]])
            if b + 1 < nblk:
                norm_in(xbs[(b + 1) % 2])
            for m in range(8):
                pd = psd[m % 2]
                mm(k, pd, pd[:, :], [(wdn[:, j, m * 128:(m + 1) * 128], gT[j][:, :]) for j in range(NFT)],
                   [wdn] + gT)
                k.dve.op(lambda m=m, pd=pd, xb=xb: nc.vector.tensor_tensor(out=xb[:, m, :], in0=pd[:, :],
                                                                           in1=xb[:, m, :], op=ALU.add),
                         reads=[pd, xb], writes=[xb])
            if not last:
                k.sp.dma(xT[:, :, t0:t0 + TB], xb[:, :, :], xb, reads=[xb], writes=[xTb])
                rstd = nrm.stats([xb[:, c, :] for c in range(8)], [xb] * 8, D, EPS)
                for c in range(8):
                    hn = gT[NFT - 8 + c]
                    k.dve.op(lambda c=c, hn=hn, xb=xb: nc.vector.scalar_tensor_tensor(
                        out=hn[:, :], in0=xb[:, c, :], scalar=vnext[:, c:c + 1], in1=rstd[:, :],
                        op0=ALU.mult, op1=ALU.mult), reads=[xb, vnext, rstd], writes=[hn])
                    k.sp.dma(dr["hT"][:, c, t0:t0 + TB], hn[:, :], hn, reads=[hn], writes=[dr["hT_buf"]])
            else:
                rstd = nrm.stats([xb[:, c, :] for c in range(8)], [xb] * 8, D, EPS)
                for c in range(8):
                    k.dve.op(lambda c=c, xb=xb: nc.vector.scalar_tensor_tensor(
                        out=xb[:, c, :], in0=xb[:, c, :], scalar=vec[:, o_fg + c:o_fg + c + 1], in1=rstd[:, :],
                        op0=ALU.mult, op1=ALU.mult), reads=[xb, vec, rstd], writes=[xb])
                k.sp.dma(dr["outT"][:, :, t0:t0 + TB], xb[:, :, :], xb, reads=[xb], writes=[dr["outT_buf"]])


MISC_LAYOUT = [("ones", 128), ("ones2", 128), ("ident", 128), ("cmask", 512), ("mask4", 512), ("bd", 256),
               ("hm", 4), ("smask", 128), ("imaskT", 128)]
MOFF = {}
_o = 0
for _n, _c in MISC_LAYOUT:
    MOFF[_n] = (_o, _o + _c)
    _o += _c
NMISC = _o


def make_misc():
    m = np.zeros((128, NMISC), np.float32)
    p = np.arange(128)[:, None]
    f = lambda n: np.arange(n)[None, :]
    m[:, slice(*MOFF["ones"])] = 1.0
    m[:, slice(*MOFF["ones2"])] = (p // 64 == f(128) // 64)
    m[:, slice(*MOFF["ident"])] = (p == f(128))
    m[:, slice(*MOFF["cmask"])] = (f(512) % 128 != 0)
    m[:, slice(*MOFF["mask4"])] = (p <= f(512) % 128)
    m[:, slice(*MOFF["bd"])] = (p // 32 == f(256) // 64)
    m[:, slice(*MOFF["hm"])] = (p // 32 == f(4))
    m[:, slice(*MOFF["smask"])] = (p < f(128))
    m[:, slice(*MOFF["imaskT"])] = (p > f(128))
    return m


def load_misc(k, es, name, cst, names, dtype):
    out = {}
    for n in names:
        a, b = MOFF[n]
        t = k.sb(es, f"{name}_{n}", [128, b - a], dtype)
        if dtype == BF16:
            load_cast(k, t, t[:, :], cst["misc"][:, a:b])
        else:
            k.sp.dma(t[:, :], cst["misc"][:, a:b], t, writes=[t])
        out[n] = t
    return out


def phase_gla(k, T, l, dr, cst):
    nc = k.nc
    C = 128
    NCH = TB // C
    w_in = dr["w_in"]
    G0 = 704
    with k.phase() as es:
        vec = k.sb(es, "g_vec", [128, NV], F32)
        k.sp.dma(vec[:, :], dr["vecs"][l], vec, writes=[vec])
        cb = load_misc(k, es, "g", cst, ["ones2", "ident", "mask4", "bd"], BF16)
        cf = load_misc(k, es, "gf", cst, ["cmask", "hm"], F32)
        ones2, ident, mask4, bd, cmask, hm = cb["ones2"], cb["ident"], cb["mask4"], cb["bd"], cf["cmask"], cf["hm"]
        wG = k.sb(es, "g_wG", [128, 8, 784], BF16)
        load_cast3(k, wG, wG[:, :, 0:256], w_in[l], G0, G0 + 256)
        load_cast3(k, wG, wG[:, :, 256:512], w_in[l], G0 + 528, G0 + 784)
        load_cast3(k, wG, wG[:, :, 512:768], w_in[l], G0 + 256, G0 + 512)
        load_cast3(k, wG, wG[:, :, 768:784], w_in[l], G0 + 512, G0 + 528)
        wgk = k.sb(es, "g_wgk", [16, 128], BF16)
        load_cast(k, wgk, wgk[:, :], dr["gla_w_gk"][l])
        nb = k.sb(es, "g_nb", [128, 1], F32)
        o_nb, o_ng = VOFF["gla_nb"][0], VOFF["gla_ng"][0]
        k.dve.op(lambda: nc.vector.tensor_scalar(out=nb[:, :], in0=vec[:, o_nb:o_nb + 1], scalar1=-1.0, scalar2=None,
                                                 op0=ALU.mult), reads=[vec], writes=[nb])
        hT = [k.sb(es, f"g_hT{i}", [128, 8, TB], BF16) for i in range(2)]
        q32 = k.sb(es, "g_q32", [128, TB], F32)
        k32 = k.sb(es, "g_k32", [128, TB], F32)
        glo = k.sb(es, "g_glo", [16, TB], BF16)
        Lt = k.sb(es, "g_L", [128, TB], F32)
        Bc = k.sb(es, "g_Bc", [128, TB], F32)
        eb = k.sb(es, "g_eb", [128, TB], F32)
        enb = k.sb(es, "g_enb", [128, TB], F32)
        kef = k.sb(es, "g_kef", [128, TB], F32)
        nbl = k.sb(es, "g_nbl", [128, NCH], F32)
        dec = k.sb(es, "g_dec", [128, NCH], F32)
        qth = [k.sb(es, f"g_qth{i}", [128, TB], BF16) for i in range(4)]
        kt = k.sb(es, "g_kt", [128, TB], BF16)
        kend = k.sb(es, "g_kend", [128, TB], BF16)
        kendT = k.sb(es, "g_kendT", [128, NCH, 128], BF16)
        vT = k.sb(es, "g_vT", [128, NCH, 256], BF16)
        sgo = [k.sb(es, f"g_sgo{i}", [128, TB], F32) for i in range(2)]
        attnT = k.sb(es, "g_attnT", [128, 4 * C], BF16)
        S4 = k.sb(es, "g_S4", [128, 256], F32)
        Sb = k.sb(es, "g_Sb", [128, 256], BF16)
        kvm = k.sb(es, "g_kvm", [128, 256], F32)
        oacc = [k.sb(es, f"g_oacc{i}", [128, TB], F32) for i in range(2)]
        yo = [k.sb(es, f"g_yo{i}", [128, TB], BF16) for i in range(2)]
        tmp = k.sb(es, "g_tmp", [128, TB], F32)
        tmp2 = k.sb(es, "g_tmp2", [128, TB], F32)
        sets = [(qth, kt, kendT, vT, dec, sgo),
                ([k.sb(es, f"g_qthB{i}", [128, TB], BF16) for i in range(4)], k.sb(es, "g_ktB", [128, TB], BF16),
                 k.sb(es, "g_kendTB", [128, NCH, 128], BF16), k.sb(es, "g_vTB", [128, NCH, 256], BF16),
                 k.sb(es, "g_decB", [128, NCH], F32), [k.sb(es, f"g_sgoB{i}", [128, TB], F32) for i in range(2)])]
        k.pool.op(lambda: nc.gpsimd.memset(S4[:, :], 0.0), writes=[S4])
        k.pool.op(lambda: nc.gpsimd.memset(Sb[:, :], 0.0), writes=[Sb])
        nrm = Norm(k, es, "g_n", TB, ones2)
        ps_p = [k.ps(es, f"g_psp{i}", [128, TB]) for i in range(2)]
        ps_at = k.ps(es, "g_psat", [128, 4 * C])
        ps_o = [k.ps(es, f"g_pso{i}", [128, C]) for i in range(2)]
        ps_kv = k.ps(es, "g_pskv", [128, 256])
        ps_tr = k.ps(es, "g_pstr", [128, 128])
        ip = 0

        def proj(pairs, reads, m=128, n=TB):
            nonlocal ip
            p = ps_p[ip % 2]
            ip += 1
            mm(k, p, p[0:m, 0:n], pairs, reads)
            return p

        qs = float(GDK ** -0.5)
        def prepG(b):
            t0 = b * TB
            h = hT[b % 2]
            qth, kt, kendT, vT, dec, sgo = sets[b % 2]
            k.sp.dma(h[:, :, :], dr["hT"][:, :, t0:t0 + TB], h, reads=[dr["hT_buf"]], writes=[h])
            yield
            p = proj([(wG[:, c, 0:128], h[:, c, :]) for c in range(8)], [wG, h])
            k.act.op(lambda p=p: nc.scalar.activation(out=q32[:, :], in_=p[:, :], func=AF.Copy), reads=[p], writes=[q32])
            yield
            p = proj([(wG[:, c, 128:256], h[:, c, :]) for c in range(8)], [wG, h])
            k.act.op(lambda p=p: nc.scalar.activation(out=k32[:, :], in_=p[:, :], func=AF.Copy), reads=[p], writes=[k32])
            yield
            p = proj([(wG[:, c, 768:784], h[:, c, :]) for c in range(8)], [wG, h], m=16)
            k.act.op(lambda p=p: nc.scalar.activation(out=glo[:, :], in_=p[0:16, :], func=AF.Copy), reads=[p], writes=[glo])
            yield
            for i in range(2):
                p = proj([(wG[:, c, 256 + i * 128:384 + i * 128], h[:, c, :]) for c in range(8)], [wG, h])
                k.act.op(lambda p=p, i=i: nc.scalar.activation(out=sgo[i][:, :], in_=p[:, :], func=AF.Silu),
                         reads=[p], writes=[sgo[i]])
                yield
            for c in range(NCH):
                p = proj([(h[:, c8, c * C:(c + 1) * C], wG[:, c8, 512:768]) for c8 in range(8)], [wG, h], n=256)
                k.act.op(lambda p=p, c=c: nc.scalar.activation(out=vT[:, c, :], in_=p[:, 0:256], func=AF.Copy),
                         reads=[p], writes=[vT])
                yield
            p = proj([(wgk[:, :], glo[:, :])], [wgk, glo])
            k.act.op(lambda p=p: nc.scalar.activation(out=Lt[:, :], in_=p[:, :], func=AF.Exp, scale=-1.0, bias=nb[:, 0:1]),
                     reads=[p, nb], writes=[Lt])
            k.act.op(lambda: nc.scalar.activation(out=Lt[:, :], in_=Lt[:, :], func=AF.Ln, bias=1.0), reads=[Lt], writes=[Lt])
            yield
            k.dve.op(lambda: nc.vector.tensor_tensor_scan(out=Bc[:, :], data0=cmask[:, :], data1=Lt[:, :], initial=0.0,
                                                          op0=ALU.mult, op1=ALU.add), reads=[cmask, Lt], writes=[Bc])
            yield
            k.act.op(lambda: nc.scalar.activation(out=eb[:, :], in_=Bc[:, :], func=AF.Exp, scale=-1.0 / 16), reads=[Bc], writes=[eb])
            k.act.op(lambda: nc.scalar.activation(out=enb[:, :], in_=Bc[:, :], func=AF.Exp, scale=1.0 / 16), reads=[Bc], writes=[enb])
            yield
            k.dve.op(lambda: nc.vector.tensor_scalar(out=nbl[:, :], in0=Bc[:, C - 1::C], scalar1=-1.0 / 16, scalar2=None,
                                                     op0=ALU.mult), reads=[Bc], writes=[nbl])
            k.act.op(lambda: nc.scalar.activation(out=dec[:, :], in_=nbl[:, :], func=AF.Exp), reads=[nbl], writes=[dec])
            yield
            for c in range(NCH):
                k.act.op(lambda c=c: nc.scalar.activation(out=kef[:, c * C:(c + 1) * C], in_=Bc[:, c * C:(c + 1) * C],
                                                          func=AF.Exp, scale=1.0 / 16, bias=nbl[:, c:c + 1]),
                         reads=[Bc, nbl], writes=[kef])
                yield
            k.dve.op(lambda: nc.vector.tensor_tensor(out=tmp[:, :], in0=q32[:, :], in1=eb[:, :], op=ALU.mult),
                     reads=[q32, eb], writes=[tmp])
            for hh in range(4):
                k.dve.op(lambda hh=hh: nc.vector.tensor_scalar(out=qth[hh][:, :], in0=tmp[:, :], scalar1=hm[:, hh:hh + 1],
                                                               scalar2=qs, op0=ALU.mult, op1=ALU.mult),
                         reads=[tmp, hm], writes=[qth[hh]])
                yield
            k.dve.op(lambda: nc.vector.tensor_tensor(out=kt[:, :], in0=k32[:, :], in1=enb[:, :], op=ALU.mult),
                     reads=[k32, enb], writes=[kt])
            yield
            k.dve.op(lambda: nc.vector.tensor_tensor(out=kend[:, :], in0=k32[:, :], in1=kef[:, :], op=ALU.mult),
                     reads=[k32, kef], writes=[kend])
            yield
            for c in range(NCH):
                cs = slice(c * C, (c + 1) * C)
                mm(k, ps_tr, ps_tr[:, :], [(kend[:, cs], ident[:, :])], [kend, ident])
                k.act.op(lambda c=c: nc.scalar.activation(out=kendT[:, c, :], in_=ps_tr[:, :], func=AF.Copy),
                         reads=[ps_tr], writes=[kendT])
                yield
        def drain(g):
            if g is not None:
                for _ in g:
                    pass

        def step():
            nonlocal gp
            if gp is not None:
                try:
                    next(gp)
                except StopIteration:
                    gp = None

        gp = None
        drain(prepG(0))
        for b in range(T // TB):
            t0 = b * TB
            qth, kt, kendT, vT, dec, sgo = sets[b % 2]
            gp = prepG(b + 1) if (b + 1) * TB < T else None
            for c in range(NCH):
                cs = slice(c * C, (c + 1) * C)
                step()
                for hh in range(4):
                    k.pe.op(lambda hh=hh, cs=cs: nc.tensor.matmul(ps_at[:, hh * C:(hh + 1) * C], lhsT=kt[:, cs],
                                                                  rhs=qth[hh][:, cs], start=True, stop=True),
                            reads=[kt, qth[hh]], writes=[ps_at], inc=(hh == 3))
                k.dve.op(lambda: nc.vector.tensor_tensor(out=attnT[:, :], in0=ps_at[:, :], in1=mask4[:, :], op=ALU.mult),
                         reads=[ps_at, mask4], writes=[attnT])
                step()
                for pr in range(2):
                    po = ps_o[pr]
                    for hl in range(2):
                        hh = pr * 2 + hl
                        osl = po[hl * 64:(hl + 1) * 64, :]
                        k.pe.op(lambda hh=hh, osl=osl, c=c: nc.tensor.matmul(
                            osl, lhsT=vT[:, c, hh * 64:(hh + 1) * 64], rhs=attnT[:, hh * C:(hh + 1) * C],
                            start=True, stop=False), reads=[vT, attnT], writes=[po], inc=False)
                        k.pe.op(lambda hh=hh, osl=osl, cs=cs: nc.tensor.matmul(
                            osl, lhsT=Sb[:, hh * 64:(hh + 1) * 64], rhs=qth[hh][:, cs], start=False, stop=True),
                            reads=[Sb, qth[hh]], writes=[po], inc=True)
                    k.act.op(lambda pr=pr, po=po, cs=cs: nc.scalar.activation(out=oacc[pr][:, cs], in_=po[:, :],
                                                                              func=AF.Copy), reads=[po], writes=[oacc[pr]])
                step()
                mm(k, ps_kv, ps_kv[:, :], [(kendT[:, c, :], vT[:, c, :])], [kendT, vT])
                k.dve.op(lambda: nc.vector.tensor_tensor(out=kvm[:, :], in0=ps_kv[:, :], in1=bd[:, :], op=ALU.mult),
                         reads=[ps_kv, bd], writes=[kvm])
                k.dve.op(lambda c=c: nc.vector.scalar_tensor_tensor(out=S4[:, :], in0=S4[:, :], scalar=dec[:, c:c + 1],
                                                                    in1=kvm[:, :], op0=ALU.mult, op1=ALU.add),
                         reads=[S4, dec, kvm], writes=[S4])
                k.act.op(lambda: nc.scalar.activation(out=Sb[:, :], in_=S4[:, :], func=AF.Copy), reads=[S4], writes=[Sb])
                step()
                step()
            for pr in range(2):
                rstd = nrm.stats([oacc[pr][:, :]], [oacc[pr]], GDV, EPS)
                k.dve.op(lambda pr=pr: nc.vector.tensor_tensor(out=tmp2[:, :], in0=oacc[pr][:, :], in1=rstd[:, :],
                                                               op=ALU.mult), reads=[oacc[pr], rstd], writes=[tmp2])
                k.dve.op(lambda pr=pr: nc.vector.scalar_tensor_tensor(
                    out=yo[pr][:, :], in0=tmp2[:, :], scalar=vec[:, o_ng:o_ng + 1], in1=sgo[pr][:, :],
                    op0=ALU.mult, op1=ALU.mult), reads=[tmp2, vec, sgo[pr]], writes=[yo[pr]])
                k.sp.dma(dr["yT"][:, 4 + pr, t0:t0 + TB], yo[pr][:, :], yo[pr], reads=[yo[pr]], writes=[dr["yT_buf"]])
            drain(gp)
            gp = None


def TT(e, out, a, b, op, R, W):
    e.op(lambda: e.eng.tensor_tensor(out=out, in0=a, in1=b, op=op), reads=R, writes=W)


def TS(e, out, a, s1, s2, op0, op1, R, W):
    e.op(lambda: e.eng.tensor_scalar(out=out, in0=a, scalar1=s1, scalar2=s2, op0=op0, op1=op1), reads=R, writes=W)


def STT(k, out, a, sc, b, op0, op1, R, W):
    k.dve.op(lambda: k.nc.vector.scalar_tensor_tensor(out=out, in0=a, scalar=sc, in1=b, op0=op0, op1=op1),
             reads=R, writes=W)


def ACTF(k, out, in_, func, R, W, scale=1.0, bias=0.0):
    k.act.op(lambda: k.nc.scalar.activation(out=out, in_=in_, func=func, scale=scale, bias=bias), reads=R, writes=W)


def MM(k, out, lhsT, rhs, R, W, start=True, stop=True, inc=True):
    k.pe.op(lambda: k.nc.tensor.matmul(out, lhsT=lhsT, rhs=rhs, start=start, stop=stop), reads=R, writes=W, inc=inc)


class _Stop(Exception):
    pass


STOP = [None]


def chk(n):
    if STOP[0] == n:
        raise _Stop()


def phase_rwkv(k, T, l, dr, cst):
    try:
        _phase_rwkv(k, T, l, dr, cst)
    except _Stop:
        pass


NS = 3


def _phase_rwkv(k, T, l, dr, cst):
    nc = k.nc
    C = 128
    NCH = TB // C
    R0 = 704 + 784
    DEC = float(np.exp(-0.5))
    with k.phase() as es:
        vec = k.sb(es, "r_vec", [128, NV], F32)
        k.sp.dma(vec[:, :], dr["vecs"][l], vec, writes=[vec])
        cb = load_misc(k, es, "r", cst, ["ones2", "ident"], BF16)
        cf = load_misc(k, es, "rf", cst, ["cmask", "smask", "mask4", "imaskT", "ident"], F32)
        ones2, ident, cmask = cb["ones2"], cb["ident"], cf["cmask"]
        m_si = k.sb(es, "r_msi", [128, 2, 256], BF16)
        m_tt = k.sb(es, "r_mtt", [128, 2, 128], BF16)
        for hl in range(2):
            k.pool.op(lambda hl=hl: nc.gpsimd.tensor_copy(out=m_si[:, hl, 0:128], in_=cf["smask"][:, :]),
                      reads=[cf["smask"]], writes=[m_si])
            k.pool.op(lambda hl=hl: nc.gpsimd.tensor_copy(out=m_si[:, hl, 128:256], in_=cf["mask4"][:, 0:128]),
                      reads=[cf["mask4"]], writes=[m_si])
            k.pool.op(lambda hl=hl: nc.gpsimd.tensor_copy(out=m_tt[:, hl, :], in_=cf["imaskT"][:, :]),
                      reads=[cf["imaskT"]], writes=[m_tt])
        I2 = k.sb(es, "r_I2", [128, 64], F32)
        k.pool.op(lambda: nc.gpsimd.tensor_copy(out=I2[0:64, :], in_=cf["ident"][0:64, 0:64]), reads=[cf["ident"]], writes=[I2])
        k.pool.op(lambda: nc.gpsimd.tensor_copy(out=I2[64:128, :], in_=cf["ident"][64:128, 64:128]), reads=[cf["ident"]], writes=[I2])
        wR = k.sb(es, "r_wR", [128, 8, 1024], BF16)
        load_cast3(k, wR, wR[:, :, :], dr["w_in"][l], R0, R0 + 1024)
        wl_w = k.sb(es, "r_wlw", [128, 256], BF16)
        wl_a = k.sb(es, "r_wla", [128, 256], BF16)
        k.pool.op(lambda: nc.gpsimd.memset(wl_w[:, :], 0.0), writes=[wl_w])
        k.pool.op(lambda: nc.gpsimd.memset(wl_a[:, :], 0.0), writes=[wl_a])
        load_cast(k, wl_w, wl_w[0:64, :], dr["rwkv_w2"][l])
        load_cast(k, wl_a, wl_a[64:128, :], dr["rwkv_a2"][l])
        hm2 = k.sb(es, "r_hm2", [128, 2], F32)
        nhm2 = k.sb(es, "r_nhm2", [128, 2], F32)
        I2h = [k.sb(es, f"r_I2h{i}", [128, 64], F32) for i in range(2)]
        for i in range(2):
            k.pool.op(lambda i=i: nc.gpsimd.memset(I2h[i][:, :], 0.0), writes=[I2h[i]])
            k.pool.op(lambda i=i: nc.gpsimd.tensor_copy(out=I2h[i][i * 64:(i + 1) * 64, :],
                                                        in_=cf["ident"][i * 64:(i + 1) * 64, i * 64:(i + 1) * 64]),
                      reads=[cf["ident"]], writes=[I2h[i]])
        k.pool.op(lambda: nc.gpsimd.memset(hm2[:, :], 0.0), writes=[hm2])
        k.pool.op(lambda: nc.gpsimd.memset(hm2[0:64, 0:1], 1.0), writes=[hm2])
        k.pool.op(lambda: nc.gpsimd.memset(hm2[64:128, 1:2], 1.0), writes=[hm2])
        TS(k.dve, nhm2[:, :], hm2[:, :], -1.0, None, ALU.mult, ALU.bypass, [hm2], [nhm2])
        wg2 = k.sb(es, "r_wg2", [128, 256], BF16)
        load_cast(k, wg2, wg2[:, :], dr["rwkv_g2"][l])
        o_mu, o_w0, o_a0, o_kk, o_ka, o_rk, o_lg, o_lb = (VOFF[n][0] for n in (
            "rw_mu", "rw_w0", "rw_a0", "rw_kk", "rw_ka", "rw_rk", "rw_lng", "rw_lnb"))
        omm = k.sb(es, "r_omm", [128, 8], F32)
        omka = k.sb(es, "r_omka", [128, 2], F32)
        TS(k.dve, omm[:, :], vec[:, o_mu:o_mu + 8], -1.0, 1.0, ALU.mult, ALU.add, [vec], [omm])
        TS(k.dve, omka[:, :], vec[:, o_ka:o_ka + 2], -1.0, 1.0, ALU.mult, ALU.add, [vec], [omka])

        hTs = [k.sb(es, f"r_hT{i}", [128, 8, TB + 1], BF16) for i in range(2)]
        def f32t(n): return k.sb(es, "r_" + n, [128, TB], F32)
        def bft(n): return k.sb(es, "r_" + n, [128, TB], BF16)
        tprev = f32t("tprev")
        xr, xk, xv = [f32t(f"xr{i}") for i in range(2)], [f32t(f"xk{i}") for i in range(2)], [f32t(f"xv{i}") for i in range(2)]
        xlo = bft("xlo")
        sgl = bft("sgl")
        gate = [f32t(f"gate{i}") for i in range(2)]
        bon = [f32t(f"bon{i}") for i in range(2)]
        aa, kkt, kmod, cc, t1, t2, t3 = f32t("aa"), f32t("kk"), f32t("kmod"), f32t("cc"), f32t("t1"), f32t("t2"), f32t("t3")
        tb1 = bft("tb1")
        ar = [[k.sb(es, f"r_ar{i}{j}", [128, NCH, 256], BF16) for j in range(2)] for i in range(2)]
        Xw_ = [[k.sb(es, f"r_Xw{p}{i}", [128, 128], BF16) for i in range(2)] for p in range(NS)]
        for p_ in range(NS):
            for i in range(2):
                k.pool.op(lambda i=i, p_=p_: nc.gpsimd.memset(Xw_[p_][i][:, :], 0.0), writes=[Xw_[p_][i]])
        bt = [bft(f"bt{i}") for i in range(2)]
        ktl = [bft(f"ktl{i}") for i in range(2)]
        Bp = [bft(f"Bp{i}") for i in range(2)]
        Kp = [bft(f"Kp{i}") for i in range(2)]
        vb = [bft(f"vb{i}") for i in range(2)]
        nbl = k.sb(es, "r_nbl", [128, NCH], F32)
        pC = [k.sb(es, f"r_pC{i}", [128, NCH], F32) for i in range(2)]
        TM_ = [k.sb(es, f"r_TM{p}", [128, 4, 128], BF16) for p in range(NS)]
        AbRb_ = [k.sb(es, f"r_AbRb{p}", [128, 2, 256], BF16) for p in range(NS)]
        AkRk_ = [k.sb(es, f"r_AkRk{p}", [128, 2, 256], BF16) for p in range(NS)]
        PP_ = [[k.sb(es, f"r_PP{p}{i}", [128, 2, 256], BF16) for i in range(2)] for p in range(NS)]
        X_ = [k.sb(es, f"r_X{p}", [128, 2, 128], BF16) for p in range(NS)]
        QpT = [[bft(f"QpT{i}{j}") for j in range(2)] for i in range(2)]
        Y0T = [f32t(f"Y0T{i}") for i in range(2)]
        MT = [[k.sb(es, f"r_MT{i}{j}", [128, NCH, 64], BF16) for j in range(2)] for i in range(2)]
        Gm = [k.sb(es, f"r_G{i}", [128, NCH, 64], F32) for i in range(2)]
        Hb = [k.sb(es, f"r_H{i}", [128, 64], BF16) for i in range(2)]
        yacc = [f32t(f"yacc{i}") for i in range(2)]
        yo = [bft(f"yo{i}") for i in range(2)]
        o1, ob1 = f32t("o1"), bft("ob1")
        sets = [(gate, bon, vb, ar, bt, ktl, pC, Bp, Kp)]
        sets.append(([f32t(f"gateB{i}") for i in range(2)], [f32t(f"bonB{i}") for i in range(2)],
                     [bft(f"vbB{i}") for i in range(2)],
                     [[k.sb(es, f"r_arB{i}{j}", [128, NCH, 256], BF16) for j in range(2)] for i in range(2)],
                     [bft(f"btB{i}") for i in range(2)], [bft(f"ktlB{i}") for i in range(2)],
                     [k.sb(es, f"r_pCB{i}", [128, NCH], F32) for i in range(2)],
                     [bft(f"BpB{i}") for i in range(2)], [bft(f"KpB{i}") for i in range(2)]))
        for i in range(2):
            k.pool.op(lambda i=i: nc.gpsimd.memset(Hb[i][:, :], 0.0), writes=[Hb[i]])
        ps_p = [k.ps(es, f"r_psp{i}", [128, TB]) for i in range(2)]
        nrm = Norm(k, es, "r_n", TB, ones2, ps=ps_p[0])
        ps_x_ = [k.ps(es, f"r_bx{p}", [128, TB]) for p in range(NS)]
        ps_pp_ = [k.ps(es, f"r_pspp{p}", [128, 2, 256]) for p in range(NS)]
        banks = [ps_p[0], ps_p[1]]
        ip = 0
        isx = 0

        def bank():
            nonlocal isx
            isx += 1
            return banks[isx % 2]

        def proj(pairs, reads, n=TB):
            nonlocal ip
            p = ps_p[ip % 2]
            ip += 1
            mm(k, p, p[:, 0:n], pairs, reads)
            return p

        def prepR(b):
            t0 = b * TB
            h = hTs[b % 2]
            gate, bon, vb, ar, bt, ktl, pC, Bp, Kp = sets[b % 2]
            if b == 0:
                k.pool.op(lambda h=h: nc.gpsimd.memset(h[:, :, 0:1], 0.0), writes=[h])
                k.sp.dma(h[:, :, 1:TB + 1], dr["hT"][:, :, 0:TB], h, reads=[dr["hT_buf"]], writes=[h])
            else:
                k.sp.dma(h[:, :, :], dr["hT"][:, :, t0 - 1:t0 + TB], h, reads=[dr["hT_buf"]], writes=[h])

            def mixed(j, out, func=AF.Identity, out_bufs=None):
                cols = slice(j * 128, (j + 1) * 128)
                pp = proj([(wR[:, c, cols], h[:, c, 0:TB]) for c in range(8)], [wR, h])
                ACTF(k, tprev[:, :], pp[:, :], AF.Copy if False else AF.Identity, [pp, vec], [tprev],
                     scale=vec[:, o_mu + j:o_mu + j + 1])
                pc_ = proj([(wR[:, c, cols], h[:, c, 1:TB + 1]) for c in range(8)], [wR, h])
                STT(k, t1[:, :], pc_[:, :], omm[:, j:j + 1], tprev[:, :], ALU.mult, ALU.add, [pc_, omm, tprev], [t1])
                return t1

            yield
            mixed(6, None)
            ACTF(k, xlo[0:64, :], t1[0:64, :], AF.Tanh, [t1], [xlo])
            ACTF(k, xlo[64:128, :], t1[64:128, :], AF.Copy, [t1], [xlo])
            yield
            mixed(7, None)
            ACTF(k, sgl[:, :], t1[:, :], AF.Sigmoid, [t1], [sgl])
            yield
            for j in range(2):
                mixed(j, None)
                ACTF(k, xr[j][:, :], t1[:, :], AF.Copy, [t1], [xr[j]])
                yield
                mixed(2 + j, None)
                ACTF(k, xk[j][:, :], t1[:, :], AF.Copy, [t1], [xk[j]])
                yield
                mixed(4 + j, None)
                ACTF(k, xv[j][:, :], t1[:, :], AF.Copy, [t1], [xv[j]])
                yield
            for pr in range(2):
                pcols = slice(pr * 128, (pr + 1) * 128)
                r_, k_, v_ = xr[pr], xk[pr], xv[pr]
                p = proj([(wg2[:, pcols], sgl[:, :])], [wg2, sgl])
                ACTF(k, gate[pr][:, :], p[:, :], AF.Copy, [p], [gate[pr]])
                yield
                p = proj([(wl_a[:, pcols], xlo[:, :])], [wl_a, xlo])
                ACTF(k, aa[:, :], p[:, :], AF.Sigmoid, [p, vec], [aa], bias=vec[:, o_a0 + pr:o_a0 + pr + 1])
                yield
                p = proj([(wl_w[:, pcols], xlo[:, :])], [wl_w, xlo])
                ACTF(k, t2[:, :], p[:, :], AF.Sigmoid, [p, vec], [t2], bias=vec[:, o_w0 + pr:o_w0 + pr + 1])
                TS(k.dve, t2[:, :], t2[:, :], DEC, None, ALU.mult, ALU.bypass, [t2], [t2])
                k.dve.op(lambda: nc.vector.tensor_tensor_scan(out=cc[:, :], data0=cmask[:, :], data1=t2[:, :], initial=0.0,
                                                              op0=ALU.mult, op1=ALU.add), reads=[cmask, t2], writes=[cc])
                yield
                TS(k.dve, kkt[:, :], k_[:, :], vec[:, o_kk + pr:o_kk + pr + 1], None, ALU.mult, ALU.bypass, [k_, vec], [kkt])
                rstd = nrm.stats([kkt[:, :]], [kkt], 1.0, 1e-24)
                TT(k.dve, kkt[:, :], kkt[:, :], rstd[:, :], ALU.mult, [kkt, rstd], [kkt])
                yield
                TS(k.dve, t3[:, :], aa[:, :], vec[:, o_ka + pr:o_ka + pr + 1], omka[:, pr:pr + 1], ALU.mult, ALU.add,
                   [aa, vec, omka], [t3])
                TT(k.dve, kmod[:, :], k_[:, :], t3[:, :], ALU.mult, [k_, t3], [kmod])
                yield
                STT(k, tb1[:, :], r_[:, :], vec[:, o_rk + pr:o_rk + pr + 1], kmod[:, :], ALU.mult, ALU.mult,
                    [r_, vec, kmod], [tb1])
                p = proj([(ones2[:, :], tb1[:, :])], [ones2, tb1])
                TT(k.dve, bon[pr][:, :], p[:, :], v_[:, :], ALU.mult, [p, v_], [bon[pr]])
                ACTF(k, vb[pr][:, :], v_[:, :], AF.Copy, [v_], [vb[pr]])
                yield
                ACTF(k, t3[:, :], cc[:, :], AF.Exp, [cc], [t3], scale=-1.0)
                for c in range(NCH):
                    for hl in range(2):
                        STT(k, ar[pr][hl][:, c, 128:256], r_[:, c * C:(c + 1) * C], hm2[:, hl:hl + 1],
                            t3[:, c * C:(c + 1) * C], ALU.mult, ALU.mult, [r_, t3, hm2], [ar[pr][hl]])
                    yield
                TT(k.dve, t3[:, :], cc[:, :], t2[:, :], ALU.subtract, [cc, t2], [t3])
                ACTF(k, t3[:, :], t3[:, :], AF.Exp, [t3], [t3], scale=-1.0)
                for c in range(NCH):
                    for hl in range(2):
                        STT(k, ar[pr][hl][:, c, 0:128], kkt[:, c * C:(c + 1) * C], nhm2[:, hl:hl + 1],
                            t3[:, c * C:(c + 1) * C], ALU.mult, ALU.mult, [kkt, t3, nhm2], [ar[pr][hl]])
                    yield
                TT(k.dve, aa[:, :], aa[:, :], kkt[:, :], ALU.mult, [aa, kkt], [aa])
                ACTF(k, t3[:, :], cc[:, :], AF.Exp, [cc], [t3], scale=1.0)
                TT(k.dve, bt[pr][:, :], aa[:, :], t3[:, :], ALU.mult, [aa, t3], [bt[pr]])
                TT(k.dve, ktl[pr][:, :], kmod[:, :], t3[:, :], ALU.mult, [kmod, t3], [ktl[pr]])
                yield
                TS(k.dve, nbl[:, :], cc[:, C - 1::C], -1.0, None, ALU.mult, ALU.bypass, [cc], [nbl])
                ACTF(k, pC[pr][:, :], nbl[:, :], AF.Exp, [nbl], [pC[pr]])
                for c in range(NCH):
                    ACTF(k, t3[:, c * C:(c + 1) * C], cc[:, c * C:(c + 1) * C], AF.Exp, [cc, nbl], [t3], scale=1.0,
                         bias=nbl[:, c:c + 1])
                TT(k.dve, Bp[pr][:, :], aa[:, :], t3[:, :], ALU.mult, [aa, t3], [Bp[pr]])
                TT(k.dve, Kp[pr][:, :], kmod[:, :], t3[:, :], ALU.mult, [kmod, t3], [Kp[pr]])

                yield

        def drain(g):
            if g is not None:
                for _ in g:
                    pass

        drain(prepR(0))
        for b in range(T // TB):
            t0 = b * TB
            gate, bon, vb, ar, bt, ktl, pC, Bp, Kp = sets[b % 2]
            gp = prepR(b + 1) if (b + 1) * TB < T else None
            hs = [slice(0, 64), slice(64, 128)]

            def stageA(pr, c, sid):
                cs = slice(c * C, (c + 1) * C)
                TM, AbRb, AkRk, PP, X, Xw = TM_[sid], AbRb_[sid], AkRk_[sid], PP_[sid], X_[sid], Xw_[sid]
                ps_x, ps_pp = ps_x_[sid], ps_pp_[sid]
                s_ = bank()
                MM(k, s_[:, 0:128], ar[pr][0][:, c, 0:128], ident[:, :], [ar[pr][0], ident], [s_], stop=False, inc=False)
                MM(k, s_[:, 0:128], ar[pr][1][:, c, 0:128], ident[:, :], [ar[pr][1], ident], [s_], start=False, inc=False)
                for n_, src in enumerate((Bp[pr][:, cs], Kp[pr][:, cs], vb[pr][:, cs])):
                    MM(k, s_[:, (n_ + 1) * 128:(n_ + 2) * 128], src, ident[:, :], [Bp[pr], Kp[pr], vb[pr], ident],
                       [s_], inc=(n_ == 2))
                ACTF(k, TM[:, :, :], s_[:, :], AF.Copy, [s_], [TM])
                yield
                s1 = bank()
                for hl in range(2):
                    MM(k, s1[:, hl * 256:(hl + 1) * 256], bt[pr][:, cs], ar[pr][hl][:, c, :], [bt[pr], ar[pr][hl]],
                       [s1], inc=(hl == 1))
                TT(k.dve, AbRb[:, :, :], s1[:, :], m_si[:, :, :], ALU.mult, [s1, m_si], [AbRb])
                yield
                s2 = bank()
                for hl in range(2):
                    MM(k, s2[:, hl * 256:(hl + 1) * 256], ktl[pr][:, cs], ar[pr][hl][:, c, :], [ktl[pr], ar[pr][hl]],
                       [s2], inc=(hl == 1))
                TT(k.dve, AkRk[:, :, :], s2[:, :], m_si[:, :, :], ALU.mult, [s2, m_si], [AkRk])
                yield
                s3 = bank()
                for hl in range(2):
                    MM(k, s3[:, hl * 128:(hl + 1) * 128], ar[pr][hl][:, c, 0:128], bt[pr][:, cs], [bt[pr], ar[pr][hl]],
                       [s3], inc=(hl == 1))
                P0 = PP[0]
                TT(k.dve, P0[:, :, 0:128], s3[:, 0:256], m_tt[:, :, :], ALU.mult, [s3, m_tt], [P0])
                k.pool.op(lambda P0=P0: nc.gpsimd.tensor_copy(out=P0[:, :, 128:256], in_=AbRb[:, :, 0:128]),
                          reads=[AbRb], writes=[P0])
                yield
                for hl in range(2):
                    MM(k, ps_x[:, hl * 128 + 64:(hl + 1) * 128], AkRk[:, hl, 0:128], TM[:, 3, hs[hl]], [AkRk, TM],
                       [ps_x], inc=(hl == 1))
                for hl in range(2):
                    ACTF(k, X[:, hl, 0:64], TM[:, 0, hs[hl]], AF.Copy, [TM], [X])
                    ACTF(k, X[:, hl, 64:128], ps_x[:, hl * 128 + 64:(hl + 1) * 128], AF.Copy, [ps_x], [X])
                yield
                for lv in range(7):
                    Pc = PP[lv % 2]
                    for hl in range(2):
                        MM(k, ps_x[:, hl * 128:(hl + 1) * 128], Pc[:, hl, 128:256], X[:, hl, :], [Pc, X], [ps_x],
                           inc=(hl == 1))
                    if lv < 6:
                        Pn = PP[(lv + 1) % 2]
                        for hl in range(2):
                            MM(k, ps_pp[:, hl, 0:128], Pc[:, hl, 128:256], Pc[:, hl, 0:128], [Pc], [ps_pp], inc=False)
                            MM(k, ps_pp[:, hl, 128:256], Pc[:, hl, 0:128], Pc[:, hl, 128:256], [Pc], [ps_pp],
                               inc=(hl == 1))
                        ACTF(k, Pn[:, :, :], ps_pp[:, :, :], AF.Copy, [ps_pp], [Pn])
                    TT(k.dve, X[:, :, :], ps_x[:, 0:256], X[:, :, :], ALU.add, [ps_x, X], [X])
                    yield
                for hl in range(2):
                    ACTF(k, Xw[hl][:, hs[hl]], X[:, hl, 0:64], AF.Copy, [X], [Xw[hl]])
                for hl in range(2):
                    pq = bank()
                    MM(k, pq[:, 0:128], Xw[hl][:, :], AbRb[:, hl, 128:256], [Xw[hl], AbRb], [pq])
                    TT(k.dve, QpT[pr][hl][:, cs], pq[:, 0:128], ar[pr][hl][:, c, 128:256], ALU.add, [pq, ar[pr][hl]],
                       [QpT[pr][hl]])
                yield
                py0 = bank()
                for hl in range(2):
                    MM(k, py0[hs[hl], 0:128], X[:, hl, 64:128], AbRb[:, hl, 128:256], [X, AbRb], [py0], stop=False,
                       inc=False)
                    MM(k, py0[hs[hl], 0:128], TM[:, 3, hs[hl]], AkRk[:, hl, 128:256], [TM, AkRk], [py0], start=False,
                       inc=(hl == 1))
                ACTF(k, Y0T[pr][:, cs], py0[:, 0:128], AF.Copy, [py0], [Y0T[pr]])
                yield
                for hl in range(2):
                    pm = bank()
                    MM(k, pm[:, 0:64], Xw[hl][:, :], TM[:, 1, hs[hl]], [Xw[hl], TM], [pm])
                    STT(k, MT[pr][hl][:, c, :], I2h[hl][:, :], pC[pr][:, c:c + 1], pm[:, 0:64], ALU.mult, ALU.add,
                        [I2h[hl], pC[pr], pm], [MT[pr][hl]])
                yield
                pg = bank()
                for hl in range(2):
                    MM(k, pg[hs[hl], 0:64], TM[:, 1, hs[hl]], X[:, hl, 64:128], [X, TM], [pg], stop=False, inc=False)
                    MM(k, pg[hs[hl], 0:64], TM[:, 2, hs[hl]], TM[:, 3, hs[hl]], [TM], [pg], start=False,
                       inc=(hl == 1))
                ACTF(k, Gm[pr][:, c, :], pg[:, 0:64], AF.Copy, [pg], [Gm[pr]])

            pending = [(pr, c) for c in range(NCH) for pr in range(2)]
            active = {}
            while pending or active:
                for sid in range(NS):
                    if sid not in active and pending:
                        pr_, c_ = pending.pop(0)
                        active[sid] = stageA(pr_, c_, sid)
                for sid in list(active):
                    try:
                        next(active[sid])
                    except StopIteration:
                        del active[sid]
                if gp is not None:
                    try:
                        next(gp)
                    except StopIteration:
                        gp = None
            for c in range(NCH):
                cs = slice(c * C, (c + 1) * C)
                for pr in range(2):
                    py = bank()
                    for hl in range(2):
                        MM(k, py[hs[hl], 0:128], Hb[pr][:, :], QpT[pr][hl][:, cs], [Hb[pr], QpT[pr][hl]], [py], inc=(hl == 1))
                    TT(k.dve, yacc[pr][:, cs], py[:, 0:128], Y0T[pr][:, cs], ALU.add, [py, Y0T[pr]], [yacc[pr]])
                    ph = bank()
                    for hl in range(2):
                        MM(k, ph[hs[hl], 0:64], MT[pr][hl][:, c, :], Hb[pr][:, :], [MT[pr][hl], Hb[pr]], [ph], inc=(hl == 1))
                    TT(k.dve, Hb[pr][:, :], ph[:, 0:64], Gm[pr][:, c, :], ALU.add, [ph, Gm[pr]], [Hb[pr]])
            for pr in range(2):
                ACTF(k, ob1[:, :], yacc[pr][:, :], AF.Copy, [yacc[pr]], [ob1])
                p = proj([(ones2[:, :], ob1[:, :])], [ones2, ob1])
                STT(k, o1[:, :], p[:, :], -1.0 / RW_N, yacc[pr][:, :], ALU.mult, ALU.add, [p, yacc[pr]], [o1])
                rstd = nrm.stats([o1[:, :]], [o1], RW_N, RW_EPS)
                TT(k.dve, o1[:, :], o1[:, :], rstd[:, :], ALU.mult, [o1, rstd], [o1])
                TS(k.dve, o1[:, :], o1[:, :], vec[:, o_lg + pr:o_lg + pr + 1], vec[:, o_lb + pr:o_lb + pr + 1], ALU.mult,
                   ALU.add, [o1, vec], [o1])
                TT(k.dve, o1[:, :], o1[:, :], bon[pr][:, :], ALU.add, [o1, bon[pr]], [o1])
                TT(k.dve, yo[pr][:, :], o1[:, :], gate[pr][:, :], ALU.mult, [o1, gate[pr]], [yo[pr]])
                k.sp.dma(dr["yT"][:, 6 + pr, t0:t0 + TB], yo[pr][:, :], yo[pr], reads=[yo[pr]], writes=[dr["yT_buf"]])
            drain(gp)


def phase_out(k, T, l, dr, cst):
    nc = k.nc
    with k.phase() as es:
        vec = k.sb(es, "o_vec", [128, NV], F32)
        k.sp.dma(vec[:, :], dr["vecs"][l], vec, writes=[vec])
        ones = k.sb(es, "o_ones", [128, 128], BF16)
        load_cast(k, ones, ones[:, :], cst["ones"])
        wo = k.sb(es, "o_wo", [128, 8, D], BF16)
        load_cast3(k, wo, wo[:, :, :], dr["w_out"][l], 0, D)
        nrm = Norm(k, es, "o_n", TB, ones)
        xb = [k.sb(es, f"o_xb{i}", [128, 8, TB], F32) for i in range(2)]
        yb = [k.sb(es, f"o_yb{i}", [128, 8, TB], BF16) for i in range(2)]
        yn = k.sb(es, "o_yn", [128, 4, TB], BF16)
        ps = [k.ps(es, f"o_ps{i}", [128, TB]) for i in range(3)]
        o_g = VOFF["on_g"][0]
        xsrc, xsrc_buf = (dr["x_in"], Buf(None, "xin")) if l == 0 else (dr["xT"], dr["xT_buf"])
        def load_blk(b):
            x, y = xb[b % 2], yb[b % 2]
            k.sp.dma(x[:, :, :], xsrc[:, :, b * TB:(b + 1) * TB], x, reads=[xsrc_buf], writes=[x])
            k.sp.dma(y[:, :, :], dr["yT"][:, :, b * TB:(b + 1) * TB], y, reads=[dr["yT_buf"]], writes=[y])

        load_blk(0)
        for b in range(T // TB):
            t0 = b * TB
            x, y = xb[b % 2], yb[b % 2]
            if (b + 1) * TB < T:
                load_blk(b + 1)
            rstd = nrm.stats([y[:, c, :] for c in range(4)], [y] * 4, MLA_H * DV, EPS)
            for c in range(4):
                k.dve.op(lambda c=c, y=y: nc.vector.scalar_tensor_tensor(
                    out=yn[:, c, :], in0=y[:, c, :], scalar=vec[:, o_g + c:o_g + c + 1], in1=rstd[:, :],
                    op0=ALU.mult, op1=ALU.mult), reads=[y, vec, rstd], writes=[yn])
            for m in range(8):
                p = ps[m % 3]
                mm(k, p, p[:, :], [(wo[:, c, m * 128:(m + 1) * 128], yn[:, c, :] if c < 4 else y[:, c, :])
                                   for c in range(8)], [wo, yn, y])
                k.dve.op(lambda m=m, p=p, x=x: nc.vector.tensor_tensor(out=x[:, m, :], in0=p[:, :], in1=x[:, m, :],
                                                                       op=ALU.add), reads=[p, x], writes=[x])
            k.sp.dma(dr["xT"][:, :, t0:t0 + TB], x[:, :, :], x, reads=[x], writes=[dr["xT_buf"]])


def build(T, shapes, phases=("h0", "mla", "gla", "rwkv", "out", "ffn"), depth=DEPTH, debug=()):
    nc = bass.Bass("TRN2", target_bir_lowering=False)
    dr = {}
    for n, shp in shapes.items():
        dr[n] = nc.dram_tensor(n, list(shp), F32, kind="ExternalInput").ap()
    dr["x_in"] = dr["xT_in"].rearrange("(c p) t -> p c t", p=128)

    def scratch(name, dtype):
        t = nc.dram_tensor(name, [D, T], dtype, kind="ExternalOutput" if name in debug else "Internal").ap()
        dr[name] = t.rearrange("(c p) t -> p c t", p=128)
        dr[name + "_buf"] = Buf(None, name)

    outT = nc.dram_tensor("outT", [D, T], F32, kind="ExternalOutput").ap()
    dr["outT"] = outT.rearrange("(c p) t -> p c t", p=128)
    dr["outT_buf"] = Buf(None, "outT")
    scratch("xT", F32)
    scratch("hT", BF16)
    scratch("yT", BF16)
    cst = {"ones": dr["c_ones"], "amask": dr["c_amask"], "cos4": dr["c_cos4"], "sin4": dr["c_sin4"],
           "misc": dr["c_misc"]}
    with contextlib.ExitStack() as es:
        k = K(nc, es)
        if "h0" not in phases:
            with k.phase() as pes:
                t = k.sb(pes, "cp", [128, 8, TB], F32)
                for b in range(T // TB):
                    k.sp.dma(t[:, :, :], dr["x_in"][:, :, b * TB:(b + 1) * TB], t, writes=[t])
                    k.sp.dma(dr["xT"][:, :, b * TB:(b + 1) * TB], t[:, :, :], t, reads=[t], writes=[dr["xT_buf"]])
        else:
            phase_h0(k, T, dr, cst)
        for l in range(depth):
            if "mla" in phases:
                phase_mla(k, T, l, dr, cst, (0, 1))
                phase_mla(k, T, l, dr, cst, (2, 3))
            if "gla" in phases:
                phase_gla(k, T, l, dr, cst)
            if "rwkv" in phases:
                phase_rwkv(k, T, l, dr, cst)
            if "out" in phases:
                phase_out(k, T, l, dr, cst)
            if "ffn" in phases:
                phase_ffn(k, T, l, dr, cst, last=(l == depth - 1))
        k.barrier()
    return nc


W_NAMES = ["w_in", "mla_w_uq", "mla_w_ukv", "gla_w_gk", "rwkv_w2", "rwkv_a2", "rwkv_g2", "w_out",
           "ffn_w_up", "ffn_w_down"]


def pack_vecs(inp, l):
    v = np.zeros((128, NV), np.float32)

    def put(name, arr, n):
        o, c = VOFF[name]
        assert c == n
        v[:, o:o + n] = _cols(arr, n)

    put("ln1_g", inp["ln1_g"][l], 8)
    put("ln2_g", inp["ln2_g"][l], 8)
    put("final_g", inp["final_g"], 8)
    put("qn_g", inp["mla_q_norm_g"][l], 3)
    put("kvn_g", inp["mla_kv_norm_g"][l], 2)
    put("on_g", inp["mla_out_norm_g"][l], 4)
    put("gla_nb", inp["gla_b_gk"][l], 1)
    put("gla_ng", np.tile(inp["gla_norm_g"][l], 2), 1)
    put("rw_mu", inp["rwkv_mu"][l], 8)
    put("rw_w0", inp["rwkv_w0"][l], 2)
    put("rw_a0", inp["rwkv_a0"][l], 2)
    put("rw_kk", inp["rwkv_k_k"][l], 2)
    put("rw_ka", inp["rwkv_k_a"][l], 2)
    put("rw_rk", inp["rwkv_r_k"][l].reshape(-1), 2)
    put("rw_lng", inp["rwkv_ln_g"][l], 2)
    put("rw_lnb", inp["rwkv_ln_b"][l], 2)
    cw = inp["ffn_conv_w"][l]
    put("cw0", cw[0], 44)
    put("cw1", cw[1], 44)
    put("cw2", cw[2], 44)
    put("cb", inp["ffn_conv_b"][l], 44)
    return v


def host_inputs(inp, b, T):
    m = {}
    m["xT_in"] = np.ascontiguousarray(np.asarray(inp["x"][b, :T], np.float32).T)
    m["vecs"] = np.stack([pack_vecs(inp, l) for l in range(DEPTH)])
    m["c_ones"] = np.ones((128, 128), np.float32)
    m["c_misc"] = make_misc()
    kk = np.arange(128)[:, None]
    qq = np.arange(TB)[None, :]
    m["c_amask"] = np.stack([np.where(kk + 128 * d <= qq, 0.0, -30000.0).astype(np.float32) for d in range(4)])
    inv = (np.float32(1.0) / (np.float32(10000.0) ** (np.arange(0, ROPE, 2, dtype=np.float32) / np.float32(ROPE)))
           ).astype(np.float32)
    ang = (np.arange(T, dtype=np.float32)[None, :] * inv[:, None]).astype(np.float32)
    cs, sn = np.cos(ang).astype(np.float32), np.sin(ang).astype(np.float32)
    m["c_cos4"] = np.ascontiguousarray(np.concatenate([cs, cs, cs, cs], 0))
    m["c_sin4"] = np.ascontiguousarray(np.concatenate([-sn, sn, -sn, sn], 0))
    for n in W_NAMES:
        m[n] = np.ascontiguousarray(np.asarray(inp[n], np.float32))
    return m


def run(inp, T, nb, **bk):
    maps = [host_inputs(inp, b, T) for b in range(nb)]
    shapes = {n: a.shape for n, a in maps[0].items()}
    nc = build(T, shapes, **bk)
    res = run_bass_kernel_spmd(nc, maps, core_ids=list(range(nb)))
    return res


def kernel(**inputs):
    inp = {n: np.asarray(a) for n, a in inputs.items()}
    B, T = inp["x"].shape[0], inp["x"].shape[1]
    ncores = 8
    maps = [host_inputs(inp, b % B, T) for b in range(ncores)]
    shapes = {n: a.shape for n, a in maps[0].items()}
    nc = build(T, shapes)
    res = run_bass_kernel_spmd(nc, maps, core_ids=list(range(ncores)))
    out = np.stack([np.ascontiguousarray(res.results[b]["outT"].T) for b in range(B)])
    return out.astype(np.float32)


def phase_h0(k, T, dr, cst):
    nc = k.nc
    with k.phase() as es:
        vec = k.sb(es, "h_vec", [128, NV], F32)
        k.sp.dma(vec[:, :], dr["vecs"][0], vec, writes=[vec])
        ones = k.sb(es, "h_ones", [128, 128], BF16)
        load_cast(k, ones, ones[:, :], cst["ones"])
        nrm = Norm(k, es, "h_n", TB, ones)
        xb = [k.sb(es, f"h_xb{i}", [128, 8, TB], F32) for i in range(2)]
        hT = [k.sb(es, f"h_hT{i}", [128, 8, TB], BF16) for i in range(2)]
        o_g = VOFF["ln1_g"][0]
        k.sp.dma(xb[0][:, :, :], dr["x_in"][:, :, 0:TB], xb[0], writes=[xb[0]])
        for b in range(T // TB):
            x, h = xb[b % 2], hT[b % 2]
            t0 = b * TB
            if (b + 1) * TB < T:
                xn = xb[(b + 1) % 2]
                k.sp.dma(xn[:, :, :], dr["x_in"][:, :, t0 + TB:t0 + 2 * TB], xn, writes=[xn])
            rstd = nrm.stats([x[:, c, :] for c in range(8)], [x] * 8, D, EPS)
            for c in range(8):
                k.dve.op(lambda c=c, x=x, h=h: nc.vector.scalar_tensor_tensor(
                    out=h[:, c, :], in0=x[:, c, :], scalar=vec[:, o_g + c:o_g + c + 1], in1=rstd[:, :],
                    op0=ALU.mult, op1=ALU.mult), reads=[x, vec, rstd], writes=[h])
            k.sp.dma(dr["hT"][:, :, t0:t0 + TB], h[:, :, :], h, reads=[h], writes=[dr["hT_buf"]])


def phase_mla(k, T, l, dr, cst, heads):
    nc = k.nc
    nh = len(heads)
    nblk = T // TB
    NKT = T // 128
    scale = float((NOPE + ROPE) ** -0.5)
    w_in, w_uq, w_ukv = dr["w_in"], dr["mla_w_uq"], dr["mla_w_ukv"]
    with k.phase() as es:
        vec = k.sb(es, "a_vec", [128, NV], F32)
        k.sp.dma(vec[:, :], dr["vecs"][l], vec, writes=[vec])
        ones = k.sb(es, "a_ones", [128, 128], BF16)
        load_cast(k, ones, ones[:, :], cst["ones"])
        masks = k.sb(es, "a_mask", [128, 4, TB], BF16)
        k.pool.dma(masks[:, :, :], cst["amask"].rearrange("d p n -> p d n"), masks, writes=[masks])
        ident = k.sb(es, "a_ident", [128, 128], BF16)
        load_cast(k, ident, ident[:, :], cst["misc"][:, MOFF["ident"][0]:MOFF["ident"][1]])
        wA = k.sb(es, "a_wA", [128, 8, 896], BF16)
        load_cast3(k, wA, wA[:, :, 0:640], w_in[l], 0, 640)
        for r in range(2):
            load_cast3(k, wA, wA[:, :, 640 + 64 * r:704 + 64 * r], w_in[l], 640, 704)
            load_cast3(k, wA, wA[:, :, 768 + 64 * r:800 + 64 * r], w_in[l], 672, 704)
            load_cast3(k, wA, wA[:, :, 800 + 64 * r:832 + 64 * r], w_in[l], 640, 672)
        wq = k.sb(es, "a_wq", [128, 3, 3 * nh * 128], BF16)
        ro = nh * 128
        so = 2 * nh * 128
        k.pool.op(lambda: nc.gpsimd.memset(wq[:, :, :], 0.0), writes=[wq])
        for i, h in enumerate(heads):
            b0 = h * 192
            load_cast3(k, wq, wq[:, :, i * 128:(i + 1) * 128], w_uq[l], b0, b0 + 128)
            load_cast3(k, wq, wq[:, :, ro + i * 128:ro + i * 128 + 64], w_uq[l], b0 + 128, b0 + 192)
            load_cast3(k, wq, wq[:, :, so + i * 128:so + i * 128 + 32], w_uq[l], b0 + 160, b0 + 192)
            load_cast3(k, wq, wq[:, :, so + i * 128 + 32:so + i * 128 + 64], w_uq[l], b0 + 128, b0 + 160)
        wk = k.sb(es, "a_wk", [128, 2, nh * 128], BF16)
        wv = k.sb(es, "a_wv", [128, 2, nh * 128], BF16)
        for i, h in enumerate(heads):
            load_cast3(k, wk, wk[:, :, i * 128:(i + 1) * 128], w_ukv[l], h * 256, h * 256 + 128)
            load_cast3(k, wv, wv[:, :, i * 128:(i + 1) * 128], w_ukv[l], h * 256 + 128, h * 256 + 256)
        kn = [k.sb(es, f"a_kn{i}", [128, T], BF16) for i in range(nh)]
        kr = k.sb(es, "a_kr", [128, T], BF16)
        vc = k.sb(es, "a_vc", [128, NKT, nh * 128], BF16)
        hT = [k.sb(es, f"a_hT{i}", [128, 8, TB], BF16) for i in range(2)]
        cq = k.sb(es, "a_cq", [128, 3, TB], F32)
        ckv = k.sb(es, "a_ckv", [128, 2, TB], F32)
        cqn = k.sb(es, "a_cqn", [128, 3, TB], BF16)
        ckvn = k.sb(es, "a_ckvn", [128, 2, TB], BF16)
        qn = [k.sb(es, f"a_qn{i}", [128, TB], BF16) for i in range(nh)]
        qr = [k.sb(es, f"a_qr{i}", [128, TB], BF16) for i in range(nh)]
        cos = k.sb(es, "a_cos", [128, TB], F32)
        sin = k.sb(es, "a_sin", [128, TB], F32)
        t1 = k.sb(es, "a_t1", [128, TB], F32)
        t2 = k.sb(es, "a_t2", [128, TB], F32)
        pT = [k.sb(es, f"a_pT{i}", [128, TB], BF16) for i in range(3)]
        oT = [k.sb(es, f"a_oT{i}", [128, TB], BF16) for i in range(2)]
        rcp = k.sb(es, "a_rcp", [128, TB], F32)
        paccs = [k.sb(es, f"a_pacc{i}", [128, TB], F32) for i in range(2)]
        oraw = k.sb(es, "a_oraw", [128, TB], F32)
        ones_f = k.sb(es, "a_onesf", [128, 128], F32)
        k.sp.dma(ones_f[:, :], cst["ones"], ones_f, writes=[ones_f])
        nrm = Norm(k, es, "a_n", TB, ones)
        ps_p = [k.ps(es, f"a_psp{i}", [128, TB]) for i in range(2)]
        ps_s = [k.ps(es, f"a_pss{i}", [128, TB]) for i in range(3)]
        ps_o = k.ps(es, "a_pso", [128, TB])
        ps_l = k.ps(es, "a_psl", [128, TB])
        o_qg, o_kg = VOFF["qn_g"][0], VOFF["kvn_g"][0]
        ip = 0
        ipt = 0

        def proj(lhs_list, rhs_list, reads, m=128, n=TB):
            nonlocal ip
            p = ps_p[ip % 2]
            ip += 1
            mm(k, p, p[0:m, 0:n], list(zip(lhs_list, rhs_list)), reads)
            return p

        kn_b = [[Buf(kn[i].ap[:, bb * TB:(bb + 1) * TB], f"kn{i}_{bb}") for bb in range(nblk)] for i in range(nh)]
        kr_b = [Buf(kr.ap[:, bb * TB:(bb + 1) * TB], f"kr_{bb}") for bb in range(nblk)]
        vc_b = [Buf(vc.ap[:, bb * 4:(bb + 1) * 4, :], f"vc_{bb}") for bb in range(nblk)]
        qn2 = [qn, [k.sb(es, f"a_qnB{i}", [128, TB], BF16) for i in range(nh)]]
        qr2 = [qr, [k.sb(es, f"a_qrB{i}", [128, TB], BF16) for i in range(nh)]]

        def prep(b):
            t0 = b * TB
            h = hT[b % 2]
            qn_, qr_ = qn2[b % 2], qr2[b % 2]
            k.sp.dma(h[:, :, :], dr["hT"][:, :, t0:t0 + TB], h, reads=[dr["hT_buf"]], writes=[h])
            k.sp.dma(cos[:, :], cst["cos4"][:, t0:t0 + TB], cos, writes=[cos])
            k.sp.dma(sin[:, :], cst["sin4"][:, t0:t0 + TB], sin, writes=[sin])
            yield
            for j in range(3):
                p = proj([wA[:, c, j * 128:(j + 1) * 128] for c in range(8)], [h[:, c, :] for c in range(8)], [wA, h])
                k.act.op(lambda p=p, j=j: nc.scalar.activation(out=cq[:, j, :], in_=p[:, :], func=AF.Copy),
                         reads=[p], writes=[cq])
                yield
            for j in range(2):
                p = proj([wA[:, c, 384 + j * 128:384 + (j + 1) * 128] for c in range(8)],
                         [h[:, c, :] for c in range(8)], [wA, h])
                k.act.op(lambda p=p, j=j: nc.scalar.activation(out=ckv[:, j, :], in_=p[:, :], func=AF.Copy),
                         reads=[p], writes=[ckv])
                yield
            rstd = nrm.stats([cq[:, j, :] for j in range(3)], [cq] * 3, QLORA, EPS)
            yield
            for j in range(3):
                k.dve.op(lambda j=j: nc.vector.scalar_tensor_tensor(
                    out=cqn[:, j, :], in0=cq[:, j, :], scalar=vec[:, o_qg + j:o_qg + j + 1], in1=rstd[:, :],
                    op0=ALU.mult, op1=ALU.mult), reads=[cq, vec, rstd], writes=[cqn])
            yield
            rstd = nrm.stats([ckv[:, j, :] for j in range(2)], [ckv] * 2, KVLORA, EPS)
            yield
            for j in range(2):
                k.dve.op(lambda j=j: nc.vector.scalar_tensor_tensor(
                    out=ckvn[:, j, :], in0=ckv[:, j, :], scalar=vec[:, o_kg + j:o_kg + j + 1], in1=rstd[:, :],
                    op0=ALU.mult, op1=ALU.mult), reads=[ckv, vec, rstd], writes=[ckvn])
            yield
            pa = proj([wA[:, c, 640:768] for c in range(8)], [h[:, c, :] for c in range(8)], [wA, h])
            k.dve.op(lambda pa=pa: nc.vector.tensor_tensor(out=t1[:, :], in0=pa[:, :], in1=cos[:, :], op=ALU.mult),
                     reads=[pa, cos], writes=[t1])
            yield
            pb = proj([wA[:, c, 768:896] for c in range(8)], [h[:, c, :] for c in range(8)], [wA, h])
            k.dve.op(lambda pb=pb: nc.vector.tensor_tensor(out=t2[:, :], in0=pb[:, :], in1=sin[:, :], op=ALU.mult),
                     reads=[pb, sin], writes=[t2])
            k.pool.op(lambda: nc.gpsimd.tensor_tensor(out=kr_b[b][:, :], in0=t1[:, :], in1=t2[:, :], op=ALU.add),
                      reads=[t1, t2], writes=[kr_b[b]])
            yield
            for i in range(nh):
                p = proj([wk[:, c, i * 128:(i + 1) * 128] for c in range(2)], [ckvn[:, c, :] for c in range(2)],
                         [wk, ckvn])
                k.act.op(lambda p=p, i=i: nc.scalar.activation(out=kn_b[i][b][:, :], in_=p[:, :], func=AF.Copy),
                         reads=[p], writes=[kn_b[i][b]])
                yield
            for s_ in range(TB // 128):
                p = proj([ckvn[:, c, s_ * 128:(s_ + 1) * 128] for c in range(2)], [wv[:, c, :] for c in range(2)],
                         [wv, ckvn], n=nh * 128)
                k.act.op(lambda p=p, s_=s_: nc.scalar.activation(out=vc_b[b][:, s_, :], in_=p[:, 0:nh * 128],
                                                                 func=AF.Copy), reads=[p], writes=[vc_b[b]])
                yield
            for i in range(nh):
                p = proj([wq[:, c, i * 128:(i + 1) * 128] for c in range(3)], [cqn[:, c, :] for c in range(3)],
                         [wq, cqn])
                k.act.op(lambda p=p, i=i: nc.scalar.activation(out=qn_[i][:, :], in_=p[:, :], func=AF.Copy),
                         reads=[p], writes=[qn_[i]])
                yield
            for pr in range(nh):
                pa = proj([wq[:, c, ro + pr * 128:ro + (pr + 1) * 128] for c in range(3)],
                          [cqn[:, c, :] for c in range(3)], [wq, cqn])
                k.dve.op(lambda pa=pa: nc.vector.tensor_tensor(out=t1[:, :], in0=pa[:, :], in1=cos[:, :],
                                                               op=ALU.mult), reads=[pa, cos], writes=[t1])
                yield
                pb = proj([wq[:, c, so + pr * 128:so + (pr + 1) * 128] for c in range(3)],
                          [cqn[:, c, :] for c in range(3)], [wq, cqn])
                k.dve.op(lambda pb=pb: nc.vector.tensor_tensor(out=t2[:, :], in0=pb[:, :], in1=sin[:, :],
                                                               op=ALU.mult), reads=[pb, sin], writes=[t2])
                k.pool.op(lambda pr=pr: nc.gpsimd.tensor_tensor(out=qr_[pr][:, :], in0=t1[:, :], in1=t2[:, :],
                                                                op=ALU.add), reads=[t1, t2], writes=[qr_[pr]])
                yield

        def drain(g):
            if g is not None:
                for _ in g:
                    pass

        drain(prep(0))
        for b in range(nblk):
            t0 = b * TB
            qn_, qr_ = qn2[b % 2], qr2[b % 2]
            gnext = prep(b + 1) if b + 1 < nblk else None
            nkt = (t0 + TB) // 128
            its = [(i, kt) for i in range(nh) for kt in range(nkt)]
            sb_of = {}

            def issue_s(n):
                nonlocal ipt
                i, kt = its[n]
                s = ps_s[ipt % 3]
                ipt += 1
                bb, ks = kt // 4, slice((kt % 4) * 128, (kt % 4 + 1) * 128)
                pairs = [(kn_b[i][bb][:, ks], qn_[i][:, :]), (kr_b[bb][:, ks], qr_[i][:, :])]
                d_ = kt - (t0 // 128)
                if d_ >= 0:
                    pairs.append((ident[:, :], masks[:, d_, :]))
                mm(k, s, s[:, :], pairs, [kn_b[i][bb], qn_[i], kr_b[bb], qr_[i], ident, masks])
                sb_of[n] = s

            for n in range(min(2, len(its))):
                issue_s(n)
            for n, (i, kt) in enumerate(its):
                if n + 2 < len(its):
                    issue_s(n + 2)
                if gnext is not None:
                    try:
                        next(gnext)
                    except StopIteration:
                        gnext = None
                s = sb_of.pop(n)
                pt = pT[n % 3]
                k.act.op(lambda s=s, pt=pt: nc.scalar.activation(out=pt[:, :], in_=s[:, :], func=AF.Exp,
                                                                 scale=scale), reads=[s], writes=[pt])
                bb = kt // 4
                k.pe.op(lambda pt=pt, kt=kt, i=i, bb=bb: nc.tensor.matmul(
                    ps_o[:, :], lhsT=vc_b[bb][:, kt % 4, i * 128:(i + 1) * 128], rhs=pt[:, :], start=(kt == 0),
                    stop=(kt == nkt - 1)), reads=[vc_b[bb], pt], writes=[ps_o], inc=True)
                pacc = paccs[kt % 2]
                e_ = k.dve if kt % 2 == 0 else k.pool
                if kt < 2:
                    e_.op(lambda pt=pt, e_=e_, pacc=pacc: e_.eng.tensor_copy(out=pacc[:, :], in_=pt[:, :]),
                          reads=[pt], writes=[pacc])
                else:
                    e_.op(lambda pt=pt, e_=e_, pacc=pacc: e_.eng.tensor_tensor(out=pacc[:, :], in0=pacc[:, :],
                                                                               in1=pt[:, :], op=ALU.add),
                          reads=[pt, pacc], writes=[pacc])
                if kt == nkt - 1:
                    k.act.op(lambda: nc.scalar.activation(out=oraw[:, :], in_=ps_o[:, :], func=AF.Copy),
                             reads=[ps_o], writes=[oraw])
                    k.pe.op(lambda: nc.tensor.matmul(ps_l[:, :], lhsT=ones_f[:, :], rhs=paccs[0][:, :], start=True,
                                                     stop=False), reads=[ones_f, paccs[0]], writes=[ps_l], inc=False)
                    k.pe.op(lambda: nc.tensor.matmul(ps_l[:, :], lhsT=ones_f[:, :], rhs=paccs[1][:, :], start=False,
                                                     stop=True), reads=[ones_f, paccs[1]], writes=[ps_l], inc=True)
                    k.dve.op(lambda: nc.vector.reciprocal(out=rcp[:, :], in_=ps_l[:, :]), reads=[ps_l], writes=[rcp])
                    o = oT[i % 2]
                    k.dve.op(lambda o=o: nc.vector.tensor_tensor(out=o[:, :], in0=oraw[:, :], in1=rcp[:, :],
                                                                 op=ALU.mult), reads=[oraw, rcp], writes=[o])
                    k.sp.dma(dr["yT"][:, heads[i], t0:t0 + TB], o[:, :], o, reads=[o], writes=[dr["yT_buf"]])
            drain(gnext)
```

```python
import contextlib
import numpy as np
import concourse.bass as bass
import concourse.mybir as mybir
from concourse.bass_utils import run_bass_kernel_spmd

F32 = mybir.dt.float32
BF16 = mybir.dt.bfloat16
AF = mybir.ActivationFunctionType
ALU = mybir.AluOpType

D = 1024
DEPTH = 2
MLA_H, NOPE, ROPE, DV = 4, 128, 64, 128
QLORA, KVLORA = 384, 256
GLA_H, GDK, GDV, GRANK = 4, 32, 64, 16
RW_H, RW_N = 4, 64
DFF = 2816
NFT = DFF // 128
EPS = 1e-6
RW_EPS = 64e-5
TB = 512
REPORT_SBUF = False
NO_SELF_WAIT = False


class Buf:
    def __init__(self, ap=None, name=""):
        self.ap = ap
        self.name = name
        self.w = None
        self.r = {}
        self.d = None

    def __getitem__(self, idx):
        return self.ap[idx]


class Eng:
    def __init__(self, k, name, eng, sem):
        self.k, self.name, self.eng, self.sem = k, name, eng, sem
        self.count = 0
        self.known = {}
        self.pending = False

    def wait(self, tok):
        if tok is None:
            return
        sem, val = tok
        if sem is self.sem and (self.name == "pe" or val > self.count or NO_SELF_WAIT):
            return
        if self.known.get(sem, 0) >= val:
            return
        self.eng.wait_ge(sem, val)
        self.known[sem] = val

    def deps(self, reads, writes):
        for b in reads:
            self.wait(b.w)
        for b in writes:
            self.wait(b.w)
            for s, v in list(b.r.items()):
                self.wait((s, v))

    def mark(self, tok, reads, writes):
        s, v = tok
        for b in reads:
            if b.r.get(s, 0) < v:
                b.r[s] = v
        for b in writes:
            b.w = tok
            b.r = {}

    def op(self, fn, reads=(), writes=(), inc=True):
        self.deps(reads, writes)
        inst = fn()
        tok = (self.sem, self.count + 1)
        if inc:
            inst.then_inc(self.sem, 1)
            self.count += 1
            self.pending = False
        else:
            self.pending = True
        self.mark(tok, reads, writes)
        return inst

    def dma(self, out, in_, sb, reads=(), writes=()):
        self.deps(reads, writes)
        if sb.d is None:
            sb.d = {}
        if self.name not in sb.d:
            sb.d[self.name] = self.k.get_dsem(self.name)
            self.k.phase_dma.append((sb, self.name))
        e = sb.d[self.name]
        e[1] += 16
        self.eng.dma_start(out=out, in_=in_).then_inc(e[0], 16)
        self.mark((e[0], e[1]), reads, writes)


class K:
    def __init__(self, nc, es):
        self.nc, self.es = nc, es
        self.nsem = 0
        self.pe = Eng(self, "pe", nc.tensor, self.new_sem("pe"))
        self.act = Eng(self, "act", nc.scalar, self.new_sem("act"))
        self.dve = Eng(self, "dve", nc.vector, self.new_sem("dve"))
        self.pool = Eng(self, "pool", nc.gpsimd, self.new_sem("pool"))
        self.sp = Eng(self, "sp", nc.sync, self.new_sem("sp"))
        self.engs = [self.pe, self.act, self.dve, self.pool, self.sp]
        self.dsem_free = {"sp": [], "pool": []}
        self.phase_dma = []

    def get_dsem(self, q):
        if self.dsem_free[q]:
            return self.dsem_free[q].pop()
        return [self.new_sem("dma" + q), 0]

    @contextlib.contextmanager
    def phase(self):
        with contextlib.ExitStack() as es:
            yield es
            if REPORT_SBUF:
                print("sbuf remaining at phase end:", self.nc.sbuf_bytes_remaining)
            self.barrier()
            for b, q in self.phase_dma:
                self.dsem_free[q].append(b.d.pop(q))
            self.phase_dma = []

    def new_sem(self, name):
        self.nsem += 1
        return self.es.enter_context(self.nc.semaphore(f"{name}_{self.nsem}"))

    def sb(self, es, name, shape, dtype):
        self.nsem += 1
        t = es.enter_context(self.nc.sbuf_tensor(f"{name}_{self.nsem}", list(shape), dtype))
        return Buf(t, name)

    def ps(self, es, name, shape, dtype=F32):
        self.nsem += 1
        t = es.enter_context(self.nc.psum_tensor(f"{name}_{self.nsem}", list(shape), dtype))
        return Buf(t, name)

    def barrier(self):
        toks = []
        for e in self.engs:
            assert not e.pending, e.name
            if e.count:
                toks.append((e.sem, e.count))
        for b, q in self.phase_dma:
            toks.append((b.d[q][0], b.d[q][1]))
        for e in self.engs:
            for t in toks:
                e.wait(t)


def mm(k, ps_buf, ps_ap, pairs, reads):
    n = len(pairs)
    for i, (l, r) in enumerate(pairs):
        k.pe.op(lambda l=l, r=r, i=i: k.nc.tensor.matmul(ps_ap, lhsT=l, rhs=r, start=(i == 0),
                                                         stop=(i == n - 1)),
                reads=reads, writes=[ps_buf], inc=(i == n - 1))


def load_cast(k, wbuf, dst_ap, src_ap, dram_reads=()):
    n = src_ap.shape[-1]
    for c0 in range(0, n, 2048):
        c1 = min(n, c0 + 2048)
        k.pool.dma(dst_ap[:, c0:c1], src_ap[:, c0:c1], wbuf, reads=list(dram_reads), writes=[wbuf])


def load_cast3(k, wbuf, dst3, src2, c0, c1):
    src3 = src2.rearrange("(c p) n -> p c n", p=128)
    n = c1 - c0
    for o in range(0, n, 2048):
        e = min(n, o + 2048)
        k.pool.dma(dst3[:, :, o:e], src3[:, :, c0 + o:c0 + e], wbuf, writes=[wbuf])


class Norm:
    def __init__(self, k, es, name, n, ones_bf, ps=None):
        self.k, self.n, self.ones = k, n, ones_bf
        self.sq = [k.sb(es, f"{name}_sq{i}", [128, n], BF16) for i in range(2)]
        self.ps = k.ps(es, f"{name}_ps", [128, n]) if ps is None else ps
        self.sd = k.sb(es, f"{name}_sd", [128, n], F32)
        self.rstd = self.sd
        self.i = 0

    def stats(self, srcs, src_bufs, dim, eps, ones_ap=None, np_=128):
        k, n = self.k, self.n
        ones_ap = self.ones[0:np_, 0:np_] if ones_ap is None else ones_ap
        nchunk = len(srcs)
        for c, (s, sbuf) in enumerate(zip(srcs, src_bufs)):
            sq = self.sq[self.i % 2]
            self.i += 1
            k.act.op(lambda s=s, sq=sq: k.nc.scalar.activation(out=sq[0:np_, :], in_=s, func=AF.Square),
                     reads=[sbuf], writes=[sq])
            k.pe.op(lambda sq=sq, c=c: k.nc.tensor.matmul(self.ps[0:np_, :], lhsT=ones_ap, rhs=sq[0:np_, :],
                                                          start=(c == 0), stop=(c == nchunk - 1)),
                    reads=[sq, self.ones], writes=[self.ps], inc=True)
        k.act.op(lambda: k.nc.scalar.activation(out=self.sd[0:np_, :], in_=self.ps[0:np_, :], func=AF.Ln,
                                                scale=1.0 / dim, bias=float(eps)),
                 reads=[self.ps], writes=[self.sd])
        k.act.op(lambda: k.nc.scalar.activation(out=self.sd[0:np_, :], in_=self.sd[0:np_, :], func=AF.Exp,
                                                scale=-0.5),
                 reads=[self.sd], writes=[self.sd])
        return self.rstd


VEC_LAYOUT = [
    ("ln1_g", 8), ("ln2_g", 8), ("final_g", 8), ("qn_g", 3), ("kvn_g", 2), ("on_g", 4),
    ("cw0", 44), ("cw1", 44), ("cw2", 44), ("cb", 44),
    ("gla_nb", 1), ("gla_ng", 1),
    ("rw_mu", 8), ("rw_1mmu", 8), ("rw_w0", 2), ("rw_a0", 2), ("rw_kk", 2), ("rw_ka", 2), ("rw_rk", 2),
    ("rw_lng", 2), ("rw_lnb", 2),
]
VOFF = {}
_o = 0
for _n, _c in VEC_LAYOUT:
    VOFF[_n] = (_o, _c)
    _o += _c
NV = _o


def _cols(v, n):
    return np.ascontiguousarray(np.asarray(v, np.float32).reshape(n, 128).T)


def phase_ffn(k, T, l, dr, cst, last):
    nc = k.nc
    nblk = T // TB
    with k.phase() as es:
        vec = k.sb(es, "f_vec", [128, NV], F32)
        k.sp.dma(vec[:, :], dr["vecs"][l], vec, writes=[vec])
        vnext = k.sb(es, "f_vnext", [128, 8], F32)
        if not last:
            o1 = VOFF["ln1_g"][0]
            k.sp.dma(vnext[:, :], dr["vecs"][l + 1][:, o1:o1 + 8], vnext, writes=[vnext])
        ones = k.sb(es, "f_ones", [128, 128], BF16)
        load_cast(k, ones, ones[:, :], cst["ones"])
        wup = k.sb(es, "f_wup", [128, 8, 2 * DFF], BF16)
        load_cast3(k, wup, wup[:, :, :], dr["ffn_w_up"][l], 0, 2 * DFF)
        wdn = k.sb(es, "f_wdn", [128, NFT, D], BF16)
        load_cast3(k, wdn, wdn[:, :, :], dr["ffn_w_down"][l], 0, D)
        xbs = [k.sb(es, f"f_xb{i}", [128, 8, TB], F32) for i in range(2)]
        hT = k.sb(es, "f_hT", [128, 8, TB], BF16)
        gT = [k.sb(es, f"f_gT{j}", [128, TB], BF16) for j in range(NFT)]
        halo = k.sb(es, "f_halo", [128, 2 * NFT, 2], F32)
        k.pool.op(lambda: nc.gpsimd.memset(halo[:, :, :], 0.0), writes=[halo])
        ug = [k.sb(es, f"f_ug{i}", [128, TB + 2], F32) for i in range(2)]
        ugh = [Buf(ug[i].ap[:, 0:2], f"ugh{i}") for i in range(2)]
        ugm = [Buf(ug[i].ap[:, 2:TB + 2], f"ugm{i}") for i in range(2)]
        a1 = [k.sb(es, f"f_a1{i}", [128, TB], F32) for i in range(2)]
        psu = [k.ps(es, f"f_psu{i}", [128, TB]) for i in range(4)]
        psd = [k.ps(es, f"f_psd{i}", [128, TB]) for i in range(2)]
        nrm = Norm(k, es, "f_n", TB, ones)
        o_g = VOFF["ln2_g"][0]
        o_w0, o_w1, o_w2, o_cb = (VOFF[n][0] for n in ("cw0", "cw1", "cw2", "cb"))
        o_fg = VOFF["final_g"][0]
        xT = dr["xT"]
        xTb = dr["xT_buf"]
        it = 0
        def norm_in(xb):
            rstd = nrm.stats([xb[:, c, :] for c in range(8)], [xb] * 8, D, EPS)
            for c in range(8):
                k.dve.op(lambda c=c, xb=xb: nc.vector.scalar_tensor_tensor(
                    out=hT[:, c, :], in0=xb[:, c, :], scalar=vec[:, o_g + c:o_g + c + 1], in1=rstd[:, :],
                    op0=ALU.mult, op1=ALU.mult), reads=[xb, vec, rstd], writes=[hT])

        k.sp.dma(xbs[0][:, :, :], xT[:, :, 0:TB], xbs[0], reads=[xTb], writes=[xbs[0]])
        norm_in(xbs[0])
        for b in range(nblk):
            t0 = b * TB
            xb = xbs[b % 2]
            if b + 1 < nblk:
                xn = xbs[(b + 1) % 2]
                k.sp.dma(xn[:, :, :], xT[:, :, t0 + TB:t0 + 2 * TB], xn, reads=[xTb], writes=[xn])
            for j in range(NFT):
                res = []
                for half in range(2):
                    jt = j + half * NFT
                    u = half
                    pu = psu[it % 4]
                    it += 1
                    col0 = half * DFF + j * 128
                    mm(k, pu, pu[:, :],
                       [(wup[:, c, col0:col0 + 128], hT[:, c, :]) for c in range(8)], [wup, hT])
                    k.pool.op(lambda u=u, jt=jt: nc.gpsimd.tensor_copy(out=ug[u][:, 0:2], in_=halo[:, jt, :]),
                              reads=[halo], writes=[ugh[u]])
                    k.act.op(lambda u=u, pu=pu: nc.scalar.activation(out=ug[u][:, 2:TB + 2], in_=pu[:, :],
                                                                     func=AF.Copy), reads=[pu], writes=[ugm[u]])
                    k.act.op(lambda u=u, jt=jt, pu=pu: nc.scalar.activation(
                        out=a1[u][:, :], in_=pu[:, :], func=AF.Identity,
                        scale=vec[:, o_w2 + jt:o_w2 + jt + 1], bias=vec[:, o_cb + jt:o_cb + jt + 1]),
                        reads=[pu, vec], writes=[a1[u]])
                    k.pool.op(lambda u=u, jt=jt: nc.gpsimd.tensor_copy(out=halo[:, jt, :], in_=ug[u][:, TB:TB + 2]),
                              reads=[ugm[u]], writes=[halo])
                    k.dve.op(lambda u=u, jt=jt: nc.vector.scalar_tensor_tensor(
                        out=a1[u][:, :], in0=ug[u][:, 1:TB + 1], scalar=vec[:, o_w1 + jt:o_w1 + jt + 1],
                        in1=a1[u][:, :], op0=ALU.mult, op1=ALU.add), reads=[ugm[u], ugh[u], vec, a1[u]], writes=[a1[u]])
                    k.dve.op(lambda u=u, jt=jt: nc.vector.scalar_tensor_tensor(
                        out=a1[u][:, :], in0=ug[u][:, 0:TB], scalar=vec[:, o_w0 + jt:o_w0 + jt + 1],
                        in1=a1[u][:, :], op0=ALU.mult, op1=ALU.add), reads=[ugm[u], ugh[u], vec, a1[u]], writes=[a1[u]])
                    res.append(a1[u])
                s = res[0]
                k.act.op(lambda s=s: nc.scalar.activation(out=s[:, :], in_=s[:, :], func=AF.Silu),
                         reads=[s], writes=[s])
                k.pool.op(lambda s=s, v=res[1], j=j: nc.gpsimd.tensor_tensor(
                    out=gT[j][:, :], in0=s[:, :], in1=v[:, :], op=ALU.mult), reads=[s, res[1]], writes=[gT[j]])
            if b + 1 < nblk:
                norm_in(xbs[(b + 1) % 2])
            for m in range(8):
                pd = psd[m % 2]
                mm(k, pd, pd[:, :], [(wdn[:, j, m * 128:(m + 1) * 128], gT[j][:, :]) for j in range(NFT)],
                   [wdn] + gT)
                k.dve.op(lambda m=m, pd=pd, xb=xb: nc.vector.tensor_tensor(out=xb[:, m, :], in0=pd[:, :],
                                                                           in1=xb[:, m, :], op=ALU.add),
                         reads=[pd, xb], writes=[xb])
            if not last:
                k.sp.dma(xT[:, :, t0:t0 + TB], xb[:, :, :], xb, reads=[xb], writes=[xTb])
                rstd = nrm.stats([xb[:, c, :] for c in range(8)], [xb] * 8, D, EPS)
                for c in range(8):
                    hn = gT[NFT - 8 + c]
                    k.dve.op(lambda c=c, hn=hn, xb=xb: nc.vector.scalar_tensor_tensor(
                        out=hn[:, :], in0=xb[:, c, :], scalar=vnext[:, c:c + 1], in1=rstd[:, :],
                        op0=ALU.mult, op1=ALU.mult), reads=[xb, vnext, rstd], writes=[hn])
                    k.sp.dma(dr["hT"][:, c, t0:t0 + TB], hn[:, :], hn, reads=[hn], writes=[dr["hT_buf"]])
            else:
                rstd = nrm.stats([xb[:, c, :] for c in range(8)], [xb] * 8, D, EPS)
                for c in range(8):
                    k.dve.op(lambda c=c, xb=xb: nc.vector.scalar_tensor_tensor(
                        out=xb[:, c, :], in0=xb[:, c, :], scalar=vec[:, o_fg + c:o_fg + c + 1], in1=rstd[:, :],
                        op0=ALU.mult, op1=ALU.mult), reads=[xb, vec, rstd], writes=[xb])
                k.sp.dma(dr["outT"][:, :, t0:t0 + TB], xb[:, :, :], xb, reads=[xb], writes=[dr["outT_buf"]])


MISC_LAYOUT = [("ones", 128), ("ones2", 128), ("ident", 128), ("cmask", 512), ("mask4", 512), ("bd", 256),
               ("hm", 4), ("smask", 128), ("imaskT", 128)]
MOFF = {}
_o = 0
for _n, _c in MISC_LAYOUT:
    MOFF[_n] = (_o, _o + _c)
    _o += _c
NMISC = _o


def make_misc():
    m = np.zeros((128, NMISC), np.float32)
    p = np.arange(128)[:, None]
    f = lambda n: np.arange(n)[None, :]
    m[:, slice(*MOFF["ones"])] = 1.0
    m[:, slice(*MOFF["ones2"])] = (p // 64 == f(128) // 64)
    m[:, slice(*MOFF["ident"])] = (p == f(128))
    m[:, slice(*MOFF["cmask"])] = (f(512) % 128 != 0)
    m[:, slice(*MOFF["mask4"])] = (p <= f(512) % 128)
    m[:, slice(*MOFF["bd"])] = (p // 32 == f(256) // 64)
    m[:, slice(*MOFF["hm"])] = (p // 32 == f(4))
    m[:, slice(*MOFF["smask"])] = (p < f(128))
    m[:, slice(*MOFF["imaskT"])] = (p > f(128))
    return m


def load_misc(k, es, name, cst, names, dtype):
    out = {}
    for n in names:
        a, b = MOFF[n]
        t = k.sb(es, f"{name}_{n}", [128, b - a], dtype)
        if dtype == BF16:
            load_cast(k, t, t[:, :], cst["misc"][:, a:b])
        else:
            k.sp.dma(t[:, :], cst["misc"][:, a:b], t, writes=[t])
        out[n] = t
    return out


def phase_gla(k, T, l, dr, cst):
    nc = k.nc
    C = 128
    NCH = TB // C
    w_in = dr["w_in"]
    G0 = 704
    with k.phase() as es:
        vec = k.sb(es, "g_vec", [128, NV], F32)
        k.sp.dma(vec[:, :], dr["vecs"][l], vec, writes=[vec])
        cb = load_misc(k, es, "g", cst, ["ones2", "ident", "mask4", "bd"], BF16)
        cf = load_misc(k, es, "gf", cst, ["cmask", "hm"], F32)
        ones2, ident, mask4, bd, cmask, hm = cb["ones2"], cb["ident"], cb["mask4"], cb["bd"], cf["cmask"], cf["hm"]
        wG = k.sb(es, "g_wG", [128, 8, 784], BF16)
        load_cast3(k, wG, wG[:, :, 0:256], w_in[l], G0, G0 + 256)
        load_cast3(k, wG, wG[:, :, 256:512], w_in[l], G0 + 528, G0 + 784)
        load_cast3(k, wG, wG[:, :, 512:768], w_in[l], G0 + 256, G0 + 512)
        load_cast3(k, wG, wG[:, :, 768:784], w_in[l], G0 + 512, G0 + 528)
        wgk = k.sb(es, "g_wgk", [16, 128], BF16)
        load_cast(k, wgk, wgk[:, :], dr["gla_w_gk"][l])
        nb = k.sb(es, "g_nb", [128, 1], F32)
        o_nb, o_ng = VOFF["gla_nb"][0], VOFF["gla_ng"][0]
        k.dve.op(lambda: nc.vector.tensor_scalar(out=nb[:, :], in0=vec[:, o_nb:o_nb + 1], scalar1=-1.0, scalar2=None,
                                                 op0=ALU.mult), reads=[vec], writes=[nb])
        hT = [k.sb(es, f"g_hT{i}", [128, 8, TB], BF16) for i in range(2)]
        q32 = k.sb(es, "g_q32", [128, TB], F32)
        k32 = k.sb(es, "g_k32", [128, TB], F32)
        glo = k.sb(es, "g_glo", [16, TB], BF16)
        Lt = k.sb(es, "g_L", [128, TB], F32)
        Bc = k.sb(es, "g_Bc", [128, TB], F32)
        eb = k.sb(es, "g_eb", [128, TB], F32)
        enb = k.sb(es, "g_enb", [128, TB], F32)
        kef = k.sb(es, "g_kef", [128, TB], F32)
        nbl = k.sb(es, "g_nbl", [128, NCH], F32)
        dec = k.sb(es, "g_dec", [128, NCH], F32)
        qth = [k.sb(es, f"g_qth{i}", [128, TB], BF16) for i in range(4)]
        kt = k.sb(es, "g_kt", [128, TB], BF16)
        kend = k.sb(es, "g_kend", [128, TB], BF16)
        kendT = k.sb(es, "g_kendT", [128, NCH, 128], BF16)
        vT = k.sb(es, "g_vT", [128, NCH, 256], BF16)
        sgo = [k.sb(es, f"g_sgo{i}", [128, TB], F32) for i in range(2)]
        attnT = k.sb(es, "g_attnT", [128, 4 * C], BF16)
        S4 = k.sb(es, "g_S4", [128, 256], F32)
        Sb = k.sb(es, "g_Sb", [128, 256], BF16)
        kvm = k.sb(es, "g_kvm", [128, 256], F32)
        oacc = [k.sb(es, f"g_oacc{i}", [128, TB], F32) for i in range(2)]
        yo = [k.sb(es, f"g_yo{i}", [128, TB], BF16) for i in range(2)]
        tmp = k.sb(es, "g_tmp", [128, TB], F32)
        tmp2 = k.sb(es, "g_tmp2", [128, TB], F32)
        sets = [(qth, kt, kendT, vT, dec, sgo),
                ([k.sb(es, f"g_qthB{i}", [128, TB], BF16) for i in range(4)], k.sb(es, "g_ktB", [128, TB], BF16),
                 k.sb(es, "g_kendTB", [128, NCH, 128], BF16), k.sb(es, "g_vTB", [128, NCH, 256], BF16),
                 k.sb(es, "g_decB", [128, NCH], F32), [k.sb(es, f"g_sgoB{i}", [128, TB], F32) for i in range(2)])]
        k.pool.op(lambda: nc.gpsimd.memset(S4[:, :], 0.0), writes=[S4])
        k.pool.op(lambda: nc.gpsimd.memset(Sb[:, :], 0.0), writes=[Sb])
        nrm = Norm(k, es, "g_n", TB, ones2)
        ps_p = [k.ps(es, f"g_psp{i}", [128, TB]) for i in range(2)]
        ps_at = k.ps(es, "g_psat", [128, 4 * C])
        ps_o = [k.ps(es, f"g_pso{i}", [128, C]) for i in range(2)]
        ps_kv = k.ps(es, "g_pskv", [128, 256])
        ps_tr = k.ps(es, "g_pstr", [128, 128])
        ip = 0

        def proj(pairs, reads, m=128, n=TB):
            nonlocal ip
            p = ps_p[ip % 2]
            ip += 1
            mm(k, p, p[0:m, 0:n], pairs, reads)
            return p

        qs = float(GDK ** -0.5)
        def prepG(b):
            t0 = b * TB
            h = hT[b % 2]
            qth, kt, kendT, vT, dec, sgo = sets[b % 2]
            k.sp.dma(h[:, :, :], dr["hT"][:, :, t0:t0 + TB], h, reads=[dr["hT_buf"]], writes=[h])
            yield
            p = proj([(wG[:, c, 0:128], h[:, c, :]) for c in range(8)], [wG, h])
            k.act.op(lambda p=p: nc.scalar.activation(out=q32[:, :], in_=p[:, :], func=AF.Copy), reads=[p], writes=[q32])
            yield
            p = proj([(wG[:, c, 128:256], h[:, c, :]) for c in range(8)], [wG, h])
            k.act.op(lambda p=p: nc.scalar.activation(out=k32[:, :], in_=p[:, :], func=AF.Copy), reads=[p], writes=[k32])
            yield
            p = proj([(wG[:, c, 768:784], h[:, c, :]) for c in range(8)], [wG, h], m=16)
            k.act.op(lambda p=p: nc.scalar.activation(out=glo[:, :], in_=p[0:16, :], func=AF.Copy), reads=[p], writes=[glo])
            yield
            for i in range(2):
                p = proj([(wG[:, c, 256 + i * 128:384 + i * 128], h[:, c, :]) for c in range(8)], [wG, h])
                k.act.op(lambda p=p, i=i: nc.scalar.activation(out=sgo[i][:, :], in_=p[:, :], func=AF.Silu),
                         reads=[p], writes=[sgo[i]])
                yield
            for c in range(NCH):
                p = proj([(h[:, c8, c * C:(c + 1) * C], wG[:, c8, 512:768]) for c8 in range(8)], [wG, h], n=256)
                k.act.op(lambda p=p, c=c: nc.scalar.activation(out=vT[:, c, :], in_=p[:, 0:256], func=AF.Copy),
                         reads=[p], writes=[vT])
                yield
            p = proj([(wgk[:, :], glo[:, :])], [wgk, glo])
            k.act.op(lambda p=p: nc.scalar.activation(out=Lt[:, :], in_=p[:, :], func=AF.Exp, scale=-1.0, bias=nb[:, 0:1]),
                     reads=[p, nb], writes=[Lt])
            k.act.op(lambda: nc.scalar.activation(out=Lt[:, :], in_=Lt[:, :], func=AF.Ln, bias=1.0), reads=[Lt], writes=[Lt])
            yield
            k.dve.op(lambda: nc.vector.tensor_tensor_scan(out=Bc[:, :], data0=cmask[:, :], data1=Lt[:, :], initial=0.0,
                                                          op0=ALU.mult, op1=ALU.add), reads=[cmask, Lt], writes=[Bc])
            yield
            k.act.op(lambda: nc.scalar.activation(out=eb[:, :], in_=Bc[:, :], func=AF.Exp, scale=-1.0 / 16), reads=[Bc], writes=[eb])
            k.act.op(lambda: nc.scalar.activation(out=enb[:, :], in_=Bc[:, :], func=AF.Exp, scale=1.0 / 16), reads=[Bc], writes=[enb])
            yield
            k.dve.op(lambda: nc.vector.tensor_scalar(out=nbl[:, :], in0=Bc[:, C - 1::C], scalar1=-1.0 / 16, scalar2=None,
                                                     op0=ALU.mult), reads=[Bc], writes=[nbl])
            k.act.op(lambda: nc.scalar.activation(out=dec[:, :], in_=nbl[:, :], func=AF.Exp), reads=[nbl], writes=[dec])
            yield
            for c in range(NCH):
                k.act.op(lambda c=c: nc.scalar.activation(out=kef[:, c * C:(c + 1) * C], in_=Bc[:, c * C:(c + 1) * C],
                                                          func=AF.Exp, scale=1.0 / 16, bias=nbl[:, c:c + 1]),
                         reads=[Bc, nbl], writes=[kef])
                yield
            k.dve.op(lambda: nc.vector.tensor_tensor(out=tmp[:, :], in0=q32[:, :], in1=eb[:, :], op=ALU.mult),
                     reads=[q32, eb], writes=[tmp])
            for hh in range(4):
                k.dve.op(lambda hh=hh: nc.vector.tensor_scalar(out=qth[hh][:, :], in0=tmp[:, :], scalar1=hm[:, hh:hh + 1],
                                                               scalar2=qs, op0=ALU.mult, op1=ALU.mult),
                         reads=[tmp, hm], writes=[qth[hh]])
                yield
            k.dve.op(lambda: nc.vector.tensor_tensor(out=kt[:, :], in0=k32[:, :], in1=enb[:, :], op=ALU.mult),
                     reads=[k32, enb], writes=[kt])
            yield
            k.dve.op(lambda: nc.vector.tensor_tensor(out=kend[:, :], in0=k32[:, :], in1=kef[:, :], op=ALU.mult),
                     reads=[k32, kef], writes=[kend])
            yield
            for c in range(NCH):
                cs = slice(c * C, (c + 1) * C)
                mm(k, ps_tr, ps_tr[:, :], [(kend[:, cs], ident[:, :])], [kend, ident])
                k.act.op(lambda c=c: nc.scalar.activation(out=kendT[:, c, :], in_=ps_tr[:, :], func=AF.Copy),
                         reads=[ps_tr], writes=[kendT])
                yield
        def drain(g):
            if g is not None:
                for _ in g:
                    pass

        def step():
            nonlocal gp
            if gp is not None:
                try:
                    next(gp)
                except StopIteration:
                    gp = None

        gp = None
        drain(prepG(0))
        for b in range(T // TB):
            t0 = b * TB
            qth, kt, kendT, vT, dec, sgo = sets[b % 2]
            gp = prepG(b + 1) if (b + 1) * TB < T else None
            for c in range(NCH):
                cs = slice(c * C, (c + 1) * C)
                step()
                for hh in range(4):
                    k.pe.op(lambda hh=hh, cs=cs: nc.tensor.matmul(ps_at[:, hh * C:(hh + 1) * C], lhsT=kt[:, cs],
                                                                  rhs=qth[hh][:, cs], start=True, stop=True),
                            reads=[kt, qth[hh]], writes=[ps_at], inc=(hh == 3))
                k.dve.op(lambda: nc.vector.tensor_tensor(out=attnT[:, :], in0=ps_at[:, :], in1=mask4[:, :], op=ALU.mult),
                         reads=[ps_at, mask4], writes=[attnT])
                step()
                for pr in range(2):
                    po = ps_o[pr]
                    for hl in range(2):
                        hh = pr * 2 + hl
                        osl = po[hl * 64:(hl + 1) * 64, :]
                        k.pe.op(lambda hh=hh, osl=osl, c=c: nc.tensor.matmul(
                            osl, lhsT=vT[:, c, hh * 64:(hh + 1) * 64], rhs=attnT[:, hh * C:(hh + 1) * C],
                            start=True, stop=False), reads=[vT, attnT], writes=[po], inc=False)
                        k.pe.op(lambda hh=hh, osl=osl, cs=cs: nc.tensor.matmul(
                            osl, lhsT=Sb[:, hh * 64:(hh + 1) * 64], rhs=qth[hh][:, cs], start=False, stop=True),
                            reads=[Sb, qth[hh]], writes=[po], inc=True)
                    k.act.op(lambda pr=pr, po=po, cs=cs: nc.scalar.activation(out=oacc[pr][:, cs], in_=po[:, :],
                                                                              func=AF.Copy), reads=[po], writes=[oacc[pr]])
                step()
                mm(k, ps_kv, ps_kv[:, :], [(kendT[:, c, :], vT[:, c, :])], [kendT, vT])
                k.dve.op(lambda: nc.vector.tensor_tensor(out=kvm[:, :], in0=ps_kv[:, :], in1=bd[:, :], op=ALU.mult),
                         reads=[ps_kv, bd], writes=[kvm])
                k.dve.op(lambda c=c: nc.vector.scalar_tensor_tensor(out=S4[:, :], in0=S4[:, :], scalar=dec[:, c:c + 1],
                                                                    in1=kvm[:, :], op0=ALU.mult, op1=ALU.add),
                         reads=[S4, dec, kvm], writes=[S4])
                k.act.op(lambda: nc.scalar.activation(out=Sb[:, :], in_=S4[:, :], func=AF.Copy), reads=[S4], writes=[Sb])
                step()
                step()
            for pr in range(2):
                rstd = nrm.stats([oacc[pr][:, :]], [oacc[pr]], GDV, EPS)
                k.dve.op(lambda pr=pr: nc.vector.tensor_tensor(out=tmp2[:, :], in0=oacc[pr][:, :], in1=rstd[:, :],
                                                               op=ALU.mult), reads=[oacc[pr], rstd], writes=[tmp2])
                k.dve.op(lambda pr=pr: nc.vector.scalar_tensor_tensor(
                    out=yo[pr][:, :], in0=tmp2[:, :], scalar=vec[:, o_ng:o_ng + 1], in1=sgo[pr][:, :],
                    op0=ALU.mult, op1=ALU.mult), reads=[tmp2, vec, sgo[pr]], writes=[yo[pr]])
                k.sp.dma(dr["yT"][:, 4 + pr, t0:t0 + TB], yo[pr][:, :], yo[pr], reads=[yo[pr]], writes=[dr["yT_buf"]])
            drain(gp)
            gp = None


def TT(e, out, a, b, op, R, W):
    e.op(lambda: e.eng.tensor_tensor(out=out, in0=a, in1=b, op=op), reads=R, writes=W)


def TS(e, out, a, s1, s2, op0, op1, R, W):
    e.op(lambda: e.eng.tensor_scalar(out=out, in0=a, scalar1=s1, scalar2=s2, op0=op0, op1=op1), reads=R, writes=W)


def STT(k, out, a, sc, b, op0, op1, R, W):
    k.dve.op(lambda: k.nc.vector.scalar_tensor_tensor(out=out, in0=a, scalar=sc, in1=b, op0=op0, op1=op1),
             reads=R, writes=W)


def ACTF(k, out, in_, func, R, W, scale=1.0, bias=0.0):
    k.act.op(lambda: k.nc.scalar.activation(out=out, in_=in_, func=func, scale=scale, bias=bias), reads=R, writes=W)


def MM(k, out, lhsT, rhs, R, W, start=True, stop=True, inc=True):
    k.pe.op(lambda: k.nc.tensor.matmul(out, lhsT=lhsT, rhs=rhs, start=start, stop=stop), reads=R, writes=W, inc=inc)


class _Stop(Exception):
    pass


STOP = [None]


def chk(n):
    if STOP[0] == n:
        raise _Stop()


def phase_rwkv(k, T, l, dr, cst):
    try:
        _phase_rwkv(k, T, l, dr, cst)
    except _Stop:
        pass


NS = 3


def _phase_rwkv(k, T, l, dr, cst):
    nc = k.nc
    C = 128
    NCH = TB // C
    R0 = 704 + 784
    DEC = float(np.exp(-0.5))
    with k.phase() as es:
        vec = k.sb(es, "r_vec", [128, NV], F32)
        k.sp.dma(vec[:, :], dr["vecs"][l], vec, writes=[vec])
        cb = load_misc(k, es, "r", cst, ["ones2", "ident"], BF16)
        cf = load_misc(k, es, "rf", cst, ["cmask", "smask", "mask4", "imaskT", "ident"], F32)
        ones2, ident, cmask = cb["ones2"], cb["ident"], cf["cmask"]
        m_si = k.sb(es, "r_msi", [128, 2, 256], BF16)
        m_tt = k.sb(es, "r_mtt", [128, 2, 128], BF16)
        for hl in range(2):
            k.pool.op(lambda hl=hl: nc.gpsimd.tensor_copy(out=m_si[:, hl, 0:128], in_=cf["smask"][:, :]),
                      reads=[cf["smask"]], writes=[m_si])
            k.pool.op(lambda hl=hl: nc.gpsimd.tensor_copy(out=m_si[:, hl, 128:256], in_=cf["mask4"][:, 0:128]),
                      reads=[cf["mask4"]], writes=[m_si])
            k.pool.op(lambda hl=hl: nc.gpsimd.tensor_copy(out=m_tt[:, hl, :], in_=cf["imaskT"][:, :]),
                      reads=[cf["imaskT"]], writes=[m_tt])
        I2 = k.sb(es, "r_I2", [128, 64], F32)
        k.pool.op(lambda: nc.gpsimd.tensor_copy(out=I2[0:64, :], in_=cf["ident"][0:64, 0:64]), reads=[cf["ident"]], writes=[I2])
        k.pool.op(lambda: nc.gpsimd.tensor_copy(out=I2[64:128, :], in_=cf["ident"][64:128, 64:128]), reads=[cf["ident"]], writes=[I2])
        wR = k.sb(es, "r_wR", [128, 8, 1024], BF16)
        load_cast3(k, wR, wR[:, :, :], dr["w_in"][l], R0, R0 + 1024)
        wl_w = k.sb(es, "r_wlw", [128, 256], BF16)
        wl_a = k.sb(es, "r_wla", [128, 256], BF16)
        k.pool.op(lambda: nc.gpsimd.memset(wl_w[:, :], 0.0), writes=[wl_w])
        k.pool.op(lambda: nc.gpsimd.memset(wl_a[:, :], 0.0), writes=[wl_a])
        load_cast(k, wl_w, wl_w[0:64, :], dr["rwkv_w2"][l])
        load_cast(k, wl_a, wl_a[64:128, :], dr["rwkv_a2"][l])
        hm2 = k.sb(es, "r_hm2", [128, 2], F32)
        nhm2 = k.sb(es, "r_nhm2", [128, 2], F32)
        I2h = [k.sb(es, f"r_I2h{i}", [128, 64], F32) for i in range(2)]
        for i in range(2):
            k.pool.op(lambda i=i: nc.gpsimd.memset(I2h[i][:, :], 0.0), writes=[I2h[i]])
            k.pool.op(lambda i=i: nc.gpsimd.tensor_copy(out=I2h[i][i * 64:(i + 1) * 64, :],
                                                        in_=cf["ident"][i * 64:(i + 1) * 64, i * 64:(i + 1) * 64]),
                      reads=[cf["ident"]], writes=[I2h[i]])
        k.pool.op(lambda: nc.gpsimd.memset(hm2[:, :], 0.0), writes=[hm2])
        k.pool.op(lambda: nc.gpsimd.memset(hm2[0:64, 0:1], 1.0), writes=[hm2])
        k.pool.op(lambda: nc.gpsimd.memset(hm2[64:128, 1:2], 1.0), writes=[hm2])
        TS(k.dve, nhm2[:, :], hm2[:, :], -1.0, None, ALU.mult, ALU.bypass, [hm2], [nhm2])
        wg2 = k.sb(es, "r_wg2", [128, 256], BF16)
        load_cast(k, wg2, wg2[:, :], dr["rwkv_g2"][l])
        o_mu, o_w0, o_a0, o_kk, o_ka, o_rk, o_lg, o_lb = (VOFF[n][0] for n in (
            "rw_mu", "rw_w0", "rw_a0", "rw_kk", "rw_ka", "rw_rk", "rw_lng", "rw_lnb"))
        omm = k.sb(es, "r_omm", [128, 8], F32)
        omka = k.sb(es, "r_omka", [128, 2], F32)
        TS(k.dve, omm[:, :], vec[:, o_mu:o_mu + 8], -1.0, 1.0, ALU.mult, ALU.add, [vec], [omm])
        TS(k.dve, omka[:, :], vec[:, o_ka:o_ka + 2], -1.0, 1.0, ALU.mult, ALU.add, [vec], [omka])

        hTs = [k.sb(es, f"r_hT{i}", [128, 8, TB + 1], BF16) for i in range(2)]
        def f32t(n): return k.sb(es, "r_" + n, [128, TB], F32)
        def bft(n): return k.sb(es, "r_" + n, [128, TB], BF16)
        tprev = f32t("tprev")
        xr, xk, xv = [f32t(f"xr{i}") for i in range(2)], [f32t(f"xk{i}") for i in range(2)], [f32t(f"xv{i}") for i in range(2)]
        xlo = bft("xlo")
        sgl = bft("sgl")
        gate = [f32t(f"gate{i}") for i in range(2)]
        bon = [f32t(f"bon{i}") for i in range(2)]
        aa, kkt, kmod, cc, t1, t2, t3 = f32t("aa"), f32t("kk"), f32t("kmod"), f32t("cc"), f32t("t1"), f32t("t2"), f32t("t3")
        tb1 = bft("tb1")
        ar = [[k.sb(es, f"r_ar{i}{j}", [128, NCH, 256], BF16) for j in range(2)] for i in range(2)]
        Xw_ = [[k.sb(es, f"r_Xw{p}{i}", [128, 128], BF16) for i in range(2)] for p in range(NS)]
        for p_ in range(NS):
            for i in range(2):
                k.pool.op(lambda i=i, p_=p_: nc.gpsimd.memset(Xw_[p_][i][:, :], 0.0), writes=[Xw_[p_][i]])
        bt = [bft(f"bt{i}") for i in range(2)]
        ktl = [bft(f"ktl{i}") for i in range(2)]
        Bp = [bft(f"Bp{i}") for i in range(2)]
        Kp = [bft(f"Kp{i}") for i in range(2)]
        vb = [bft(f"vb{i}") for i in range(2)]
        nbl = k.sb(es, "r_nbl", [128, NCH], F32)
        pC = [k.sb(es, f"r_pC{i}", [128, NCH], F32) for i in range(2)]
        TM_ = [k.sb(es, f"r_TM{p}", [128, 4, 128], BF16) for p in range(NS)]
        AbRb_ = [k.sb(es, f"r_AbRb{p}", [128, 2, 256], BF16) for p in range(NS)]
        AkRk_ = [k.sb(es, f"r_AkRk{p}", [128, 2, 256], BF16) for p in range(NS)]
        PP_ = [[k.sb(es, f"r_PP{p}{i}", [128, 2, 256], BF16) for i in range(2)] for p in range(NS)]
        X_ = [k.sb(es, f"r_X{p}", [128, 2, 128], BF16) for p in range(NS)]
        QpT = [[bft(f"QpT{i}{j}") for j in range(2)] for i in range(2)]
        Y0T = [f32t(f"Y0T{i}") for i in range(2)]
        MT = [[k.sb(es, f"r_MT{i}{j}", [128, NCH, 64], BF16) for j in range(2)] for i in range(2)]
        Gm = [k.sb(es, f"r_G{i}", [128, NCH, 64], F32) for i in range(2)]
        Hb = [k.sb(es, f"r_H{i}", [128, 64], BF16) for i in range(2)]
        yacc = [f32t(f"yacc{i}") for i in range(2)]
        yo = [bft(f"yo{i}") for i in range(2)]
        o1, ob1 = f32t("o1"), bft("ob1")
        sets = [(gate, bon, vb, ar, bt, ktl, pC, Bp, Kp)]
        sets.append(([f32t(f"gateB{i}") for i in range(2)], [f32t(f"bonB{i}") for i in range(2)],
                     [bft(f"vbB{i}") for i in range(2)],
                     [[k.sb(es, f"r_arB{i}{j}", [128, NCH, 256], BF16) for j in range(2)] for i in range(2)],
                     [bft(f"btB{i}") for i in range(2)], [bft(f"ktlB{i}") for i in range(2)],
                     [k.sb(es, f"r_pCB{i}", [128, NCH], F32) for i in range(2)],
                     [bft(f"BpB{i}") for i in range(2)], [bft(f"KpB{i}") for i in range(2)]))
        for i in range(2):
            k.pool.op(lambda i=i: nc.gpsimd.memset(Hb[i][:, :], 0.0), writes=[Hb[i]])
        ps_p = [k.ps(es, f"r_psp{i}", [128, TB]) for i in range(2)]
        nrm = Norm(k, es, "r_n", TB, ones2, ps=ps_p[0])
        ps_x_ = [k.ps(es, f"r_bx{p}", [128, TB]) for p in range(NS)]
        ps_pp_ = [k.ps(es, f"r_pspp{p}", [128, 2, 256]) for p in range(NS)]
        banks = [ps_p[0], ps_p[1]]
        ip = 0
        isx = 0

        def bank():
            nonlocal isx
            isx += 1
            return banks[isx % 2]

        def proj(pairs, reads, n=TB):
            nonlocal ip
            p = ps_p[ip % 2]
            ip += 1
            mm(k, p, p[:, 0:n], pairs, reads)
            return p

        def prepR(b):
            t0 = b * TB
            h = hTs[b % 2]
            gate, bon, vb, ar, bt, ktl, pC, Bp, Kp = sets[b % 2]
            if b == 0:
                k.pool.op(lambda h=h: nc.gpsimd.memset(h[:, :, 0:1], 0.0), writes=[h])
                k.sp.dma(h[:, :, 1:TB + 1], dr["hT"][:, :, 0:TB], h, reads=[dr["hT_buf"]], writes=[h])
            else:
                k.sp.dma(h[:, :, :], dr["hT"][:, :, t0 - 1:t0 + TB], h, reads=[dr["hT_buf"]], writes=[h])

            def mixed(j, out, func=AF.Identity, out_bufs=None):
                cols = slice(j * 128, (j + 1) * 128)
                pp = proj([(wR[:, c, cols], h[:, c, 0:TB]) for c in range(8)], [wR, h])
                ACTF(k, tprev[:, :], pp[:, :], AF.Copy if False else AF.Identity, [pp, vec], [tprev],
                     scale=vec[:, o_mu + j:o_mu + j + 1])
                pc_ = proj([(wR[:, c, cols], h[:, c, 1:TB + 1]) for c in range(8)], [wR, h])
                STT(k, t1[:, :], pc_[:, :], omm[:, j:j + 1], tprev[:, :], ALU.mult, ALU.add, [pc_, omm, tprev], [t1])
                return t1

            yield
            mixed(6, None)
            ACTF(k, xlo[0:64, :], t1[0:64, :], AF.Tanh, [t1], [xlo])
            ACTF(k, xlo[64:128, :], t1[64:128, :], AF.Copy, [t1], [xlo])
            yield
            mixed(7, None)
            ACTF(k, sgl[:, :], t1[:, :], AF.Sigmoid, [t1], [sgl])
            yield
            for j in range(2):
                mixed(j, None)
                ACTF(k, xr[j][:, :], t1[:, :], AF.Copy, [t1], [xr[j]])
                yield
                mixed(2 + j, None)
                ACTF(k, xk[j][:, :], t1[:, :], AF.Copy, [t1], [xk[j]])
                yield
                mixed(4 + j, None)
                ACTF(k, xv[j][:, :], t1[:, :], AF.Copy, [t1], [xv[j]])
                yield
            for pr in range(2):
                pcols = slice(pr * 128, (pr + 1) * 128)
                r_, k_, v_ = xr[pr], xk[pr], xv[pr]
                p = proj([(wg2[:, pcols], sgl[:, :])], [wg2, sgl])
                ACTF(k, gate[pr][:, :], p[:, :], AF.Copy, [p], [gate[pr]])
                yield
                p = proj([(wl_a[:, pcols], xlo[:, :])], [wl_a, xlo])
                ACTF(k, aa[:, :], p[:, :], AF.Sigmoid, [p, vec], [aa], bias=vec[:, o_a0 + pr:o_a0 + pr + 1])
                yield
                p = proj([(wl_w[:, pcols], xlo[:, :])], [wl_w, xlo])
                ACTF(k, t2[:, :], p[:, :], AF.Sigmoid, [p, vec], [t2], bias=vec[:, o_w0 + pr:o_w0 + pr + 1])
                TS(k.dve, t2[:, :], t2[:, :], DEC, None, ALU.mult, ALU.bypass, [t2], [t2])
                k.dve.op(lambda: nc.vector.tensor_tensor_scan(out=cc[:, :], data0=cmask[:, :], data1=t2[:, :], initial=0.0,
                                                              op0=ALU.mult, op1=ALU.add), reads=[cmask, t2], writes=[cc])
                yield
                TS(k.dve, kkt[:, :], k_[:, :], vec[:, o_kk + pr:o_kk + pr + 1], None, ALU.mult, ALU.bypass, [k_, vec], [kkt])
                rstd = nrm.stats([kkt[:, :]], [kkt], 1.0, 1e-24)
                TT(k.dve, kkt[:, :], kkt[:, :], rstd[:, :], ALU.mult, [kkt, rstd], [kkt])
                yield
                TS(k.dve, t3[:, :], aa[:, :], vec[:, o_ka + pr:o_ka + pr + 1], omka[:, pr:pr + 1], ALU.mult, ALU.add,
                   [aa, vec, omka], [t3])
                TT(k.dve, kmod[:, :], k_[:, :], t3[:, :], ALU.mult, [k_, t3], [kmod])
                yield
                STT(k, tb1[:, :], r_[:, :], vec[:, o_rk + pr:o_rk + pr + 1], kmod[:, :], ALU.mult, ALU.mult,
                    [r_, vec, kmod], [tb1])
                p = proj([(ones2[:, :], tb1[:, :])], [ones2, tb1])
                TT(k.dve, bon[pr][:, :], p[:, :], v_[:, :], ALU.mult, [p, v_], [bon[pr]])
                ACTF(k, vb[pr][:, :], v_[:, :], AF.Copy, [v_], [vb[pr]])
                yield
                ACTF(k, t3[:, :], cc[:, :], AF.Exp, [cc], [t3], scale=-1.0)
                for c in range(NCH):
                    for hl in range(2):
                        STT(k, ar[pr][hl][:, c, 128:256], r_[:, c * C:(c + 1) * C], hm2[:, hl:hl + 1],
                            t3[:, c * C:(c + 1) * C], ALU.mult, ALU.mult, [r_, t3, hm2], [ar[pr][hl]])
                    yield
                TT(k.dve, t3[:, :], cc[:, :], t2[:, :], ALU.subtract, [cc, t2], [t3])
                ACTF(k, t3[:, :], t3[:, :], AF.Exp, [t3], [t3], scale=-1.0)
                for c in range(NCH):
                    for hl in range(2):
                        STT(k, ar[pr][hl][:, c, 0:128], kkt[:, c * C:(c + 1) * C], nhm2[:, hl:hl + 1],
                            t3[:, c * C:(c + 1) * C], ALU.mult, ALU.mult, [kkt, t3, nhm2], [ar[pr][hl]])
                    yield
                TT(k.dve, aa[:, :], aa[:, :], kkt[:, :], ALU.mult, [aa, kkt], [aa])
                ACTF(k, t3[:, :], cc[:, :], AF.Exp, [cc], [t3], scale=1.0)
                TT(k.dve, bt[pr][:, :], aa[:, :], t3[:, :], ALU.mult, [aa, t3], [bt[pr]])
                TT(k.dve, ktl[pr][:, :], kmod[:, :], t3[:, :], ALU.mult, [kmod, t3], [ktl[pr]])
                yield
                TS(k.dve, nbl[:, :], cc[:, C - 1::C], -1.0, None, ALU.mult, ALU.bypass, [cc], [nbl])
                ACTF(k, pC[pr][:, :], nbl[:, :], AF.Exp, [nbl], [pC[pr]])
                for c in range(NCH):
                    ACTF(k, t3[:, c * C:(c + 1) * C], cc[:, c * C:(c + 1) * C], AF.Exp, [cc, nbl], [t3], scale=1.0,
                         bias=nbl[:, c:c + 1])
                TT(k.dve, Bp[pr][:, :], aa[:, :], t3[:, :], ALU.mult, [aa, t3], [Bp[pr]])
                TT(k.dve, Kp[pr][:, :], kmod[:, :], t3[:, :], ALU.mult, [kmod, t3], [Kp[pr]])

                yield

        def drain(g):
            if g is not None:
                for _ in g:
                    pass

        drain(prepR(0))
        for b in range(T // TB):
            t0 = b * TB
            gate, bon, vb, ar, bt, ktl, pC, Bp, Kp = sets[b % 2]
            gp = prepR(b + 1) if (b + 1) * TB < T else None
            hs = [slice(0, 64), slice(64, 128)]

            def stageA(pr, c, sid):
                cs = slice(c * C, (c + 1) * C)
                TM, AbRb, AkRk, PP, X, Xw = TM_[sid], AbRb_[sid], AkRk_[sid], PP_[sid], X_[sid], Xw_[sid]
                ps_x, ps_pp = ps_x_[sid], ps_pp_[sid]
                s_ = bank()
                MM(k, s_[:, 0:128], ar[pr][0][:, c, 0:128], ident[:, :], [ar[pr][0], ident], [s_], stop=False, inc=False)
                MM(k, s_[:, 0:128], ar[pr][1][:, c, 0:128], ident[:, :], [ar[pr][1], ident], [s_], start=False, inc=False)
                for n_, src in enumerate((Bp[pr][:, cs], Kp[pr][:, cs], vb[pr][:, cs])):
                    MM(k, s_[:, (n_ + 1) * 128:(n_ + 2) * 128], src, ident[:, :], [Bp[pr], Kp[pr], vb[pr], ident],
                       [s_], inc=(n_ == 2))
                ACTF(k, TM[:, :, :], s_[:, :], AF.Copy, [s_], [TM])
                yield
                s1 = bank()
                for hl in range(2):
                    MM(k, s1[:, hl * 256:(hl + 1) * 256], bt[pr][:, cs], ar[pr][hl][:, c, :], [bt[pr], ar[pr][hl]],
                       [s1], inc=(hl == 1))
                TT(k.dve, AbRb[:, :, :], s1[:, :], m_si[:, :, :], ALU.mult, [s1, m_si], [AbRb])
                yield
                s2 = bank()
                for hl in range(2):
                    MM(k, s2[:, hl * 256:(hl + 1) * 256], ktl[pr][:, cs], ar[pr][hl][:, c, :], [ktl[pr], ar[pr][hl]],
                       [s2], inc=(hl == 1))
                TT(k.dve, AkRk[:, :, :], s2[:, :], m_si[:, :, :], ALU.mult, [s2, m_si], [AkRk])
                yield
                s3 = bank()
                for hl in range(2):
                    MM(k, s3[:, hl * 128:(hl + 1) * 128], ar[pr][hl][:, c, 0:128], bt[pr][:, cs], [bt[pr], ar[pr][hl]],
                       [s3], inc=(hl == 1))
                P0 = PP[0]
                TT(k.dve, P0[:, :, 0:128], s3[:, 0:256], m_tt[:, :, :], ALU.mult, [s3, m_tt], [P0])
                k.pool.op(lambda P0=P0: nc.gpsimd.tensor_copy(out=P0[:, :, 128:256], in_=AbRb[:, :, 0:128]),
                          reads=[AbRb], writes=[P0])
                yield
                for hl in range(2):
                    MM(k, ps_x[:, hl * 128 + 64:(hl + 1) * 128], AkRk[:, hl, 0:128], TM[:, 3, hs[hl]], [AkRk, TM],
                       [ps_x], inc=(hl == 1))
                for hl in range(2):
                    ACTF(k, X[:, hl, 0:64], TM[:, 0, hs[hl]], AF.Copy, [TM], [X])
                    ACTF(k, X[:, hl, 64:128], ps_x[:, hl * 128 + 64:(hl + 1) * 128], AF.Copy, [ps_x], [X])
                yield
                for lv in range(7):
                    Pc = PP[lv % 2]
                    for hl in range(2):
                        MM(k, ps_x[:, hl * 128:(hl + 1) * 128], Pc[:, hl, 128:256], X[:, hl, :], [Pc, X], [ps_x],
                           inc=(hl == 1))
                    if lv < 6:
                        Pn = PP[(lv + 1) % 2]
                        for hl in range(2):
                            MM(k, ps_pp[:, hl, 0:128], Pc[:, hl, 128:256], Pc[:, hl, 0:128], [Pc], [ps_pp], inc=False)
                            MM(k, ps_pp[:, hl, 128:256], Pc[:, hl, 0:128], Pc[:, hl, 128:256], [Pc], [ps_pp],
                               inc=(hl == 1))
                        ACTF(k, Pn[:, :, :], ps_pp[:, :, :], AF.Copy, [ps_pp], [Pn])
                    TT(k.dve, X[:, :, :], ps_x[:, 0:256], X[:, :, :], ALU.add, [ps_x, X], [X])
                    yield
                for hl in range(2):
                    ACTF(k, Xw[hl][:, hs[hl]], X[:, hl, 0:64], AF.Copy, [X], [Xw[hl]])
                for hl in range(2):
                    pq = bank()
                    MM(k, pq[:, 0:128], Xw[hl][:, :], AbRb[:, hl, 128:256], [Xw[hl], AbRb], [pq])
                    TT(k.dve, QpT[pr][hl][:, cs], pq[:, 0:128], ar[pr][hl][:, c, 128:256], ALU.add, [pq, ar[pr][hl]],
                       [QpT[pr][hl]])
                yield
                py0 = bank()
                for hl in range(2):
                    MM(k, py0[hs[hl], 0:128], X[:, hl, 64:128], AbRb[:, hl, 128:256], [X, AbRb], [py0], stop=False,
                       inc=False)
                    MM(k, py0[hs[hl], 0:128], TM[:, 3, hs[hl]], AkRk[:, hl, 128:256], [TM, AkRk], [py0], start=False,
                       inc=(hl == 1))
                ACTF(k, Y0T[pr][:, cs], py0[:, 0:128], AF.Copy, [py0], [Y0T[pr]])
                yield
                for hl in range(2):
                    pm = bank()
                    MM(k, pm[:, 0:64], Xw[hl][:, :], TM[:, 1, hs[hl]], [Xw[hl], TM], [pm])
                    STT(k, MT[pr][hl][:, c, :], I2h[hl][:, :], pC[pr][:, c:c + 1], pm[:, 0:64], ALU.mult, ALU.add,
                        [I2h[hl], pC[pr], pm], [MT[pr][hl]])
                yield
                pg = bank()
                for hl in range(2):
                    MM(k, pg[hs[hl], 0:64], TM[:, 1, hs[hl]], X[:, hl, 64:128], [X, TM], [pg], stop=False, inc=False)
                    MM(k, pg[hs[hl], 0:64], TM[:, 2, hs[hl]], TM[:, 3, hs[hl]], [TM], [pg], start=False,
                       inc=(hl == 1))
                ACTF(k, Gm[pr][:, c, :], pg[:, 0:64], AF.Copy, [pg], [Gm[pr]])

            pending = [(pr, c) for c in range(NCH) for pr in range(2)]
            active = {}
            while pending or active:
                for sid in range(NS):
                    if sid not in active and pending:
                        pr_, c_ = pending.pop(0)
                        active[sid] = stageA(pr_, c_, sid)
                for sid in list(active):
                    try:
                        next(active[sid])
                    except StopIteration:
                        del active[sid]
                if gp is not None:
                    try:
                        next(gp)
                    except StopIteration:
                        gp = None
            for c in range(NCH):
                cs = slice(c * C, (c + 1) * C)
                for pr in range(2):
                    py = bank()
                    for hl in range(2):
                        MM(k, py[hs[hl], 0:128], Hb[pr][:, :], QpT[pr][hl][:, cs], [Hb[pr], QpT[pr][hl]], [py], inc=(hl == 1))
                    TT(k.dve, yacc[pr][:, cs], py[:, 0:128], Y0T[pr][:, cs], ALU.add, [py, Y0T[pr]], [yacc[pr]])
                    ph = bank()
                    for hl in range(2):
                        MM(k, ph[hs[hl], 0:64], MT[pr][hl][:, c, :], Hb[pr][:, :], [MT[pr][hl], Hb[pr]], [ph], inc=(hl == 1))
                    TT(k.dve, Hb[pr][:, :], ph[:, 0:64], Gm[pr][:, c, :], ALU.add, [ph, Gm[pr]], [Hb[pr]])
            for pr in range(2):
                ACTF(k, ob1[:, :], yacc[pr][:, :], AF.Copy, [yacc[pr]], [ob1])
                p = proj([(ones2[:, :], ob1[:, :])], [ones2, ob1])
                STT(k, o1[:, :], p[:, :], -1.0 / RW_N, yacc[pr][:, :], ALU.mult, ALU.add, [p, yacc[pr]], [o1])
                rstd = nrm.stats([o1[:, :]], [o1], RW_N, RW_EPS)
                TT(k.dve, o1[:, :], o1[:, :], rstd[:, :], ALU.mult, [o1, rstd], [o1])
                TS(k.dve, o1[:, :], o1[:, :], vec[:, o_lg + pr:o_lg + pr + 1], vec[:, o_lb + pr:o_lb + pr + 1], ALU.mult,
                   ALU.add, [o1, vec], [o1])
                TT(k.dve, o1[:, :], o1[:, :], bon[pr][:, :], ALU.add, [o1, bon[pr]], [o1])
                TT(k.dve, yo[pr][:, :], o1[:, :], gate[pr][:, :], ALU.mult, [o1, gate[pr]], [yo[pr]])
                k.sp.dma(dr["yT"][:, 6 + pr, t0:t0 + TB], yo[pr][:, :], yo[pr], reads=[yo[pr]], writes=[dr["yT_buf"]])
            drain(gp)


def phase_out(k, T, l, dr, cst):
    nc = k.nc
    with k.phase() as es:
        vec = k.sb(es, "o_vec", [128, NV], F32)
        k.sp.dma(vec[:, :], dr["vecs"][l], vec, writes=[vec])
        ones = k.sb(es, "o_ones", [128, 128], BF16)
        load_cast(k, ones, ones[:, :], cst["ones"])
        wo = k.sb(es, "o_wo", [128, 8, D], BF16)
        load_cast3(k, wo, wo[:, :, :], dr["w_out"][l], 0, D)
        nrm = Norm(k, es, "o_n", TB, ones)
        xb = [k.sb(es, f"o_xb{i}", [128, 8, TB], F32) for i in range(2)]
        yb = [k.sb(es, f"o_yb{i}", [128, 8, TB], BF16) for i in range(2)]
        yn = k.sb(es, "o_yn", [128, 4, TB], BF16)
        ps = [k.ps(es, f"o_ps{i}", [128, TB]) for i in range(3)]
        o_g = VOFF["on_g"][0]
        xsrc, xsrc_buf = (dr["x_in"], Buf(None, "xin")) if l == 0 else (dr["xT"], dr["xT_buf"])
        def load_blk(b):
            x, y = xb[b % 2], yb[b % 2]
            k.sp.dma(x[:, :, :], xsrc[:, :, b * TB:(b + 1) * TB], x, reads=[xsrc_buf], writes=[x])
            k.sp.dma(y[:, :, :], dr["yT"][:, :, b * TB:(b + 1) * TB], y, reads=[dr["yT_buf"]], writes=[y])

        load_blk(0)
        for b in range(T // TB):
            t0 = b * TB
            x, y = xb[b % 2], yb[b % 2]
            if (b + 1) * TB < T:
                load_blk(b + 1)
            rstd = nrm.stats([y[:, c, :] for c in range(4)], [y] * 4, MLA_H * DV, EPS)
            for c in range(4):
                k.dve.op(lambda c=c, y=y: nc.vector.scalar_tensor_tensor(
                    out=yn[:, c, :], in0=y[:, c, :], scalar=vec[:, o_g + c:o_g + c + 1], in1=rstd[:, :],
                    op0=ALU.mult, op1=ALU.mult), reads=[y, vec, rstd], writes=[yn])
            for m in range(8):
                p = ps[m % 3]
                mm(k, p, p[:, :], [(wo[:, c, m * 128:(m + 1) * 128], yn[:, c, :] if c < 4 else y[:, c, :])
                                   for c in range(8)], [wo, yn, y])
                k.dve.op(lambda m=m, p=p, x=x: nc.vector.tensor_tensor(out=x[:, m, :], in0=p[:, :], in1=x[:, m, :],
                                                                       op=ALU.add), reads=[p, x], writes=[x])
            k.sp.dma(dr["xT"][:, :, t0:t0 + TB], x[:, :, :], x, reads=[x], writes=[dr["xT_buf"]])


def build(T, shapes, phases=("h0", "mla", "gla", "rwkv", "out", "ffn"), depth=DEPTH, debug=()):
    nc = bass.Bass("TRN2", target_bir_lowering=False)
    dr = {}
    for n, shp in shapes.items():
        dr[n] = nc.dram_tensor(n, list(shp), F32, kind="ExternalInput").ap()
    dr["x_in"] = dr["xT_in"].rearrange("(c p) t -> p c t", p=128)

    def scratch(name, dtype):
        t = nc.dram_tensor(name, [D, T], dtype, kind="ExternalOutput" if name in debug else "Internal").ap()
        dr[name] = t.rearrange("(c p) t -> p c t", p=128)
        dr[name + "_buf"] = Buf(None, name)

    outT = nc.dram_tensor("outT", [D, T], F32, kind="ExternalOutput").ap()
    dr["outT"] = outT.rearrange("(c p) t -> p c t", p=128)
    dr["outT_buf"] = Buf(None, "outT")
    scratch("xT", F32)
    scratch("hT", BF16)
    scratch("yT", BF16)
    cst = {"ones": dr["c_ones"], "amask": dr["c_amask"], "cos4": dr["c_cos4"], "sin4": dr["c_sin4"],
           "misc": dr["c_misc"]}
    with contextlib.ExitStack() as es:
        k = K(nc, es)
        if "h0" not in phases:
            with k.phase() as pes:
                t = k.sb(pes, "cp", [128, 8, TB], F32)
                for b in range(T // TB):
                    k.sp.dma(t[:, :, :], dr["x_in"][:, :, b * TB:(b + 1) * TB], t, writes=[t])
                    k.sp.dma(dr["xT"][:, :, b * TB:(b + 1) * TB], t[:, :, :], t, reads=[t], writes=[dr["xT_buf"]])
        else:
            phase_h0(k, T, dr, cst)
        for l in range(depth):
            if "mla" in phases:
                phase_mla(k, T, l, dr, cst, (0, 1))
                phase_mla(k, T, l, dr, cst, (2, 3))
            if "gla" in phases:
                phase_gla(k, T, l, dr, cst)
            if "rwkv" in phases:
                phase_rwkv(k, T, l, dr, cst)
            if "out" in phases:
                phase_out(k, T, l, dr, cst)
            if "ffn" in phases:
                phase_ffn(k, T, l, dr, cst, last=(l == depth - 1))
        k.barrier()
    return nc


W_NAMES = ["w_in", "mla_w_uq", "mla_w_ukv", "gla_w_gk", "rwkv_w2", "rwkv_a2", "rwkv_g2", "w_out",
           "ffn_w_up", "ffn_w_down"]


def pack_vecs(inp, l):
    v = np.zeros((128, NV), np.float32)

    def put(name, arr, n):
        o, c = VOFF[name]
        assert c == n
        v[:, o:o + n] = _cols(arr, n)

    put("ln1_g", inp["ln1_g"][l], 8)
    put("ln2_g", inp["ln2_g"][l], 8)
    put("final_g", inp["final_g"], 8)
    put("qn_g", inp["mla_q_norm_g"][l], 3)
    put("kvn_g", inp["mla_kv_norm_g"][l], 2)
    put("on_g", inp["mla_out_norm_g"][l], 4)
    put("gla_nb", inp["gla_b_gk"][l], 1)
    put("gla_ng", np.tile(inp["gla_norm_g"][l], 2), 1)
    put("rw_mu", inp["rwkv_mu"][l], 8)
    put("rw_w0", inp["rwkv_w0"][l], 2)
    put("rw_a0", inp["rwkv_a0"][l], 2)
    put("rw_kk", inp["rwkv_k_k"][l], 2)
    put("rw_ka", inp["rwkv_k_a"][l], 2)
    put("rw_rk", inp["rwkv_r_k"][l].reshape(-1), 2)
    put("rw_lng", inp["rwkv_ln_g"][l], 2)
    put("rw_lnb", inp["rwkv_ln_b"][l], 2)
    cw = inp["ffn_conv_w"][l]
    put("cw0", cw[0], 44)
    put("cw1", cw[1], 44)
    put("cw2", cw[2], 44)
    put("cb", inp["ffn_conv_b"][l], 44)
    return v


def host_inputs(inp, b, T):
    m = {}
    m["xT_in"] = np.ascontiguousarray(np.asarray(inp["x"][b, :T], np.float32).T)
    m["vecs"] = np.stack([pack_vecs(inp, l) for l in range(DEPTH)])
    m["c_ones"] = np.ones((128, 128), np.float32)
    m["c_misc"] = make_misc()
    kk = np.arange(128)[:, None]
    qq = np.arange(TB)[None, :]
    m["c_amask"] = np.stack([np.where(kk + 128 * d <= qq, 0.0, -30000.0).astype(np.float32) for d in range(4)])
    inv = (np.float32(1.0) / (np.float32(10000.0) ** (np.arange(0, ROPE, 2, dtype=np.float32) / np.float32(ROPE)))
           ).astype(np.float32)
    ang = (np.arange(T, dtype=np.float32)[None, :] * inv[:, None]).astype(np.float32)
    cs, sn = np.cos(ang).astype(np.float32), np.sin(ang).astype(np.float32)
    m["c_cos4"] = np.ascontiguousarray(np.concatenate([cs, cs, cs, cs], 0))
    m["c_sin4"] = np.ascontiguousarray(np.concatenate([-sn, sn, -sn, sn], 0))
    for n in W_NAMES:
        m[n] = np.ascontiguousarray(np.asarray(inp[n], np.float32))
    return m


def run(inp, T, nb, **bk):
    maps = [host_inputs(inp, b, T) for b in range(nb)]
    shapes = {n: a.shape for n, a in maps[0].items()}
    nc = build(T, shapes, **bk)
    res = run_bass_kernel_spmd(nc, maps, core_ids=list(range(nb)))
    return res


def kernel(**inputs):
    inp = {n: np.asarray(a) for n, a in inputs.items()}
    B, T = inp["x"].shape[0], inp["x"].shape[1]
    ncores = 8
    maps = [host_inputs(inp, b % B, T) for b in range(ncores)]
    shapes = {n: a.shape for n, a in maps[0].items()}
    nc = build(T, shapes)
    res = run_bass_kernel_spmd(nc, maps, core_ids=list(range(ncores)))
    out = np.stack([np.ascontiguousarray(res.results[b]["outT"].T) for b in range(B)])
    return out.astype(np.float32)


def phase_h0(k, T, dr, cst):
    nc = k.nc
    with k.phase() as es:
        vec = k.sb(es, "h_vec", [128, NV], F32)
        k.sp.dma(vec[:, :], dr["vecs"][0], vec, writes=[vec])
        ones = k.sb(es, "h_ones", [128, 128], BF16)
        load_cast(k, ones, ones[:, :], cst["ones"])
        nrm = Norm(k, es, "h_n", TB, ones)
        xb = [k.sb(es, f"h_xb{i}", [128, 8, TB], F32) for i in range(2)]
        hT = [k.sb(es, f"h_hT{i}", [128, 8, TB], BF16) for i in range(2)]
        o_g = VOFF["ln1_g"][0]
        k.sp.dma(xb[0][:, :, :], dr["x_in"][:, :, 0:TB], xb[0], writes=[xb[0]])
        for b in range(T // TB):
            x, h = xb[b % 2], hT[b % 2]
            t0 = b * TB
            if (b + 1) * TB < T:
                xn = xb[(b + 1) % 2]
                k.sp.dma(xn[:, :, :], dr["x_in"][:, :, t0 + TB:t0 + 2 * TB], xn, writes=[xn])
            rstd = nrm.stats([x[:, c, :] for c in range(8)], [x] * 8, D, EPS)
            for c in range(8):
                k.dve.op(lambda c=c, x=x, h=h: nc.vector.scalar_tensor_tensor(
                    out=h[:, c, :], in0=x[:, c, :], scalar=vec[:, o_g + c:o_g + c + 1], in1=rstd[:, :],
                    op0=ALU.mult, op1=ALU.mult), reads=[x, vec, rstd], writes=[h])
            k.sp.dma(dr["hT"][:, :, t0:t0 + TB], h[:, :, :], h, reads=[h], writes=[dr["hT_buf"]])


def phase_mla(k, T, l, dr, cst, heads):
    nc = k.nc
    nh = len(heads)
    nblk = T // TB
    NKT = T // 128
    scale = float((NOPE + ROPE) ** -0.5)
    w_in, w_uq, w_ukv = dr["w_in"], dr["mla_w_uq"], dr["mla_w_ukv"]
    with k.phase() as es:
        vec = k.sb(es, "a_vec", [128, NV], F32)
        k.sp.dma(vec[:, :], dr["vecs"][l], vec, writes=[vec])
        ones = k.sb(es, "a_ones", [128, 128], BF16)
        load_cast(k, ones, ones[:, :], cst["ones"])
        masks = k.sb(es, "a_mask", [128, 4, TB], BF16)
        k.pool.dma(masks[:, :, :], cst["amask"].rearrange("d p n -> p d n"), masks, writes=[masks])
        ident = k.sb(es, "a_ident", [128, 128], BF16)
        load_cast(k, ident, ident[:, :], cst["misc"][:, MOFF["ident"][0]:MOFF["ident"][1]])
        wA = k.sb(es, "a_wA", [128, 8, 896], BF16)
        load_cast3(k, wA, wA[:, :, 0:640], w_in[l], 0, 640)
        for r in range(2):
            load_cast3(k, wA, wA[:, :, 640 + 64 * r:704 + 64 * r], w_in[l], 640, 704)
            load_cast3(k, wA, wA[:, :, 768 + 64 * r:800 + 64 * r], w_in[l], 672, 704)
            load_cast3(k, wA, wA[:, :, 800 + 64 * r:832 + 64 * r], w_in[l], 640, 672)
        wq = k.sb(es, "a_wq", [128, 3, 3 * nh * 128], BF16)
        ro = nh * 128
        so = 2 * nh * 128
        k.pool.op(lambda: nc.gpsimd.memset(wq[:, :, :], 0.0), writes=[wq])
        for i, h in enumerate(heads):
            b0 = h * 192
            load_cast3(k, wq, wq[:, :, i * 128:(i + 1) * 128], w_uq[l], b0, b0 + 128)
            load_cast3(k, wq, wq[:, :, ro + i * 128:ro + i * 128 + 64], w_uq[l], b0 + 128, b0 + 192)
            load_cast3(k, wq, wq[:, :, so + i * 128:so + i * 128 + 32], w_uq[l], b0 + 160, b0 + 192)
            load_cast3(k, wq, wq[:, :, so + i * 128 + 32:so + i * 128 + 64], w_uq[l], b0 + 128, b0 + 160)
        wk = k.sb(es, "a_wk", [128, 2, nh * 128], BF16)
        wv = k.sb(es, "a_wv", [128, 2, nh * 128], BF16)
        for i, h in enumerate(heads):
            load_cast3(k, wk, wk[:, :, i * 128:(i + 1) * 128], w_ukv[l], h * 256, h * 256 + 128)
            load_cast3(k, wv, wv[:, :, i * 128:(i + 1) * 128], w_ukv[l], h * 256 + 128, h * 256 + 256)
        kn = [k.sb(es, f"a_kn{i}", [128, T], BF16) for i in range(nh)]
        kr = k.sb(es, "a_kr", [128, T], BF16)
        vc = k.sb(es, "a_vc", [128, NKT, nh * 128], BF16)
        hT = [k.sb(es, f"a_hT{i}", [128, 8, TB], BF16) for i in range(2)]
        cq = k.sb(es, "a_cq", [128, 3, TB], F32)
        ckv = k.sb(es, "a_ckv", [128, 2, TB], F32)
        cqn = k.sb(es, "a_cqn", [128, 3, TB], BF16)
        ckvn = k.sb(es, "a_ckvn", [128, 2, TB], BF16)
        qn = [k.sb(es, f"a_qn{i}", [128, TB], BF16) for i in range(nh)]
        qr = [k.sb(es, f"a_qr{i}", [128, TB], BF16) for i in range(nh)]
        cos = k.sb(es, "a_cos", [128, TB], F32)
        sin = k.sb(es, "a_sin", [128, TB], F32)
        t1 = k.sb(es, "a_t1", [128, TB], F32)
        t2 = k.sb(es, "a_t2", [128, TB], F32)
        pT = [k.sb(es, f"a_pT{i}", [128, TB], BF16) for i in range(3)]
        oT = [k.sb(es, f"a_oT{i}", [128, TB], BF16) for i in range(2)]
        rcp = k.sb(es, "a_rcp", [128, TB], F32)
        paccs = [k.sb(es, f"a_pacc{i}", [128, TB], F32) for i in range(2)]
        oraw = k.sb(es, "a_oraw", [128, TB], F32)
        ones_f = k.sb(es, "a_onesf", [128, 128], F32)
        k.sp.dma(ones_f[:, :], cst["ones"], ones_f, writes=[ones_f])
        nrm = Norm(k, es, "a_n", TB, ones)
        ps_p = [k.ps(es, f"a_psp{i}", [128, TB]) for i in range(2)]
        ps_s = [k.ps(es, f"a_pss{i}", [128, TB]) for i in range(3)]
        ps_o = k.ps(es, "a_pso", [128, TB])
        ps_l = k.ps(es, "a_psl", [128, TB])
        o_qg, o_kg = VOFF["qn_g"][0], VOFF["kvn_g"][0]
        ip = 0
        ipt = 0

        def proj(lhs_list, rhs_list, reads, m=128, n=TB):
            nonlocal ip
            p = ps_p[ip % 2]
            ip += 1
            mm(k, p, p[0:m, 0:n], list(zip(lhs_list, rhs_list)), reads)
            return p

        kn_b = [[Buf(kn[i].ap[:, bb * TB:(bb + 1) * TB], f"kn{i}_{bb}") for bb in range(nblk)] for i in range(nh)]
        kr_b = [Buf(kr.ap[:, bb * TB:(bb + 1) * TB], f"kr_{bb}") for bb in range(nblk)]
        vc_b = [Buf(vc.ap[:, bb * 4:(bb + 1) * 4, :], f"vc_{bb}") for bb in range(nblk)]
        qn2 = [qn, [k.sb(es, f"a_qnB{i}", [128, TB], BF16) for i in range(nh)]]
        qr2 = [qr, [k.sb(es, f"a_qrB{i}", [128, TB], BF16) for i in range(nh)]]

        def prep(b):
            t0 = b * TB
            h = hT[b % 2]
            qn_, qr_ = qn2[b % 2], qr2[b % 2]
            k.sp.dma(h[:, :, :], dr["hT"][:, :, t0:t0 + TB], h, reads=[dr["hT_buf"]], writes=[h])
            k.sp.dma(cos[:, :], cst["cos4"][:, t0:t0 + TB], cos, writes=[cos])
            k.sp.dma(sin[:, :], cst["sin4"][:, t0:t0 + TB], sin, writes=[sin])
            yield
            for j in range(3):
                p = proj([wA[:, c, j * 128:(j + 1) * 128] for c in range(8)], [h[:, c, :] for c in range(8)], [wA, h])
                k.act.op(lambda p=p, j=j: nc.scalar.activation(out=cq[:, j, :], in_=p[:, :], func=AF.Copy),
                         reads=[p], writes=[cq])
                yield
            for j in range(2):
                p = proj([wA[:, c, 384 + j * 128:384 + (j + 1) * 128] for c in range(8)],
                         [h[:, c, :] for c in range(8)], [wA, h])
                k.act.op(lambda p=p, j=j: nc.scalar.activation(out=ckv[:, j, :], in_=p[:, :], func=AF.Copy),
                         reads=[p], writes=[ckv])
                yield
            rstd = nrm.stats([cq[:, j, :] for j in range(3)], [cq] * 3, QLORA, EPS)
            yield
            for j in range(3):
                k.dve.op(lambda j=j: nc.vector.scalar_tensor_tensor(
                    out=cqn[:, j, :], in0=cq[:, j, :], scalar=vec[:, o_qg + j:o_qg + j + 1], in1=rstd[:, :],
                    op0=ALU.mult, op1=ALU.mult), reads=[cq, vec, rstd], writes=[cqn])
            yield
            rstd = nrm.stats([ckv[:, j, :] for j in range(2)], [ckv] * 2, KVLORA, EPS)
            yield
            for j in range(2):
                k.dve.op(lambda j=j: nc.vector.scalar_tensor_tensor(
                    out=ckvn[:, j, :], in0=ckv[:, j, :], scalar=vec[:, o_kg + j:o_kg + j + 1], in1=rstd[:, :],
                    op0=ALU.mult, op1=ALU.mult), reads=[ckv, vec, rstd], writes=[ckvn])
            yield
            pa = proj([wA[:, c, 640:768] for c in range(8)], [h[:, c, :] for c in range(8)], [wA, h])
            k.dve.op(lambda pa=pa: nc.vector.tensor_tensor(out=t1[:, :], in0=pa[:, :], in1=cos[:, :], op=ALU.mult),
                     reads=[pa, cos], writes=[t1])
            yield
            pb = proj([wA[:, c, 768:896] for c in range(8)], [h[:, c, :] for c in range(8)], [wA, h])
            k.dve.op(lambda pb=pb: nc.vector.tensor_tensor(out=t2[:, :], in0=pb[:, :], in1=sin[:, :], op=ALU.mult),
                     reads=[pb, sin], writes=[t2])
            k.pool.op(lambda: nc.gpsimd.tensor_tensor(out=kr_b[b][:, :], in0=t1[:, :], in1=t2[:, :], op=ALU.add),
                      reads=[t1, t2], writes=[kr_b[b]])
            yield
            for i in range(nh):
                p = proj([wk[:, c, i * 128:(i + 1) * 128] for c in range(2)], [ckvn[:, c, :] for c in range(2)],
                         [wk, ckvn])
                k.act.op(lambda p=p, i=i: nc.scalar.activation(out=kn_b[i][b][:, :], in_=p[:, :], func=AF.Copy),
                         reads=[p], writes=[kn_b[i][b]])
                yield
            for s_ in range(TB // 128):
                p = proj([ckvn[:, c, s_ * 128:(s_ + 1) * 128] for c in range(2)], [wv[:, c, :] for c in range(2)],
                         [wv, ckvn], n=nh * 128)
                k.act.op(lambda p=p, s_=s_: nc.scalar.activation(out=vc_b[b][:, s_, :], in_=p[:, 0:nh * 128],
                                                                 func=AF.Copy), reads=[p], writes=[vc_b[b]])
                yield
            for i in range(nh):
                p = proj([wq[:, c, i * 128:(i + 1) * 128] for c in range(3)], [cqn[:, c, :] for c in range(3)],
                         [wq, cqn])
                k.act.op(lambda p=p, i=i: nc.scalar.activation(out=qn_[i][:, :], in_=p[:, :], func=AF.Copy),
                         reads=[p], writes=[qn_[i]])
                yield
            for pr in range(nh):
                pa = proj([wq[:, c, ro + pr * 128:ro + (pr + 1) * 128] for c in range(3)],
                          [cqn[:, c, :] for c in range(3)], [wq, cqn])
                k.dve.op(lambda pa=pa: nc.vector.tensor_tensor(out=t1[:, :], in0=pa[:, :], in1=cos[:, :],
                                                               op=ALU.mult), reads=[pa, cos], writes=[t1])
                yield
                pb = proj([wq[:, c, so + pr * 128:so + (pr + 1) * 128] for c in range(3)],
                          [cqn[:, c, :] for c in range(3)], [wq, cqn])
                k.dve.op(lambda pb=pb: nc.vector.tensor_tensor(out=t2[:, :], in0=pb[:, :], in1=sin[:, :],
                                                               op=ALU.mult), reads=[pb, sin], writes=[t2])
                k.pool.op(lambda pr=pr: nc.gpsimd.tensor_tensor(out=qr_[pr][:, :], in0=t1[:, :], in1=t2[:, :],
                                                                op=ALU.add), reads=[t1, t2], writes=[qr_[pr]])
                yield

        def drain(g):
            if g is not None:
                for _ in g:
                    pass

        drain(prep(0))
        for b in range(nblk):
            t0 = b * TB
            qn_, qr_ = qn2[b % 2], qr2[b % 2]
            gnext = prep(b + 1) if b + 1 < nblk else None
            nkt = (t0 + TB) // 128
            its = [(i, kt) for i in range(nh) for kt in range(nkt)]
            sb_of = {}

            def issue_s(n):
                nonlocal ipt
                i, kt = its[n]
                s = ps_s[ipt % 3]
                ipt += 1
                bb, ks = kt // 4, slice((kt % 4) * 128, (kt % 4 + 1) * 128)
                pairs = [(kn_b[i][bb][:, ks], qn_[i][:, :]), (kr_b[bb][:, ks], qr_[i][:, :])]
                d_ = kt - (t0 // 128)
                if d_ >= 0:
                    pairs.append((ident[:, :], masks[:, d_, :]))
                mm(k, s, s[:, :], pairs, [kn_b[i][bb], qn_[i], kr_b[bb], qr_[i], ident, masks])
                sb_of[n] = s

            for n in range(min(2, len(its))):
                issue_s(n)
            for n, (i, kt) in enumerate(its):
                if n + 2 < len(its):
                    issue_s(n + 2)
                if gnext is not None:
                    try:
                        next(gnext)
                    except StopIteration:
                        gnext = None
                s = sb_of.pop(n)
                pt = pT[n % 3]
                k.act.op(lambda s=s, pt=pt: nc.scalar.activation(out=pt[:, :], in_=s[:, :], func=AF.Exp,
                                                                 scale=scale), reads=[s], writes=[pt])
                bb = kt // 4
                k.pe.op(lambda pt=pt, kt=kt, i=i, bb=bb: nc.tensor.matmul(
                    ps_o[:, :], lhsT=vc_b[bb][:, kt % 4, i * 128:(i + 1) * 128], rhs=pt[:, :], start=(kt == 0),
                    stop=(kt == nkt - 1)), reads=[vc_b[bb], pt], writes=[ps_o], inc=True)
                if kt % 3 == 2:
                    k.pe.op(lambda pt=pt, kt=kt: nc.tensor.matmul(ps_l[:, :], lhsT=ones[:, :], rhs=pt[:, :],
                                                                  start=(kt == 2), stop=False),
                            reads=[ones, pt], writes=[ps_l], inc=True)
                else:
                    pacc = paccs[kt % 3]
                    e_ = k.dve if kt % 3 == 0 else k.pool
                    if kt < 2:
                        e_.op(lambda pt=pt, e_=e_, pacc=pacc: e_.eng.tensor_copy(out=pacc[:, :], in_=pt[:, :]),
                              reads=[pt], writes=[pacc])
                    else:
                        e_.op(lambda pt=pt, e_=e_, pacc=pacc: e_.eng.tensor_tensor(out=pacc[:, :], in0=pacc[:, :],
                                                                                   in1=pt[:, :], op=ALU.add),
                              reads=[pt, pacc], writes=[pacc])
                if kt == nkt - 1:
                    k.act.op(lambda: nc.scalar.activation(out=oraw[:, :], in_=ps_o[:, :], func=AF.Copy),
                             reads=[ps_o], writes=[oraw])
                    k.pe.op(lambda: nc.tensor.matmul(ps_l[:, :], lhsT=ones_f[:, :], rhs=paccs[0][:, :], start=False,
                                                     stop=False), reads=[ones_f, paccs[0]], writes=[ps_l], inc=False)
                    k.pe.op(lambda: nc.tensor.matmul(ps_l[:, :], lhsT=ones_f[:, :], rhs=paccs[1][:, :], start=False,
                                                     stop=True), reads=[ones_f, paccs[1]], writes=[ps_l], inc=True)
                    k.dve.op(lambda: nc.vector.reciprocal(out=rcp[:, :], in_=ps_l[:, :]), reads=[ps_l], writes=[rcp])
                    o = oT[i % 2]
                    k.dve.op(lambda o=o: nc.vector.tensor_tensor(out=o[:, :], in0=oraw[:, :], in1=rcp[:, :],
                                                                 op=ALU.mult), reads=[oraw, rcp], writes=[o])
                    k.sp.dma(dr["yT"][:, heads[i], t0:t0 + TB], o[:, :], o, reads=[o], writes=[dr["yT_buf"]])
            drain(gnext)
```
